# Optimizing a Trainium2 kernel written in Bass

```python
import jax
import jax.numpy as jnp
from jax import lax
import numpy as np

D_MODEL = 1024
BATCH = 16
SEQ = 2048
DEPTH = 1

SB_HEAD_DIM = 64
SB_WIDTH = D_MODEL
SB_HEADS = SB_WIDTH // SB_HEAD_DIM
QUERY_BLOCK = 128
ML_HEADS = 4
ML_WIDTH = D_MODEL
ML_HEAD_DIM = ML_WIDTH // ML_HEADS
ML_CHUNK = 64
CONV_WIDTH = 4
N_EXPERTS = 32
TOP_K = 4
D_FF = D_MODEL
SWIGLU_LIMIT = 7.0
SWIGLU_ALPHA = 1.702
MOE_BLOCK = 128
N_MOD = 6
EPS = 1e-6
IN_SPLIT = (SB_WIDTH, SB_WIDTH, SB_WIDTH, 2 * ML_WIDTH, ML_WIDTH, ML_WIDTH, ML_HEADS, ML_HEADS, D_MODEL, D_MODEL)
N_IN = 3 * SB_WIDTH + 4 * ML_WIDTH + 2 * ML_HEADS + 2 * D_MODEL

kernel_name = "hybrid_stickbreak_mlstm_moe_adaln"


def rmsnorm(x, g):
    xf = x.astype(jnp.float32)
    y = xf * lax.rsqrt(jnp.mean(xf * xf, axis=-1, keepdims=True) + EPS)
    return (y * g.astype(jnp.float32)).astype(x.dtype)


def split_heads(t, n_heads):
    b, s, w = t.shape
    return t.reshape(b, s, n_heads, w // n_heads).transpose(0, 2, 1, 3)


def merge_heads(t):
    b, h, s, d = t.shape
    return t.transpose(0, 2, 1, 3).reshape(b, s, h * d)


def stick_breaking_attention(q, k, v):
    seq = q.shape[2]
    scale = q.shape[-1] ** -0.5
    outs = []
    for blk in range(seq // QUERY_BLOCK):
        t0 = blk * QUERY_BLOCK
        t1 = t0 + QUERY_BLOCK
        z = jnp.einsum('bhtd,bhsd->bhts', q[:, :, t0:t1], k[:, :, :t1],
                       preferred_element_type=jnp.float32) * scale
        strict = jnp.arange(t1)[None, :] < jnp.arange(t0, t1)[:, None]
        log_beta = jax.nn.log_sigmoid(z)
        log_keep = jnp.where(strict, jax.nn.log_sigmoid(-z), 0.0)
        between = lax.cumsum(log_keep, axis=3, reverse=True) - log_keep
        a = jnp.where(strict, jnp.exp(log_beta + between), 0.0)
        outs.append(jnp.einsum('bhts,bhsd->bhtd', a, v[:, :, :t1].astype(jnp.float32)))
    return jnp.concatenate(outs, axis=2).astype(v.dtype)


def causal_conv(x, w, b):
    width = w.shape[0]
    seq = x.shape[1]
    xp = jnp.pad(x, ((0, 0), (width - 1, 0), (0, 0)))
    y = b
    for tap in range(width):
        y = y + xp[:, tap:tap + seq] * w[tap]
    return y


def mlstm_chunkwise(q, k, v, i_pre, f_pre):
    f32 = jnp.float32
    bsz, nh, seq, dk = q.shape
    dv = v.shape[-1]
    L = ML_CHUNK
    nc = seq // L
    q = q.astype(f32)
    k = k.astype(f32) * (dk ** -0.5)
    v = v.astype(f32)
    ig = i_pre.astype(f32)
    lf = jax.nn.log_sigmoid(f_pre.astype(f32))

    def to_chunks(t):
        t = t.reshape(bsz, nh, nc, L, *t.shape[3:])
        return jnp.moveaxis(t, 2, 0)

    incl = jnp.tril(jnp.ones((L, L), dtype=bool))

    def step(carry, xs):
        c_mat, n_vec, m_prev = carry
        qc, kc, vc, ic, fc = xs
        b = jnp.cumsum(fc, axis=-1)
        d = jnp.where(incl, b[..., :, None] - b[..., None, :] + ic[..., None, :], -jnp.inf)
        m_row = jnp.maximum(b + m_prev[..., None], jnp.max(d, axis=-1))
        w_inter = jnp.exp(b + m_prev[..., None] - m_row)
        w_intra = jnp.exp(d - m_row[..., None])
        s = jnp.einsum('bhtd,bhsd->bhts', qc, kc) * w_intra
        num = (w_inter[..., None] * jnp.einsum('bhtd,bhde->bhte', qc, c_mat)
               + jnp.einsum('bhts,bhse->bhte', s, vc))
        den = w_inter * jnp.einsum('bhtd,bhd->bht', qc, n_vec) + jnp.sum(s, axis=-1)
        h = num / jnp.maximum(jnp.abs(den), jnp.exp(-m_row))[..., None]
        m_new = m_row[..., -1]
        decay = jnp.exp(b[..., -1] + m_prev - m_new)
        w_state = jnp.exp(b[..., -1:] - b + ic - m_new[..., None])
        wk = w_state[..., None] * kc
        c_new = decay[..., None, None] * c_mat + jnp.einsum('bhsd,bhse->bhde', wk, vc)
        n_new = decay[..., None] * n_vec + jnp.sum(wk, axis=2)
        return (c_new, n_new, m_new), h

    init = (jnp.zeros((bsz, nh, dk, dv), f32), jnp.zeros((bsz, nh, dk), f32), jnp.zeros((bsz, nh), f32))
    _, h = lax.scan(step, init, (to_chunks(q), to_chunks(k), to_chunks(v), to_chunks(ig), to_chunks(lf)))
    return jnp.moveaxis(h, 0, 2).reshape(bsz, nh, seq, dv)


def hybrid_mixer(h, w_in, conv_w, conv_b, b_i, b_f, norm_g, w_a, w_b, w_out):
    proj = h @ w_in
    cuts = np.cumsum(IN_SPLIT)[:-1].tolist()
    sb_q, sb_k, sb_v, ml_qk, ml_v, ml_o, ml_i, ml_f, g_a, g_b = jnp.split(proj, cuts, axis=-1)
    y_a = merge_heads(stick_breaking_attention(split_heads(sb_q, SB_HEADS), split_heads(sb_k, SB_HEADS),
                                               split_heads(sb_v, SB_HEADS)))
    ml_qk = jax.nn.silu(causal_conv(ml_qk, conv_w, conv_b))
    ml_q, ml_k = jnp.split(ml_qk, 2, axis=-1)
    i_pre = (ml_i + b_i).transpose(0, 2, 1)
    f_pre = (ml_f + b_f).transpose(0, 2, 1)
    hb = mlstm_chunkwise(split_heads(ml_q, ML_HEADS), split_heads(ml_k, ML_HEADS),
                         split_heads(ml_v, ML_HEADS), i_pre, f_pre)
    hb = rmsnorm(hb, norm_g.reshape(ML_HEADS, 1, ML_HEAD_DIM))
    y_b = merge_heads(hb).astype(h.dtype) * jax.nn.sigmoid(ml_o)
    merged = jax.nn.sigmoid(g_a) * (y_a @ w_a) + jax.nn.sigmoid(g_b) * (y_b @ w_b)
    return merged @ w_out


def clamped_swiglu_expert(xb, w1, b1, w2, b2):
    a = xb @ w1 + b1
    x_glu, x_lin = jnp.split(a, 2, axis=-1)
    x_glu = jnp.minimum(x_glu, SWIGLU_LIMIT)
    x_lin = jnp.clip(x_lin, -SWIGLU_LIMIT, SWIGLU_LIMIT)
    return ((x_lin + 1.0) * (x_glu * jax.nn.sigmoid(SWIGLU_ALPHA * x_glu))) @ w2 + b2


def routed_moe(h, router_w, router_b, w1, b1, w2, b2):
    bsz, seq, d = h.shape
    n_tok = bsz * seq
    n_assign = n_tok * TOP_K
    xt = h.reshape(n_tok, d)
    logits = (xt @ router_w + router_b).astype(jnp.float32)
    top_logit, top_idx = lax.top_k(logits, TOP_K)
    top_w = jax.nn.softmax(top_logit, axis=-1)
    flat_e = top_idx.reshape(-1)
    flat_tok = jnp.repeat(jnp.arange(n_tok, dtype=jnp.int32), TOP_K)
    flat_w = top_w.reshape(-1)
    order = jnp.argsort(flat_e)
    sorted_e = flat_e[order]
    counts = jnp.bincount(flat_e, length=N_EXPERTS)
    padded = (counts + MOE_BLOCK - 1) // MOE_BLOCK * MOE_BLOCK
    start = jnp.cumsum(counts) - counts
    pad_end = jnp.cumsum(padded)
    pad_start = pad_end - padded
    dest = pad_start[sorted_e] + jnp.arange(n_assign, dtype=jnp.int32) - start[sorted_e]
    n_rows = n_assign + N_EXPERTS * MOE_BLOCK
    n_blocks = n_rows // MOE_BLOCK
    row_tok = jnp.zeros((n_rows,), jnp.int32).at[dest].set(flat_tok[order])
    row_w = jnp.zeros((n_rows,), jnp.float32).at[dest].set(flat_w[order])
    block_e = jnp.minimum(jnp.searchsorted(pad_end, jnp.arange(n_blocks, dtype=jnp.int32) * MOE_BLOCK,
                                           side='right'), N_EXPERTS - 1)
    xs = xt[row_tok].reshape(n_blocks, MOE_BLOCK, d)

    def run_block(args):
        xb, e = args
        return clamped_swiglu_expert(xb, w1[e], b1[e], w2[e], b2[e])

    rows = lax.map(run_block, (xs, block_e)).reshape(n_rows, d)
    y = jnp.zeros((n_tok, d), h.dtype).at[row_tok].add((rows * row_w[:, None]).astype(h.dtype))
    return y.reshape(bsz, seq, d)


def setup_inputs(seed: int = 0) -> dict:
    key = jax.random.key(seed)
    ks = jax.random.split(key, 24)
    f32 = jnp.float32
    nrm = lambda k, shape, s: jax.random.normal(k, shape, f32) * s
    return {
        "x": nrm(ks[0], (BATCH, SEQ, D_MODEL), 1.0),
        "c": nrm(ks[1], (BATCH, D_MODEL), 1.0),
        "ada_w": nrm(ks[2], (DEPTH, D_MODEL, N_MOD * D_MODEL), 0.2 * D_MODEL ** -0.5),
        "ada_b": nrm(ks[3], (DEPTH, N_MOD * D_MODEL), 0.02),
        "norm1_g": 1.0 + nrm(ks[4], (DEPTH, D_MODEL), 0.02),
        "w_in": nrm(ks[5], (DEPTH, D_MODEL, N_IN), D_MODEL ** -0.5),
        "conv_w": nrm(ks[6], (DEPTH, CONV_WIDTH, 2 * ML_WIDTH), CONV_WIDTH ** -0.5),
        "conv_b": nrm(ks[7], (DEPTH, 2 * ML_WIDTH), 0.02),
        "ml_b_i": nrm(ks[8], (DEPTH, ML_HEADS), 0.1),
        "ml_b_f": jnp.broadcast_to(jnp.linspace(3.0, 6.0, ML_HEADS, dtype=f32), (DEPTH, ML_HEADS))
                   + nrm(ks[9], (DEPTH, ML_HEADS), 0.1),
        "ml_norm_g": 1.0 + nrm(ks[10], (DEPTH, ML_WIDTH), 0.02),
        "w_branch_a": nrm(ks[11], (DEPTH, SB_WIDTH, D_MODEL), SB_WIDTH ** -0.5),
        "w_branch_b": nrm(ks[12], (DEPTH, ML_WIDTH, D_MODEL), ML_WIDTH ** -0.5),
        "w_out": nrm(ks[13], (DEPTH, D_MODEL, D_MODEL), D_MODEL ** -0.5),
        "norm2_g": 1.0 + nrm(ks[14], (DEPTH, D_MODEL), 0.02),
        "router_w": nrm(ks[15], (DEPTH, D_MODEL, N_EXPERTS), D_MODEL ** -0.5),
        "router_b": nrm(ks[16], (DEPTH, N_EXPERTS), 0.01),
        "expert_w1": nrm(ks[17], (DEPTH, N_EXPERTS, D_MODEL, 2 * D_FF), D_MODEL ** -0.5),
        "expert_b1": nrm(ks[18], (DEPTH, N_EXPERTS, 2 * D_FF), 0.01),
        "expert_w2": nrm(ks[19], (DEPTH, N_EXPERTS, D_FF, D_MODEL), D_FF ** -0.5),
        "expert_b2": nrm(ks[20], (DEPTH, N_EXPERTS, D_MODEL), 0.01),
        "final_g": 1.0 + nrm(ks[21], (D_MODEL,), 0.02),
    }


def reference(x, c, ada_w, ada_b, norm1_g, w_in, conv_w, conv_b, ml_b_i, ml_b_f, ml_norm_g,
              w_branch_a, w_branch_b, w_out, norm2_g, router_w, router_b,
              expert_w1, expert_b1, expert_w2, expert_b2, final_g):
    for layer in range(DEPTH):
        mod = (jax.nn.silu(c) @ ada_w[layer] + ada_b[layer])[:, None, :]
        sh1, sc1, g1, sh2, sc2, g2 = jnp.split(mod, N_MOD, axis=-1)
        h = rmsnorm(x, norm1_g[layer]) * (1.0 + sc1) + sh1
        x = x + g1 * hybrid_mixer(h, w_in[layer], conv_w[layer], conv_b[layer], ml_b_i[layer], ml_b_f[layer],
                                  ml_norm_g[layer], w_branch_a[layer], w_branch_b[layer], w_out[layer])
        h = rmsnorm(x, norm2_g[layer]) * (1.0 + sc2) + sh2
        x = x + g2 * routed_moe(h, router_w[layer], router_b[layer], expert_w1[layer], expert_b1[layer],
                                expert_w2[layer], expert_b2[layer])
    return rmsnorm(x, final_g)
```

```python
import numpy as np
from contextlib import ExitStack
import ml_dtypes
import concourse.bass as bass
import concourse.mybir as mybir
from concourse.bass_utils import run_bass_kernel_spmd

F32 = mybir.dt.float32
BF16 = mybir.dt.bfloat16
F32R = mybir.dt.float32r
AF = mybir.ActivationFunctionType
ALU = mybir.AluOpType

NCORES = 8
D_MODEL = 1024
SEQ = 2048
NTOK = 2 * SEQ
NE = 32
EPS = 1e-6
KC = 8
COL_Q, COL_K, COL_V = 0, 1024, 2048
COL_MQ, COL_MK, COL_MV, COL_MO = 3072, 4096, 5120, 6144
COL_I, COL_F, COL_GA, COL_GB = 7168, 7172, 7176, 8200


class _Op:
    __slots__ = ("id", "eng", "fn", "deps", "chan", "chan_val", "sig")


class _SemPool:
    def __init__(self, nc):
        self.nc = nc
        self.esem = {e: nc.alloc_semaphore(f"eng_{e}") for e in ("pe", "act", "dve", "pool", "sp")}
        self.ecnt = {e: 0 for e in self.esem}
        self.csem = {}
        self.ccnt = {}

    def chan(self, c):
        if c not in self.csem:
            self.csem[c] = self.nc.alloc_semaphore(f"ch_{len(self.csem)}")
            self.ccnt[c] = 0
        return self.csem[c]


_POOLS = {}


class Prog:
    def __init__(self, nc, name):
        self.nc = nc
        self.name = name
        self.es = ExitStack()
        self.ops = []
        self.lastw = {}
        self.readers = {}
        self.chan_n = {}
        self.nsb = 0
        if id(nc) not in _POOLS:
            _POOLS.clear()
            _POOLS[id(nc)] = _SemPool(nc)
        self.pool = _POOLS[id(nc)]

    def sb(self, shape, dt=F32, name=None):
        self.nsb += 1
        return self.es.enter_context(self.nc.sbuf_tensor(f"{self.name}_s{self.nsb}_{name or ''}", list(shape), dt))

    def ps(self, shape=(128, 512), dt=F32, name=None):
        self.nsb += 1
        return self.es.enter_context(self.nc.psum_tensor(f"{self.name}_p{self.nsb}_{name or ''}", list(shape), dt))

    def op(self, eng, fn, r=(), w=(), chan=None):
        o = _Op()
        o.id = len(self.ops)
        o.eng = eng
        o.fn = fn
        o.chan = chan
        o.chan_val = 0
        o.sig = 0
        deps = {}
        for k in r:
            d = self.lastw.get(k)
            if d is not None:
                deps[d] = "raw"
        for k in w:
            d = self.lastw.get(k)
            if d is not None and d not in deps:
                deps[d] = "waw"
            for d in self.readers.get(k, {}).values():
                if d not in deps:
                    deps[d] = "war"
        o.deps = deps
        if chan is not None:
            self.pool.chan(chan)
            self.pool.ccnt[chan] += 1
            self.chan_n[chan] = self.pool.ccnt[chan]
            o.chan_val = 16 * self.chan_n[chan]
        self.ops.append(o)
        rk = (eng, chan)
        for k in r:
            self.readers.setdefault(k, {})[rk] = o.id
        for k in w:
            self.lastw[k] = o.id
            self.readers[k] = {}
        return o.id

    def mm(self, out, lhsT, rhs, start, stop, r, w, sgc=False):
        if sgc:
            return self.op("pe", lambda e: e.matmul(out, lhsT, rhs, start=start, stop=stop, skip_group_check=True), r, w)
        return self.op("pe", lambda e: e.matmul(out, lhsT, rhs, start=start, stop=stop), r, w)

    def dma(self, eng, out, in_, r, w, chan):
        return self.op(eng, lambda e: e.dma_start(out=out, in_=in_), r, w, chan=chan)

    def _skip(self, p, o, typ):
        return (p.chan is None and o.chan is None and p.eng == o.eng and p.eng == "pe")

    def run(self):
        nc = self.nc
        ops = self.ops
        engs = ("pe", "act", "dve", "pool", "sp")
        need = [False] * len(ops)
        for o in ops:
            for d, typ in o.deps.items():
                p = ops[d]
                if p.chan is not None or self._skip(p, o, typ):
                    continue
                need[d] = True
        cnt = self.pool.ecnt
        for o in ops:
            if o.chan is None and need[o.id]:
                cnt[o.eng] += 1
                o.sig = cnt[o.eng]
        with ExitStack() as es:
            esem = self.pool.esem
            csem = self.pool.csem
            by_eng = {e: [o for o in ops if o.eng == e] for e in engs}
            with nc.Block() as block:
                def emit(e, name):
                    waited = {}
                    for o in by_eng[name]:
                        want = {}
                        for d, typ in o.deps.items():
                            p = ops[d]
                            if p.chan is not None:
                                key, val = ("c", p.chan), p.chan_val
                            else:
                                if self._skip(p, o, typ):
                                    continue
                                key, val = ("e", p.eng), p.sig
                            if val > want.get(key, 0):
                                want[key] = val
                        for key, val in want.items():
                            if waited.get(key, 0) >= val:
                                continue
                            waited[key] = val
                            e.wait_ge(csem[key[1]] if key[0] == "c" else esem[key[1]], val)
                        ins = o.fn(e)
                        if o.chan is not None:
                            ins.then_inc(csem[o.chan], 16)
                        elif o.sig:
                            ins.then_inc(esem[name], 1)
                    if name == "sp":
                        for c, n in self.chan_n.items():
                            if waited.get(("c", c), 0) < 16 * n:
                                e.wait_ge(csem[c], 16 * n)

                @block.tensor
                def _(e):
                    emit(e, "pe")

                @block.scalar
                def _(e):
                    emit(e, "act")

                @block.vector
                def _(e):
                    emit(e, "dve")

                @block.gpsimd
                def _(e):
                    emit(e, "pool")

                @block.sync
                def _(e):
                    emit(e, "sp")
        self.es.close()


def _tap(nc, taps, name, ap, shape, dt=F32):
    if taps is None or name not in taps:
        return
    d = nc.dram_tensor("dbg_" + name, list(shape), dt, kind="ExternalOutput").ap()
    if id(nc) not in _POOLS:
        _POOLS.clear()
        _POOLS[id(nc)] = _SemPool(nc)
    pool = _POOLS[id(nc)]
    s = pool.chan("tap")
    pool.ccnt["tap"] += 1
    v = 16 * pool.ccnt["tap"]
    with nc.Block() as blk:
        @blk.sync
        def _(e):
            e.dma_start(out=d, in_=ap).then_inc(s, 16)
            e.wait_ge(s, v)


def build(upto=99, taps=None):
    nc = bass.Bass("TRN2", target_bir_lowering=False)
    D = {}

    def din(name, shape, dt=F32):
        D[name] = nc.dram_tensor(name, list(shape), dt, kind="ExternalInput").ap()

    din("xT", [128, KC, NTOK]); din("cT", [128, KC, 2]); din("ada_w", [1024, 6144]); din("ada_bT", [128, 48])
    din("n1g", [128, KC]); din("n2g", [128, KC]); din("fg", [128, KC]); din("mlg", [128, KC])
    din("w_in", [1024, 9224]); din("cwT", [128, 16, 4]); din("cbT", [128, 16]); din("bi", [4, 1]); din("bf", [4, 1])
    din("w_a", [1024, 1024]); din("w_b", [1024, 1024]); din("w_out", [1024, 1024])
    din("router_w", [1024, NE]); din("rb_rep", [128, NE])
    din("w1", [NE, 1024, 2048]); din("b1T", [128, NE, 16]); din("w2", [NE, 1024, 1024]); din("b2", [NE, 1024])
    din("c_ones", [128, 128]); din("c_tri", [128, 128]); din("c_maskD", [128, 4, 512]); din("c_maskLE", [128, 128])
    din("c_ident", [128, 128]); din("c_identb", [128, 128], BF16); din("c_sel4", [4, 4, 128]); din("c_pid", [128, 128])
    outT = nc.dram_tensor("outT", [128, KC, NTOK], F32, kind="ExternalOutput").ap()

    def wview(ap2d):
        return ap2d.rearrange("(kc p) n -> p kc n", p=128)

    with ExitStack() as top:
        def tsb(name, shape, dt=F32):
            return top.enter_context(nc.sbuf_tensor("g_" + name, list(shape), dt))

        modT = tsb("modT", [128, 48, 2])
        A1 = tsb("A1", [128, KC, 2]); A2 = tsb("A2", [128, KC, 2])
        ones = tsb("ones", [128, 128]); n1g = tsb("n1g", [128, KC]); n2g = tsb("n2g", [128, KC])
        fg = tsb("fg", [128, KC]); mlg = tsb("mlg", [128, KC])
        ident = tsb("ident", [128, 128])
        hT = tsb("hT", [128, KC, SEQ], BF16)
        Y = tsb("Y", [128, 2, KC, SEQ], BF16)
        yaT = Y[:, 0]
        ybT = Y[:, 1]
        x1T = Y[:].rearrange("p a k t -> p (a k t)").bitcast(F32).rearrange("p (k t) -> p k t", k=KC)

        P = Prog(nc, "p0")
        cT = P.sb([128, KC, 2], name="cT"); sc = P.sb([128, KC, 2], name="sc")
        abT = P.sb([128, 48], name="abT")
        wbuf = [P.sb([128, KC, 1024], name=f"adaw{i}") for i in range(2)]
        pm = [P.ps(name=f"pm{i}") for i in range(2)]
        for i, (t, nm) in enumerate([(ones, "c_ones"), (n1g, "n1g"), (n2g, "n2g"), (fg, "fg"), (mlg, "mlg"),
                                     (ident, "c_ident"), (cT, "cT"), (abT, "ada_bT")]):
            P.dma("sp", t[:], D[nm], r=(), w=[nm], chan="ld" + nm)
        P.op("act", lambda e: e.activation(out=sc[:], in_=cT[:], func=AF.Silu), r=["cT"], w=["sc"])
        adaw = wview(D["ada_w"])
        for m in range(6):
            wb = wbuf[m % 2]
            P.dma("sp", wb[:], adaw[:, :, m * 1024:(m + 1) * 1024], r=(), w=[f"adaw{m % 2}"], chan=f"adaw{m % 2}")
            pmm = pm[m % 2][:, 0:16].rearrange("p (j b) -> p j b", b=2)
            for jj in range(8):
                for kc in range(KC):
                    P.mm(pmm[:, jj, :], wb[:, kc, jj * 128:(jj + 1) * 128], sc[:, kc, :], kc == 0, kc == KC - 1,
                         r=[f"adaw{m % 2}", "sc"], w=[f"pm{m % 2}"])
            for b in range(2):
                P.op("dve", lambda e, m=m, b=b, pmm=pmm: e.tensor_tensor(
                    modT[:, m * 8:(m + 1) * 8, b], pmm[:, :, b], abT[:, m * 8:(m + 1) * 8], ALU.add),
                    r=[f"pm{m % 2}", "ada_bT"], w=["modT"])
        for b in range(2):
            P.op("dve", lambda e, b=b: e.scalar_tensor_tensor(A1[:, :, b], modT[:, 8:16, b], 1.0, n1g[:], ALU.add, ALU.mult),
                 r=["modT", "n1g"], w=["A1"])
            P.op("dve", lambda e, b=b: e.scalar_tensor_tensor(A2[:, :, b], modT[:, 32:40, b], 1.0, n2g[:], ALU.add, ALU.mult),
                 r=["modT", "n2g"], w=["A2"])
        P.run()
        _tap(nc, taps, "modT", modT[:], [128, 48, 2])
        _tap(nc, taps, "A1", A1[:], [128, KC, 2])

        def SH1(kc, b): return modT[:, 0 + kc, b:b + 1]
        def G1(kc, b): return modT[:, 16 + kc, b:b + 1]
        def SH2(kc, b): return modT[:, 24 + kc, b:b + 1]
        def G2(kc, b): return modT[:, 40 + kc, b:b + 1]

        def norm_phase(name, b, src_dram, src_sb, Aap, SHap, dst_bf, extra=None):
            P = Prog(nc, name)
            xt = [P.sb([128, KC, 512], name=f"xt{i}") for i in range(2)] if src_sb is None else None
            sq = [P.sb([128, KC, 512], name=f"sq{i}") for i in range(2)]
            rt = [P.sb([128, 512], name=f"rt{i}") for i in range(2)]
            rs = [P.sb([128, 512], name=f"rs{i}") for i in range(2)]
            tmp = [P.sb([128, 512], name=f"tmp{i}") for i in range(4)]
            pss = [P.ps(name=f"ss{i}") for i in range(2)]
            ctx = extra(P) if extra else None
            for tt in range(4):
                s = tt % 2
                tok = slice(tt * 512, (tt + 1) * 512)
                if src_sb is None:
                    P.dma("sp", xt[s][:], src_dram[:, :, b * SEQ + tt * 512: b * SEQ + (tt + 1) * 512], r=(), w=[f"xt{s}"], chan=f"xt{s}")
                    xin = xt[s]
                    xk = [f"xt{s}"]
                    xv = lambda kc, xin=xin: xin[:, kc, :]
                    xall = xin[:]
                else:
                    xk = [f"x1.{tt}"]
                    xv = lambda kc, tok=tok: src_sb[:, kc, tok]
                    xall = src_sb[:, :, tok]
                P.op("act", lambda e, s=s, xall=xall: e.activation(out=sq[s][:], in_=xall, func=AF.Square), r=xk, w=[f"sq{s}"])
                for kc in range(KC):
                    P.mm(pss[s][:], ones[:], sq[s][:, kc, :], kc == 0, kc == KC - 1, r=[f"sq{s}"], w=[f"ss{s}"])
                P.op("act", lambda e, s=s: e.activation(out=rt[s][:], in_=pss[s][:], func=AF.Ln, bias=EPS_AP[:], scale=1.0 / D_MODEL),
                     r=[f"ss{s}"], w=[f"rt{s}"])
                P.op("act", lambda e, s=s: e.activation(out=rs[s][:], in_=rt[s][:], func=AF.Exp, scale=-0.5), r=[f"rt{s}"], w=[f"rs{s}"])
                for kc in range(KC):
                    q = kc % 4
                    P.op("dve", lambda e, kc=kc, q=q, s=s, xv=xv: e.scalar_tensor_tensor(
                        tmp[q][:], xv(kc), Aap(kc, b), rs[s][:], ALU.mult, ALU.mult),
                        r=xk + [f"rs{s}"], w=[f"tmp{q}"])
                    if ctx is None:
                        P.op("act", lambda e, kc=kc, q=q, tok=tok: e.activation(
                            out=dst_bf[:, kc, tok], in_=tmp[q][:], func=AF.Identity, bias=SHap(kc, b), scale=1.0),
                            r=[f"tmp{q}"], w=[f"h.{tt}"])
                    else:
                        ctx["per_kc"](P, tt, kc, q, tmp[q], tok)
                if ctx is not None:
                    ctx["per_tile"](P, tt, tok)
            if ctx is not None and "final" in ctx:
                ctx["final"](P)
            P.run()

        EPS_AP = tsb("eps", [128, 1])
        P = Prog(nc, "pc")
        P.op("dve", lambda e: e.memset(EPS_AP[:], EPS), r=(), w=["eps"])
        P.run()

        for b in range(2):
            norm_phase(f"n1_{b}", b, D["xT"], None, lambda kc, b: A1[:, kc, b:b + 1], SH1, hT)
            if b == 0:
                _tap(nc, taps, "hT", hT[:], [128, KC, SEQ], BF16)
            if upto <= 1:
                continue
            scratch = Y[:, 0].rearrange("p k t -> p (k t)").bitcast(F32)
            mlstm_phase(nc, D, b, hT, ybT, scratch, wview, mlg, ones, ident, EPS_AP, taps)
            if b == 0:
                _tap(nc, taps, "ybT", ybT, [128, KC, SEQ], BF16)
            if upto <= 2:
                continue
            sb_attention(nc, D, b, hT, yaT, wview)
            if b == 0:
                _tap(nc, taps, "yaT", yaT, [128, KC, SEQ], BF16)
            if upto <= 3:
                continue
            with nc.sbuf_tensor(f"mergedT{b}", [128, KC, SEQ], BF16) as mergedT:
                merge_phase(nc, D, b, hT, yaT, ybT, mergedT, wview)
                if b == 0:
                    _tap(nc, taps, "mergedT", mergedT[:], [128, KC, SEQ], BF16)
                outproj_phase(nc, D, b, mergedT, x1T, G1, wview)
            if b == 0:
                _tap(nc, taps, "x1T", x1T, [128, KC, SEQ])
            if upto <= 4:
                continue
            with nc.sbuf_tensor(f"gT{b}", [128, SEQ], BF16) as gT:
                norm2_router_phase(nc, D, b, x1T, hT, gT, A2, SH2, G2, ones, ident, EPS_AP, wview)
                if b == 0:
                    _tap(nc, taps, "h2T", hT[:], [128, KC, SEQ], BF16)
                    _tap(nc, taps, "gT", gT[0:NE, :], [NE, SEQ], BF16)
                    _tap(nc, taps, "x1bT", x1T, [128, KC, SEQ])
                if upto <= 5:
                    continue
                moe_phase(nc, D, b, hT, x1T, gT, G2)
            if b == 0:
                _tap(nc, taps, "x2T", x1T, [128, KC, SEQ])
            final_phase(nc, D, b, x1T, fg, ones, EPS_AP, outT)
    return nc


def sb_attention(nc, D, b, hT, yaT, wview):
    P = Prog(nc, f"sb{b}")
    win = wview(D["w_in"])
    ones = P.sb([128, 128], name="ones"); tri = P.sb([128, 128], name="tri")
    maskD = P.sb([128, 4, 512], name="maskD")
    for t, nm in [(ones, "c_ones"), (tri, "c_tri"), (maskD, "c_maskD")]:
        P.dma("sp", t[:], D[nm], r=(), w=[nm], chan="ld" + nm)
    P.op("dve", lambda e: e.tensor_copy(trir[:], tri[:]), r=["c_tri"], w=["trir"])
    P.op("dve", lambda e: e.tensor_copy(onesr[:], ones[:]), r=["c_ones"], w=["onesr"])
    Wq = [P.sb([128, KC, 128], BF16, name=f"wq{i}") for i in range(2)]
    Wk = [P.sb([128, KC, 128], BF16, name=f"wk{i}") for i in range(2)]
    Wv = [P.sb([128, KC, 128], BF16, name=f"wv{i}") for i in range(2)]
    qT = [P.sb([128, SEQ], BF16, name=f"qT{i}") for i in range(2)]
    kTh_ = [[P.sb([128, SEQ], BF16, name=f"kT{i}_{hh}") for hh in range(2)] for i in range(2)]
    V = [P.sb([128, 16, 2, 128], BF16, name=f"V{i}") for i in range(2)]
    for i in range(2):
        P.op("pool", lambda e, i=i: e.memset(kTh_[i][0][64:128, :], 0.0), r=(), w=[f"kz{i}"])
        P.op("pool", lambda e, i=i: e.memset(kTh_[i][1][0:64, :], 0.0), r=(), w=[f"kz{i}"])
        P.op("pool", lambda e, i=i: e.memset(V[i][:], 0.0), r=(), w=[f"vz{i}"])
    et = [P.sb([128, 512], name=f"et{i}") for i in range(4)]
    spt = [P.sb([128, 512], F32R, name=f"spt{i}") for i in range(4)]
    ext = [P.sb([128, 512], name=f"ext{i}") for i in range(4)]
    AT = [P.sb([128, 512], BF16, name=f"AT{i}") for i in range(4)]
    R = [P.sb([128, 512], F32R, name=f"R{i}") for i in range(2)]
    trir = P.sb([128, 128], F32R, name="trir"); onesr = P.sb([128, 128], F32R, name="onesr")
    psz = [P.ps(name=f"z{i}") for i in range(2)]
    pscs = [P.ps(name=f"cs{i}") for i in range(2)]
    psy = [P.ps(name=f"y{i}") for i in range(2)]
    pspj = [P.ps(name=f"pj{i}") for i in range(2)]
    npj = [0]
    it = 0

    def proj_chunks(hp):
        s = hp % 2
        ch = []

        def dmas():
            for (W, col, nm) in [(Wq, COL_Q, "wq"), (Wk, COL_K, "wk"), (Wv, COL_V, "wv")]:
                P.dma("pool", W[s][:], win[:, :, col + hp * 128: col + (hp + 1) * 128], r=(), w=[f"{nm}{s}"], chan=f"{nm}{s}")
        ch.append(dmas)
        for tt in range(4):
            for nm in ("q", "k"):
                def c_(tt=tt, nm=nm):
                    tok = slice(tt * 512, (tt + 1) * 512)
                    W = Wq if nm == "q" else Wk
                    pj = npj[0] % 2
                    npj[0] += 1
                    for kc in range(KC):
                        P.mm(pspj[pj][:], W[s][:, kc, :], hT[:, kc, tok], kc == 0, kc == KC - 1,
                             r=[f"w{nm}{s}", f"h.{tt}"], w=[f"pj{pj}"])
                    if nm == "q":
                        P.op("dve", lambda e, pj=pj, o_=qT[s][:, tok]: e.tensor_scalar(o_, pspj[pj][:], 0.125, None, ALU.mult),
                             r=[f"pj{pj}"], w=[f"qT{s}.{tt}"])
                    else:
                        P.op("dve", lambda e, pj=pj, o_=kTh_[s][0][0:64, tok]: e.tensor_copy(o_, pspj[pj][0:64, :]),
                             r=[f"pj{pj}", f"kz{s}"], w=[f"kT{s}.{tt}"])
                        P.op("dve", lambda e, pj=pj, o_=kTh_[s][1][64:128, tok]: e.tensor_copy(o_, pspj[pj][64:128, :]),
                             r=[f"pj{pj}", f"kz{s}"], w=[f"kT{s}.{tt}"])
                ch.append(c_)
        for g4 in range(4):
            def c_(g4=g4):
                pj = npj[0] % 2
                npj[0] += 1
                pv = pspj[pj][:].rearrange("p (j c) -> p j c", c=128)
                for j in range(4):
                    blk = g4 * 4 + j
                    for kc in range(KC):
                        P.mm(pv[:, j, :], hT[:, kc, blk * 128:(blk + 1) * 128], Wv[s][:, kc, :], kc == 0, kc == KC - 1,
                             r=[f"wv{s}", f"h.{g4}"], w=[f"pj{pj}"])
                P.op("dve", lambda e, o_=V[s][:, g4 * 4:(g4 + 1) * 4, 0, 0:64], pv=pv: e.tensor_copy(o_, pv[:, :, 0:64]),
                     r=[f"pj{pj}", f"vz{s}"], w=[f"V{s}.{g4}"])
                P.op("dve", lambda e, o_=V[s][:, g4 * 4:(g4 + 1) * 4, 1, 64:128], pv=pv: e.tensor_copy(o_, pv[:, :, 64:128]),
                     r=[f"pj{pj}", f"vz{s}"], w=[f"V{s}.{g4}"])
            ch.append(c_)
        return ch

    for c_ in proj_chunks(0):
        c_()
    for hp in range(8):
        s = hp % 2
        nxt = proj_chunks(hp + 1) if hp + 1 < 8 else []
        iters = []
        for qc in range(4):
            nkb = 4 * qc + 4
            for idx, kb in enumerate(range(nkb - 1, -1, -1)):
                for hh in range(2):
                    iters.append((qc, idx, kb, hh))
        base = it
        it += len(iters)
        triS = maskD[:, 0, 0:128]

        def prm(t, iters=iters, base=base):
            qc, idx, kb, hh = iters[t]
            g = base + t
            c0 = 128 * max(0, kb - 4 * qc)
            return qc, idx, kb, hh, g % 4, g % 2, c0

        def SA_mm(t, s=s):
            qc, idx, kb, hh, u, z, c0 = prm(t)
            P.mm(psz[z][:, c0:], kTh_[s][hh][:, kb * 128:(kb + 1) * 128], qT[s][:, qc * 512 + c0:(qc + 1) * 512], True, True,
                 r=[f"kT{s}.{kb // 4}", f"qT{s}.{qc}"], w=[f"z{z}"])

        def SA_exp(t):
            qc, idx, kb, hh, u, z, c0 = prm(t)
            P.op("act", lambda e, u=u, z=z, c0=c0: e.activation(out=et[u][:, c0:], in_=psz[z][:, c0:], func=AF.Exp), r=[f"z{z}"], w=[f"et{u}"])

        def SA_ln(t):
            qc, idx, kb, hh, u, z, c0 = prm(t)
            P.op("act", lambda e, u=u, c0=c0: e.activation(out=spt[u][:, c0:], in_=et[u][:, c0:], func=AF.Ln, bias=1.0, scale=1.0),
                 r=[f"et{u}"], w=[f"spt{u}"])

        def SA_mask(t):
            qc, idx, kb, hh, u, z, c0 = prm(t)
            if kb >= 4 * qc:
                P.op("dve", lambda e, u=u, c0=c0: e.tensor_tensor(spt[u][:, c0:c0 + 128], spt[u][:, c0:c0 + 128], triS, ALU.mult),
                     r=[f"spt{u}", "c_maskD"], w=[f"spt{u}"])
                P.op("dve", lambda e, u=u, c0=c0: e.tensor_tensor(et[u][:, c0:c0 + 128], et[u][:, c0:c0 + 128], triS, ALU.mult),
                     r=[f"et{u}", "c_maskD"], w=[f"et{u}"])

        def SB_mm(t):
            qc, idx, kb, hh, u, z, c0 = prm(t)
            P.mm(pscs[z][:, c0:], trir[:], spt[u][:, c0:], True, idx == 0, r=[f"spt{u}", "trir"], w=[f"cs{z}"])
            if idx > 0:
                P.mm(pscs[z][:, c0:], onesr[:], R[hh][:, c0:], False, True, r=[f"R{hh}", "onesr"], w=[f"cs{z}"])

        def SB_pool(t):
            qc, idx, kb, hh, u, z, c0 = prm(t)
            if kb > 0:
                if idx == 0:
                    if c0 > 0:
                        P.op("dve", lambda e, hh=hh, c0=c0: e.tensor_copy(R[hh][:, 0:c0], maskD[:, 3, 0:c0]), r=["c_maskD"], w=[f"R{hh}"])
                    P.op("dve", lambda e, u=u, hh=hh, c0=c0: e.tensor_copy(R[hh][:, c0:], spt[u][:, c0:]), r=[f"spt{u}"], w=[f"R{hh}"])
                else:
                    P.op("dve", lambda e, u=u, hh=hh, c0=c0: e.tensor_tensor(R[hh][:, c0:], R[hh][:, c0:], spt[u][:, c0:], ALU.add),
                         r=[f"R{hh}", f"spt{u}"], w=[f"R{hh}"])

        def SB_exp(t):
            qc, idx, kb, hh, u, z, c0 = prm(t)
            P.op("act", lambda e, u=u, z=z, c0=c0: e.activation(out=ext[u][:, c0:], in_=pscs[z][:, c0:], func=AF.Exp, scale=-1.0),
                 r=[f"cs{z}"], w=[f"ext{u}"])

        def SB_mult(t):
            qc, idx, kb, hh, u, z, c0 = prm(t)
            P.op("dve", lambda e, u=u, c0=c0: e.tensor_tensor(AT[u][:, c0:], et[u][:, c0:], ext[u][:, c0:], ALU.mult),
                 r=[f"et{u}", f"ext{u}"], w=[f"AT{u}"])

        def SC(t, s=s, hp=hp):
            qc, idx, kb, hh, u, z, c0 = prm(t)
            yb = (hp * 4 + qc) % 2
            P.mm(psy[yb][:, c0:], V[s][:, kb, hh, :], AT[u][:, c0:], idx == 0 and hh == 0, kb == 0 and hh == 1,
                 r=[f"V{s}.{kb // 4}", f"AT{u}"], w=[f"y{yb}.0", f"y{yb}.1"], sgc=True)
            if kb == 0 and hh == 1:
                P.op("dve", lambda e, yb=yb, o_=yaT[:, hp, qc * 512:(qc + 1) * 512]: e.tensor_copy(o_, psy[yb][:]),
                     r=[f"y{yb}.0", f"y{yb}.1"], w=[f"ya.{qc}"])

        n_pairs = len(iters) // 2
        for p in range(n_pairs + 2):
            if nxt and p % 3 == 0:
                nxt.pop(0)()
            if p < n_pairs:
                for f in (SA_mm, SA_exp, SA_ln, SA_mask):
                    f(2 * p)
                    f(2 * p + 1)
            if 0 <= p - 1 < n_pairs:
                for f in (SB_mm, SB_pool, SB_exp, SB_mult):
                    f(2 * (p - 1))
                    f(2 * (p - 1) + 1)
            if 0 <= p - 2 < n_pairs:
                SC(2 * (p - 2))
                SC(2 * (p - 2) + 1)
        while nxt:
            nxt.pop(0)()
    P.run()


def mlstm_phase(nc, D, b, hT, ybT, scratch, wview, mlg, ones, ident, EPS_AP, taps=None):
    win = wview(D["w_in"])
    NUMv = scratch[:, 0:4096].rearrange("p (a t) -> p a t", a=2)
    XP = scratch[:, 4096:6147]
    with ExitStack() as mes:
        def msb(name, shape, dt=F32):
            return mes.enter_context(nc.sbuf_tensor(f"ml{b}_{name}", list(shape), dt))
        Mrow = msb("Mrow", [4, SEQ]); WIrow = msb("WIrow", [4, SEQ]); EMRrow = msb("EMRrow", [4, SEQ])
        GT = msb("GT", [128, 16, 4]); WST = msb("WST", [128, 16, 4]); DEC = msb("DEC", [4, 16])
        sel4 = msb("sel4", [4, 4, 128]); maskLE = msb("maskLE", [128, 128]); identb = msb("identb", [128, 128], BF16)
        cwT = msb("cwT", [128, 16, 4]); cbT = msb("cbT", [128, 16])
        P = Prog(nc, f"mla{b}")
        for t, nm in [(sel4, "c_sel4"), (maskLE, "c_maskLE"), (identb, "c_identb"), (cwT, "cwT"), (cbT, "cbT")]:
            P.dma("sp", t[:], D[nm], r=(), w=[nm], chan="ld" + nm)
        bi = P.sb([4, 1], name="bi"); bfv = P.sb([4, 1], name="bf"); nbf = P.sb([4, 1], name="nbf")
        P.dma("sp", bi[:], D["bi"], r=(), w=["bi"], chan="ldbi")
        P.dma("sp", bfv[:], D["bf"], r=(), w=["bf"], chan="ldbf")
        Wif32 = P.sb([128, KC, 8], name="Wif32")
        Wi = P.sb([128, KC, 4], BF16, name="Wi"); Wf = P.sb([128, KC, 4], BF16, name="Wf")
        P.dma("sp", Wif32[:], win[:, :, COL_I:COL_I + 8], r=(), w=["Wif32"], chan="ldWif")
        P.op("dve", lambda e: e.tensor_copy(Wi[:], Wif32[:, :, 0:4]), r=["Wif32"], w=["Wi"])
        P.op("dve", lambda e: e.tensor_copy(Wf[:], Wif32[:, :, 4:8]), r=["Wif32"], w=["Wf"])
        Grow = P.sb([4, SEQ], name="Grow"); SP = P.sb([4, SEQ], name="SP"); CS = P.sb([4, SEQ], name="CS")
        WSrow = P.sb([4, SEQ], name="WSrow"); Mprev = P.sb([4, 16], name="Mprev")
        psI = [P.ps(name=f"psI{i}") for i in range(2)]
        psF = [P.ps(name=f"psF{i}") for i in range(2)]
        psT = P.ps(name="psT"); psT2 = P.ps(name="psT2")
        P.op("dve", lambda e: e.tensor_scalar(nbf[:], bfv[:], -1.0, None, ALU.mult), r=["bf"], w=["nbf"])
        for tt in range(4):
            s = tt % 2
            tok = slice(tt * 512, (tt + 1) * 512)
            for kc in range(KC):
                P.mm(psI[s][0:4, :], Wi[:, kc, :], hT[:, kc, tok], kc == 0, kc == KC - 1, r=["Wi"], w=[f"psI{s}"])
            for kc in range(KC):
                P.mm(psF[s][0:4, :], Wf[:, kc, :], hT[:, kc, tok], kc == 0, kc == KC - 1, r=["Wf"], w=[f"psF{s}"])
            P.op("dve", lambda e, s=s, tok=tok: e.tensor_scalar(Grow[:, tok], psI[s][0:4, :], bi[:, 0:1], None, ALU.add),
                 r=[f"psI{s}", "bi"], w=["Grow"])
            P.op("act", lambda e, s=s, tok=tok: e.activation(out=SP[:, tok], in_=psF[s][0:4, :], func=AF.Exp, bias=nbf[:, 0:1], scale=-1.0),
                 r=[f"psF{s}", "nbf"], w=["SP"])
        P.op("act", lambda e: e.activation(out=SP[:], in_=SP[:], func=AF.Ln, bias=1.0, scale=1.0), r=["SP"], w=["SP"])
        P.op("dve", lambda e: e.tensor_tensor_scan(CS[:], SP[:], SP[:], 0.0, ALU.add, ALU.max), r=["SP"], w=["CS"])
        P.op("dve", lambda e: e.tensor_tensor(Grow[:], Grow[:], CS[:], ALU.add), r=["Grow", "CS"], w=["Grow"])
        P.op("dve", lambda e: e.tensor_tensor_scan(Mrow[:], Grow[:], Grow[:], 0.0, ALU.max, ALU.max), r=["Grow"], w=["Mrow"])
        M3 = Mrow[:].rearrange("p (c t) -> p c t", t=128)
        G3 = Grow[:].rearrange("p (c t) -> p c t", t=128)
        WI3 = WIrow[:].rearrange("p (c t) -> p c t", t=128)
        WS3 = WSrow[:].rearrange("p (c t) -> p c t", t=128)
        P.op("dve", lambda e: e.memset(Mprev[:, 0:1], 0.0), r=(), w=["Mprev0"])
        P.op("dve", lambda e: e.tensor_copy(Mprev[:, 1:16], M3[:, 0:15, 127]), r=["Mrow"], w=["Mprev"])
        for c in range(16):
            P.op("dve", lambda e, c=c: e.tensor_scalar(WI3[:, c, :], M3[:, c, :], Mprev[:, c:c + 1], None, ALU.subtract),
                 r=["Mrow", "Mprev", "Mprev0"], w=["WIrow"])
            P.op("dve", lambda e, c=c: e.tensor_scalar(WS3[:, c, :], G3[:, c, :], M3[:, c, 127:128], None, ALU.subtract),
                 r=["Mrow", "Grow"], w=["WSrow"])
        P.op("act", lambda e: e.activation(out=WIrow[:], in_=WIrow[:], func=AF.Exp, scale=-1.0), r=["WIrow"], w=["WIrow"])
        P.op("act", lambda e: e.activation(out=WSrow[:], in_=WSrow[:], func=AF.Exp), r=["WSrow"], w=["WSrow"])
        P.op("dve", lambda e: e.tensor_tensor(EMRrow[:], CS[:], Mrow[:], ALU.subtract), r=["CS", "Mrow"], w=["EMRrow"])
        P.op("act", lambda e: e.activation(out=EMRrow[:], in_=EMRrow[:], func=AF.Exp), r=["EMRrow"], w=["EMRrow"])
        P.op("dve", lambda e: e.tensor_tensor(DEC[:], Mprev[:], M3[:, :, 127], ALU.subtract), r=["Mprev", "Mprev0", "Mrow"], w=["DEC"])
        P.op("act", lambda e: e.activation(out=DEC[:], in_=DEC[:], func=AF.Exp), r=["DEC"], w=["DEC"])
        pT = psT[:, 0:64].rearrange("p (c h) -> p c h", h=4)
        pT2 = psT2[:, 0:64].rearrange("p (c h) -> p c h", h=4)
        for blk in range(16):
            P.op("pe", lambda e, blk=blk: e.transpose(pT[:, blk, :], Grow[0:4, blk * 128:(blk + 1) * 128], ident[0:4, 0:4]),
                 r=["Grow"], w=["psT"])
            P.op("pe", lambda e, blk=blk: e.transpose(pT2[:, blk, :], WSrow[0:4, blk * 128:(blk + 1) * 128], ident[0:4, 0:4]),
                 r=["WSrow"], w=["psT2"])
        P.op("dve", lambda e: e.tensor_copy(GT[:], pT), r=["psT"], w=["GT"])
        P.op("dve", lambda e: e.tensor_copy(WST[:], pT2), r=["psT2"], w=["WST"])
        P.run()
        _tap(nc, taps, f"Mrow{b}", Mrow[:], [4, SEQ]); _tap(nc, taps, f"WIrow{b}", WIrow[:], [4, SEQ])
        _tap(nc, taps, f"EMRrow{b}", EMRrow[:], [4, SEQ]); _tap(nc, taps, f"GT{b}", GT[:], [128, 16, 4])
        _tap(nc, taps, f"WST{b}", WST[:], [128, 16, 4]); _tap(nc, taps, f"DEC{b}", DEC[:], [4, 16])

        import os
        ML_STAGE = float(os.environ.get("ML_STAGE", "9"))
        for h in range(4 if ML_STAGE > 0 else 0):
            P = Prog(nc, f"mlh{b}_{h}")
            try:
              _ml_head(P, nc, D, b, h, hT, ybT, scratch, NUMv, XP, win, mlg, ones, EPS_AP, Mrow, WIrow, EMRrow, GT, WST, DEC,
                       sel4, maskLE, identb, cwT, cbT, ML_STAGE)
            except _StopStage:
              pass
            P.run()


class _StopStage(Exception):
    pass


def _ml_head(P, nc, D, b, h, hT, ybT, scratch, NUMv, XP, win, mlg, ones, EPS_AP, Mrow, WIrow, EMRrow, GT, WST, DEC,
             sel4, maskLE, identb, cwT, cbT, ML_STAGE):
    def chk(k):
        if ML_STAGE < k:
            raise _StopStage()
    if True:
        if True:
            Wb = [P.sb([128, KC, 256], BF16, name=f"Wb{i}") for i in range(2)]
            ACC = [NUMv[:, 0, :], NUMv[:, 1, :]]
            qTh = P.sb([128, 2, SEQ], BF16, name="qTh"); kTh = P.sb([128, 2, SEQ], BF16, name="kTh")
            QW = P.sb([128, 2, SEQ], BF16, name="QW")
            KW = P.sb([128, 16, 256], BF16, name="KW"); VA = P.sb([128, 16, 258], BF16, name="VA")
            WT = P.sb([128, 16, 128], BF16, name="WT")
            DEN = P.sb([1, SEQ], name="DEN")
            C = P.sb([128, 2, 258], name="C"); Cbf = P.sb([128, 2, 258], BF16, name="Cbf")
            DECsb = P.sb([128, 16], name="DECsb")
            PT = [P.sb([128, 128], BF16, name=f"PT{i}") for i in range(2)]
            B = [P.ps(name=f"B{i}") for i in range(8)]
            Bb = [bk[:].bitcast(BF16) for bk in B]
            nb_ = [0]

            def nbank():
                nb_[0] += 1
                return nb_[0] % 8
            wslot = [0]

            def loadW(col):
                sl = wslot[0] % 2
                wslot[0] += 1
                P.dma("pool", Wb[sl][:], win[:, :, col + h * 256: col + (h + 1) * 256], r=(), w=[f"Wb{sl}"], chan=f"Wb{sl}")
                return sl
            XPb = P.sb([128, SEQ + 3], name="XPb")
            XPs = [XP, XPb[:]]
            P.op("dve", lambda e: e.memset(XP[:, 0:3], 0.0), r=(), w=["XP0"])
            P.op("dve", lambda e: e.memset(XPb[:, 0:3], 0.0), r=(), w=["XP0"])
            P.op("dve", lambda e: e.memset(VA[:, :, 256:257], 1.0), r=(), w=["VA1"])
            for wi, (col, dstT, cbase) in enumerate([(COL_MQ, qTh, 0), (COL_MK, kTh, 8)]):
                sl = loadW(col)
                chk(1.1)
                for dch in range(2):
                    cidx = cbase + h * 2 + dch
                    xi = (wi * 2 + dch) % 2
                    XPc = XPs[xi]
                    for tt in range(4):
                        tok = slice(tt * 512, (tt + 1) * 512)
                        bk = nbank()
                        for kc in range(KC):
                            P.mm(B[bk][:], Wb[sl][:, kc, dch * 128:(dch + 1) * 128], hT[:, kc, tok], kc == 0, kc == KC - 1,
                                 r=[f"Wb{sl}"], w=[f"B{bk}"])
                        P.op("act", lambda e, bk=bk, tt=tt, XPc=XPc: e.activation(out=XPc[:, 3 + tt * 512: 3 + (tt + 1) * 512], in_=B[bk][:], func=AF.Copy),
                             r=[f"B{bk}"], w=[f"XP{xi}.{tt}"])
                    chk(1.2)
                    a = ACC[dch]
                    xk = [f"XP{xi}.{t_}" for t_ in range(4)] + ["XP0"]
                    P.op("dve", lambda e, a=a, cidx=cidx, XPc=XPc: e.tensor_scalar(a, XPc[:, 0:SEQ], cwT[:, cidx, 0:1], cbT[:, cidx:cidx + 1], ALU.mult, ALU.add),
                         r=xk, w=[f"N{dch}"])
                    for tap in range(1, 4):
                        P.op("dve", lambda e, a=a, cidx=cidx, tap=tap, XPc=XPc: e.scalar_tensor_tensor(
                            a, XPc[:, tap:tap + SEQ], cwT[:, cidx, tap:tap + 1], a, ALU.mult, ALU.add),
                            r=xk + [f"N{dch}"], w=[f"N{dch}"])
                    chk(1.3)
                    if wi == 0:
                        P.op("act", lambda e, a=a, o_=qTh[:, dch, :]: e.activation(out=o_, in_=a, func=AF.Silu), r=[f"N{dch}"], w=[f"qTh{dch}"])
                    else:
                        P.op("act", lambda e, a=a: e.activation(out=a, in_=a, func=AF.Silu), r=[f"N{dch}"], w=[f"N{dch}"])
                        P.op("dve", lambda e, a=a, o_=kTh[:, dch, :]: e.tensor_scalar(o_, a, 1.0 / 16.0, None, ALU.mult),
                             r=[f"N{dch}"], w=[f"kTh{dch}"])
            chk(2)
            sl = loadW(COL_MV)
            for sb2 in range(8):
                bk = nbank()
                pv = B[bk][:].rearrange("p (j c) -> p j c", c=256)
                for j in range(2):
                    blk = sb2 * 2 + j
                    for kc in range(KC):
                        P.mm(pv[:, j, :], hT[:, kc, blk * 128:(blk + 1) * 128], Wb[sl][:, kc, :], kc == 0, kc == KC - 1,
                             r=[f"Wb{sl}"], w=[f"B{bk}"])
                P.op("act", lambda e, pv=pv, o_=VA[:, sb2 * 2:sb2 * 2 + 2, 0:256]: e.activation(out=o_, in_=pv, func=AF.Copy),
                     r=[f"B{bk}"], w=[f"VA.{sb2}"])
            slo = loadW(COL_MO)
            chk(3)
            TD = ACC[1]
            TD3 = TD.rearrange("p (c t) -> p c t", t=128)
            for tt in range(4):
                tok = slice(tt * 512, (tt + 1) * 512)
                bk = nbank()
                P.mm(B[bk][:], sel4[0:4, h, :], Mrow[0:4, tok], True, True, r=(), w=[f"B{bk}"])
                for j in range(4):
                    c = tt * 4 + j
                    P.op("dve", lambda e, bk=bk, j=j, c=c: e.tensor_scalar(
                        TD3[:, c, :], B[bk][:, j * 128:(j + 1) * 128], GT[:, c, h:h + 1], 0.0, ALU.subtract, ALU.max),
                        r=[f"B{bk}", "kTh1"], w=["N1"])
                bk = nbank()
                P.mm(B[bk][:], sel4[0:4, h, :], WIrow[0:4, tok], True, True, r=(), w=[f"B{bk}"])
                for dch in range(2):
                    P.op("dve", lambda e, bk=bk, o_=QW[:, dch, tok], i_=qTh[:, dch, tok]: e.tensor_tensor(o_, i_, B[bk][:], ALU.mult),
                         r=[f"B{bk}", f"qTh{dch}"], w=[f"QW{dch}"])
            P.op("act", lambda e: e.activation(out=WT[:].rearrange("p c t -> p (c t)"), in_=TD, func=AF.Exp, scale=-1.0), r=["N1"], w=["WT"])
            for c in range(16):
                P.op("pool", lambda e, c=c: e.tensor_tensor(WT[:, c, :], WT[:, c, :], maskLE[:], ALU.mult), r=["WT"], w=["WT"])
            bk = nbank()
            P.mm(B[bk][:, 0:16], sel4[0:4, h, :], DEC[0:4, :], True, True, r=(), w=[f"B{bk}"])
            P.op("act", lambda e, bk=bk: e.activation(out=DECsb[:], in_=B[bk][:, 0:16], func=AF.Copy), r=[f"B{bk}"], w=["DECsb"])
            for sbk in range(16):
                bk = nbank()
                for dch in range(2):
                    P.op("pe", lambda e, bk=bk, dch=dch, sbk=sbk: e.transpose(
                        Bb[bk][:, dch * 128:(dch + 1) * 128], kTh[:, dch, sbk * 128:(sbk + 1) * 128], identb[:]),
                        r=[f"kTh{dch}"], w=[f"B{bk}"])
                P.op("dve", lambda e, bk=bk, sbk=sbk: e.tensor_scalar(KW[:, sbk, :], Bb[bk][:, 0:256], WST[:, sbk, h:h + 1], None, ALU.mult),
                     r=[f"B{bk}"], w=[f"KW.{sbk}"])
            chk(4)
            bS = [0, 1]; bN = [2, 3]; bD = 4; bC = [5, 6]
            def s_pt(c):
                blk = slice(c * 128, (c + 1) * 128)
                s = c % 2
                for dch in range(2):
                    P.mm(B[bS[s]][:, 0:128], kTh[:, dch, blk], qTh[:, dch, blk], dch == 0, dch == 1,
                         r=[f"kTh{dch}", f"qTh{dch}"], w=[f"B{bS[s]}"])
                P.op("dve", lambda e, s=s, c=c: e.tensor_tensor(PT[s][:], B[bS[s]][:, 0:128], WT[:, c, :], ALU.mult),
                     r=[f"B{bS[s]}", "WT"], w=[f"PT{s}"])
            s_pt(0)
            for c in range(16):
                blk = slice(c * 128, (c + 1) * 128)
                cs4 = slice((c % 4) * 128, (c % 4 + 1) * 128)
                s = c % 2
                if c + 1 < 16:
                    s_pt(c + 1)
                for ech in range(3):
                    if ech < 2:
                        outp = B[bN[ech]][:, cs4]; wk = f"B{bN[ech]}"
                        cs_ = slice(ech * 128, (ech + 1) * 128)
                    else:
                        outp = B[bD][0:1, cs4]; wk = f"B{bD}"
                        cs_ = slice(256, 257)
                    if c > 0:
                        for dch in range(2):
                            P.mm(outp, Cbf[:, dch, cs_], QW[:, dch, blk], dch == 0, False, r=[f"Cbf{dch}", f"QW{dch}"], w=[wk])
                    P.mm(outp, VA[:, c, cs_], PT[s][:], c == 0, True, r=[f"VA.{c // 2}", "VA1", f"PT{s}"], w=[wk])
                if c % 4 == 3:
                    t4 = slice((c // 4) * 512, (c // 4 + 1) * 512)
                    for ech in range(2):
                        P.op("act", lambda e, ech=ech, t4=t4: e.activation(out=NUMv[:, ech, t4], in_=B[bN[ech]][:], func=AF.Copy),
                             r=[f"B{bN[ech]}"], w=[f"NUM{ech}.{c // 4}"])
                    P.op("dve", lambda e, t4=t4: e.tensor_copy(DEN[0:1, t4], B[bD][0:1, :]), r=[f"B{bD}"], w=[f"DEN.{c // 4}"])
                if c < 15:
                    for dch in range(2):
                        P.mm(B[bC[dch]][:, 0:257], KW[:, c, dch * 128:(dch + 1) * 128], VA[:, c, 0:257], True, True,
                             r=[f"KW.{c}", f"VA.{c // 2}", "VA1"], w=[f"B{bC[dch]}"])
                        if c == 0:
                            P.op("dve", lambda e, dch=dch: e.tensor_copy(C[:, dch, 0:257], B[bC[dch]][:, 0:257]),
                                 r=[f"B{bC[dch]}"], w=[f"C{dch}"])
                        else:
                            P.op("dve", lambda e, dch=dch, c=c: e.scalar_tensor_tensor(
                                C[:, dch, 0:257], C[:, dch, 0:257], DECsb[:, c:c + 1], B[bC[dch]][:, 0:257], ALU.mult, ALU.add),
                                r=[f"B{bC[dch]}", f"C{dch}", "DECsb"], w=[f"C{dch}"])
                        P.op("act", lambda e, dch=dch: e.activation(out=Cbf[:, dch, 0:257], in_=C[:, dch, 0:257], func=AF.Copy),
                             r=[f"C{dch}"], w=[f"Cbf{dch}"])
            chk(5)
            T = [scratch[:, 4096 + i * 512: 4096 + (i + 1) * 512] for i in range(4)] + \
                [scratch[:, 6148 + i * 512: 6148 + (i + 1) * 512] for i in range(3)]
            T.append(P.sb([128, 512], name="T7")[:]); T.append(P.sb([128, 512], name="T8")[:])
            for tt in range(4):
                tok = slice(tt * 512, (tt + 1) * 512)
                bA = nbank()
                P.mm(B[bA][:], ones[0:1, :], DEN[0:1, tok], True, True, r=[f"DEN.{tt}"], w=[f"B{bA}"])
                bB = nbank()
                P.mm(B[bB][:], sel4[0:4, h, :], EMRrow[0:4, tok], True, True, r=(), w=[f"B{bB}"])
                P.op("act", lambda e, bB=bB: e.activation(out=T[0], in_=B[bB][:], func=AF.Copy), r=[f"B{bB}"], w=["T0"])
                P.op("act", lambda e, bA=bA: e.activation(out=T[1], in_=B[bA][:], func=AF.Abs), r=[f"B{bA}"], w=["T1"])
                P.op("dve", lambda e: e.tensor_tensor(T[1], T[1], T[0], ALU.max), r=["T1", "T0"], w=["T1"])
                P.op("act", lambda e: e.activation(out=T[1], in_=T[1], func=AF.Ln), r=["T1"], w=["T1"])
                P.op("act", lambda e: e.activation(out=T[1], in_=T[1], func=AF.Exp, scale=-1.0), r=["T1"], w=["T1"])
                for ech in range(2):
                    P.op("dve", lambda e, ech=ech, tok=tok: e.tensor_tensor(NUMv[:, ech, tok], NUMv[:, ech, tok], T[1], ALU.mult),
                         r=["T1", f"NUM{ech}.{tt}"], w=[f"NUM{ech}.{tt}"])
                    P.op("act", lambda e, ech=ech, tok=tok: e.activation(out=T[2 + ech], in_=NUMv[:, ech, tok], func=AF.Square),
                         r=[f"NUM{ech}.{tt}"], w=[f"T{2 + ech}"])
                bC2 = nbank()
                for ech in range(2):
                    P.mm(B[bC2][:], ones[:], T[2 + ech], ech == 0, ech == 1, r=[f"T{2 + ech}"], w=[f"B{bC2}"])
                P.op("act", lambda e, bC2=bC2: e.activation(out=T[4], in_=B[bC2][:], func=AF.Ln, bias=EPS_AP[:], scale=1.0 / 256.0),
                     r=[f"B{bC2}"], w=["T4"])
                P.op("act", lambda e: e.activation(out=T[4], in_=T[4], func=AF.Exp, scale=-0.5), r=["T4"], w=["T4"])
                for ech in range(2):
                    bo = nbank()
                    for kc in range(KC):
                        P.mm(B[bo][:], Wb[slo][:, kc, ech * 128:(ech + 1) * 128], hT[:, kc, tok], kc == 0, kc == KC - 1,
                             r=[f"Wb{slo}"], w=[f"B{bo}"])
                    P.op("act", lambda e, bo=bo, ech=ech: e.activation(out=T[5 + ech], in_=B[bo][:], func=AF.Sigmoid), r=[f"B{bo}"], w=[f"T{5 + ech}"])
                    P.op("dve", lambda e, ech=ech, tok=tok: e.scalar_tensor_tensor(
                        T[7 + ech], NUMv[:, ech, tok], mlg[:, h * 2 + ech: h * 2 + ech + 1], T[4], ALU.mult, ALU.mult),
                        r=[f"NUM{ech}.{tt}", "T4"], w=[f"T{7 + ech}"])
                    P.op("pool", lambda e, ech=ech, o_=ybT[:, h * 2 + ech, tok]: e.tensor_tensor(o_, T[7 + ech], T[5 + ech], ALU.mult),
                         r=[f"T{7 + ech}", f"T{5 + ech}"], w=[f"yb.{tt}"])


def merge_phase(nc, D, b, hT, yaT, ybT, mergedT, wview):
    P = Prog(nc, f"mg{b}")
    win = wview(D["w_in"]); wa = wview(D["w_a"]); wb = wview(D["w_b"])
    W = {nm: [P.sb([128, KC, 128], BF16, name=f"{nm}{i}") for i in range(2)] for nm in ("wa", "wb", "wga", "wgb")}
    sa = [P.sb([128, 512], name=f"sa{i}") for i in range(2)]
    sg = [P.sb([128, 512], name=f"sg{i}") for i in range(2)]
    B = [P.ps(name=f"B{i}") for i in range(8)]
    it = 0
    for fo in range(8):
        s = fo % 2
        fsl = slice(fo * 128, (fo + 1) * 128)
        P.dma("pool", W["wa"][s][:], wa[:, :, fsl], r=(), w=[f"wa{s}"], chan=f"mwa{s}")
        P.dma("pool", W["wb"][s][:], wb[:, :, fsl], r=(), w=[f"wb{s}"], chan=f"mwb{s}")
        P.dma("pool", W["wga"][s][:], win[:, :, COL_GA + fo * 128: COL_GA + (fo + 1) * 128], r=(), w=[f"wga{s}"], chan=f"mwga{s}")
        P.dma("pool", W["wgb"][s][:], win[:, :, COL_GB + fo * 128: COL_GB + (fo + 1) * 128], r=(), w=[f"wgb{s}"], chan=f"mwgb{s}")
        for tt in range(4):
            tok = slice(tt * 512, (tt + 1) * 512)
            g = it % 2
            it += 1
            bA, bB, bGA, bGB = 4 * g, 4 * g + 1, 4 * g + 2, 4 * g + 3
            for (bk, nm, src) in [(bA, "wa", yaT), (bB, "wb", ybT), (bGA, "wga", hT), (bGB, "wgb", hT)]:
                for kc in range(KC):
                    P.mm(B[bk][:], W[nm][s][:, kc, :], src[:, kc, tok], kc == 0, kc == KC - 1, r=[f"{nm}{s}"], w=[f"B{bk}"])
            P.op("act", lambda e, g=g, bGA=bGA: e.activation(out=sa[g][:], in_=B[bGA][:], func=AF.Sigmoid), r=[f"B{bGA}"], w=[f"sa{g}"])
            P.op("act", lambda e, g=g, bGB=bGB: e.activation(out=sg[g][:], in_=B[bGB][:], func=AF.Sigmoid), r=[f"B{bGB}"], w=[f"sg{g}"])
            P.op("dve", lambda e, g=g, bA=bA: e.tensor_tensor(sa[g][:], sa[g][:], B[bA][:], ALU.mult), r=[f"sa{g}", f"B{bA}"], w=[f"sa{g}"])
            P.op("dve", lambda e, g=g, bB=bB: e.tensor_tensor(sg[g][:], sg[g][:], B[bB][:], ALU.mult), r=[f"sg{g}", f"B{bB}"], w=[f"sg{g}"])
            P.op("pool", lambda e, g=g, o_=mergedT[:, fo, tok]: e.tensor_tensor(o_, sa[g][:], sg[g][:], ALU.add),
                 r=[f"sa{g}", f"sg{g}"], w=[f"mg.{tt}"])
    P.run()


def outproj_phase(nc, D, b, mergedT, x1T, G1, wview):
    P = Prog(nc, f"op{b}")
    wo = P.sb([128, KC, 1024], BF16, name="wo")
    P.dma("pool", wo[:], wview(D["w_out"]), r=(), w=["wo"], chan="ldwo")
    xt = [P.sb([128, KC, 512], name=f"xt{i}") for i in range(2)]
    B = [P.ps(name=f"B{i}") for i in range(4)]
    it = 0
    for tt in range(4):
        s = tt % 2
        tok = slice(tt * 512, (tt + 1) * 512)
        P.dma("sp", xt[s][:], D["xT"][:, :, b * SEQ + tt * 512: b * SEQ + (tt + 1) * 512], r=(), w=[f"xt{s}"], chan=f"oxt{s}")
        for fo in range(8):
            bk = it % 4
            it += 1
            for kc in range(KC):
                P.mm(B[bk][:], wo[:, kc, fo * 128:(fo + 1) * 128], mergedT[:, kc, tok], kc == 0, kc == KC - 1, r=["wo"], w=[f"B{bk}"])
            P.op("dve", lambda e, bk=bk, fo=fo, s=s, tok=tok: e.scalar_tensor_tensor(
                x1T[:, fo, tok], B[bk][:], G1(fo, b), xt[s][:, fo, :], ALU.mult, ALU.add),
                r=[f"B{bk}", f"xt{s}"], w=[f"x1.{tt}"])
    P.run()


def norm2_router_phase(nc, D, b, x1T, h2T, gT, A2, SH2, G2, ones, ident, EPS_AP, wview):
    P = Prog(nc, f"n2_{b}")
    rw = P.sb([128, KC, NE], name="rw"); rb = P.sb([128, NE], name="rb"); b2n = P.sb([NE, 1024], name="b2n")
    gTb = gT
    gT = P.sb([NE, SEQ], name="gTf")
    P.op("pool", lambda e: e.memset(gTb[:], 0.0), r=(), w=["gTbz"])
    P.dma("sp", rw[:], wview(D["router_w"]), r=(), w=["rw"], chan="ldrw")
    P.dma("sp", rb[:], D["rb_rep"], r=(), w=["rb"], chan="ldrb")
    P.dma("sp", b2n[:], D["b2"], r=(), w=["b2n"], chan="ldb2")
    sq = [P.sb([128, KC, 512], name=f"sq{i}") for i in range(2)]
    h2f = [P.sb([128, KC, 512], name=f"h2f{i}") for i in range(2)]
    rt = [P.sb([128, 512], name=f"rt{i}") for i in range(2)]
    tmp = [P.sb([128, 512], name=f"tmp{i}") for i in range(4)]
    lg = P.sb([128, 4, NE], name="lg"); m8 = P.sb([128, 4, 8], name="m8"); negm = P.sb([128, 4], name="negm")
    ex = P.sb([128, 4, NE], name="ex"); msk = P.sb([128, 4, NE], name="msk"); ssum = P.sb([128, 4], name="ssum")
    gates = P.sb([128, 4, NE], name="gates")
    pss = [P.ps(name=f"ss{i}") for i in range(2)]
    psL = [P.ps(name=f"psL{i}") for i in range(2)]
    psG = [P.ps(name=f"psG{i}") for i in range(2)]
    psB = [P.ps(name=f"psB{i}") for i in range(2)]
    for tt in range(4):
        s = tt % 2
        tok = slice(tt * 512, (tt + 1) * 512)
        xk = [f"x1.{tt}"]
        P.op("act", lambda e, s=s, tok=tok: e.activation(out=sq[s][:], in_=x1T[:, :, tok], func=AF.Square), r=xk, w=[f"sq{s}"])
        for kc in range(KC):
            P.mm(pss[s][:], ones[:], sq[s][:, kc, :], kc == 0, kc == KC - 1, r=[f"sq{s}"], w=[f"ss{s}"])
        P.op("act", lambda e, s=s: e.activation(out=rt[s][:], in_=pss[s][:], func=AF.Ln, bias=EPS_AP[:], scale=1.0 / D_MODEL),
             r=[f"ss{s}"], w=[f"rt{s}"])
        P.op("act", lambda e, s=s: e.activation(out=rt[s][:], in_=rt[s][:], func=AF.Exp, scale=-0.5), r=[f"rt{s}"], w=[f"rt{s}"])
        for kc in range(KC):
            q = kc % 4
            P.op("dve", lambda e, kc=kc, q=q, s=s, tok=tok: e.scalar_tensor_tensor(
                tmp[q][:], x1T[:, kc, tok], A2[:, kc, b:b + 1], rt[s][:], ALU.mult, ALU.mult),
                r=xk + [f"rt{s}"], w=[f"tmp{q}"])
            P.op("act", lambda e, kc=kc, q=q, s=s: e.activation(
                out=h2f[s][:, kc, :], in_=tmp[q][:], func=AF.Identity, bias=SH2(kc, b), scale=1.0),
                r=[f"tmp{q}"], w=[f"h2f{s}.{kc}"])
            P.op("pool", lambda e, kc=kc, s=s, tok=tok: e.tensor_copy(h2T[:, kc, tok], h2f[s][:, kc, :]),
                 r=[f"h2f{s}.{kc}"], w=[f"h2.{tt}"])
        pl4 = psL[s][:, 0:4 * NE].rearrange("p (j n) -> p j n", n=NE)
        for blk in range(4):
            for kc in range(KC):
                P.mm(pl4[:, blk, :], h2f[s][:, kc, blk * 128:(blk + 1) * 128], rw[:, kc, :], kc == 0, kc == KC - 1,
                     r=[f"h2f{s}.{kc}", "rw"], w=[f"psL{s}"])
        for blk in range(4):
            P.op("dve", lambda e, blk=blk, pl4=pl4: e.tensor_tensor(lg[:, blk, :], pl4[:, blk, :], rb[:], ALU.add),
                 r=[f"psL{s}", "rb"], w=[f"lg{blk}"])
            P.op("dve", lambda e, blk=blk: e.max(m8[:, blk, :], lg[:, blk, :]), r=[f"lg{blk}"], w=[f"m8{blk}"])
            P.op("dve", lambda e, blk=blk: e.tensor_scalar(negm[:, blk:blk + 1], m8[:, blk, 0:1], -1.0, None, ALU.mult),
                 r=[f"m8{blk}"], w=[f"negm{blk}"])
            P.op("act", lambda e, blk=blk: e.activation(out=ex[:, blk, :], in_=lg[:, blk, :], func=AF.Exp, bias=negm[:, blk:blk + 1], scale=1.0),
                 r=[f"lg{blk}", f"negm{blk}"], w=[f"ex{blk}"])
            P.op("dve", lambda e, blk=blk: e.tensor_scalar(msk[:, blk, :], lg[:, blk, :], m8[:, blk, 3:4], None, ALU.is_ge),
                 r=[f"lg{blk}", f"m8{blk}"], w=[f"msk{blk}"])
            P.op("dve", lambda e, blk=blk: e.tensor_tensor(ex[:, blk, :], ex[:, blk, :], msk[:, blk, :], ALU.mult),
                 r=[f"ex{blk}", f"msk{blk}"], w=[f"ex{blk}"])
            P.op("dve", lambda e, blk=blk: e.reduce_sum(ssum[:, blk:blk + 1], ex[:, blk, :], mybir.AxisListType.X),
                 r=[f"ex{blk}"], w=[f"ssum{blk}"])
            P.op("dve", lambda e, blk=blk: e.reciprocal(ssum[:, blk:blk + 1], ssum[:, blk:blk + 1]), r=[f"ssum{blk}"], w=[f"ssum{blk}"])
            P.op("dve", lambda e, blk=blk: e.tensor_scalar(gates[:, blk, :], ex[:, blk, :], ssum[:, blk:blk + 1], None, ALU.mult),
                 r=[f"ex{blk}", f"ssum{blk}"], w=[f"gates{blk}"])
            P.op("pe", lambda e, blk=blk, s=s: e.transpose(psG[s][0:NE, blk * 128:(blk + 1) * 128], gates[:, blk, :], ident[:]),
                 r=[f"gates{blk}"], w=[f"psG{s}"])
        P.op("act", lambda e, s=s, tok=tok: e.activation(out=gT[0:NE, tok], in_=psG[s][0:NE, :], func=AF.Copy), r=[f"psG{s}"], w=[f"gT.{tt}"])
        P.op("dve", lambda e, tok=tok: e.tensor_copy(gTb[0:NE, tok], gT[0:NE, tok]), r=[f"gT.{tt}", "gTbz"], w=[f"gTb.{tt}"])
        for fo in range(8):
            bb = fo % 2
            P.mm(psB[bb][:], b2n[0:NE, fo * 128:(fo + 1) * 128], gT[0:NE, tok], True, True, r=["b2n", f"gT.{tt}"], w=[f"psB{bb}"])
            P.op("dve", lambda e, bb=bb, fo=fo, tok=tok: e.scalar_tensor_tensor(
                x1T[:, fo, tok], psB[bb][:], G2(fo, b), x1T[:, fo, tok], ALU.mult, ALU.add),
                r=[f"psB{bb}", f"x1.{tt}"], w=[f"x1.{tt}"])
    P.run()


def moe_phase(nc, D, b, h2T, x1T, gT, G2):
    P = Prog(nc, f"moe{b}")
    w1 = D["w1"].rearrange("e (kc p) n -> e p kc n", p=128)
    w2 = D["w2"].rearrange("e (kc p) n -> e p kc n", p=128)
    b1 = P.sb([128, NE, 16], name="b1"); pid = P.sb([128, 128], name="pid")
    P.dma("sp", b1[:], D["b1T"], r=(), w=["b1"], chan="ldb1")
    P.dma("sp", pid[:], D["c_pid"], r=(), w=["pid"], chan="ldpid")
    P.op("dve", lambda e_: e_.tensor_scalar(b1[:, :, 8:16], b1[:, :, 8:16], 1.0, None, ALU.add), r=["b1"], w=["b1"])
    sel = [P.sb([128, 128], BF16, name=f"sel{i}") for i in range(2)]
    Wg = [P.sb([128, KC, 512], BF16, name=f"Wg{i}") for i in range(3)]
    Wl = [P.sb([128, KC, 512], BF16, name=f"Wl{i}") for i in range(3)]
    W2 = [P.sb([128, 4, 1024], BF16, name=f"W2{i}") for i in range(3)]
    actT = [P.sb([128, 4, 512], BF16, name=f"act{i}") for i in range(3)]
    gsb = [P.sb([128, 512], BF16, name=f"gsb{i}") for i in range(2)]
    Ta = [P.sb([128, 512], name=f"Ta{i}") for i in range(2)]
    Tb = [P.sb([128, 512], name=f"Tb{i}") for i in range(2)]
    Tc = [P.sb([128, 512], name=f"Tc{i}") for i in range(2)]
    pg = [P.ps(name=f"pg{i}") for i in range(2)]
    pl = [P.ps(name=f"pl{i}") for i in range(2)]
    po = [P.ps(name=f"po{i}") for i in range(3)]
    pgt = [P.ps(name=f"pgt{i}") for i in range(1)]
    units = [(e, hf) for e in range(NE) for hf in range(2)]

    def load(u):
        e, hf = units[u]
        r = u % 3
        P.dma("pool", Wg[r][:], w1[e][:, :, hf * 512:(hf + 1) * 512], r=(), w=[f"Wg{r}"], chan=f"Wg{r}")
        P.dma("pool", Wl[r][:], w1[e][:, :, 1024 + hf * 512: 1024 + (hf + 1) * 512], r=(), w=[f"Wl{r}"], chan=f"Wl{r}")
        P.dma("pool", W2[r][:], w2[e][:, hf * 4:(hf + 1) * 4, :], r=(), w=[f"W2{r}"], chan=f"W2{r}")
    load(0)
    steps = [(u, tt) for u in range(len(units)) for tt in range(4)]

    def SA_pre(t):
        u, tt = steps[t]
        e, hf = units[u]
        if tt == 0:
            if u + 1 < len(units):
                load(u + 1)
            if hf == 0:
                P.op("dve", lambda e_, e=e: e_.tensor_scalar(sel[e % 2][:], pid[:], float(e), None, ALU.is_equal), r=["pid"], w=[f"sel{e % 2}"])
        tok = slice(tt * 512, (tt + 1) * 512)
        ga = t % 2
        P.mm(pgt[0][:], sel[e % 2][:], gT[:, tok], True, True, r=[f"sel{e % 2}"], w=["pgt0"])
        P.op("act", lambda e_, ga=ga: e_.activation(out=gsb[ga][:], in_=pgt[0][:], func=AF.Copy), r=["pgt0"], w=[f"gsb{ga}"])

    def SA_head(t, j):
        u, tt = steps[t]
        e, hf = units[u]
        r = u % 3
        tok = slice(tt * 512, (tt + 1) * 512)
        jj = hf * 4 + j
        z = j % 2
        for kc in range(KC):
            P.mm(pg[z][:], Wg[r][:, kc, j * 128:(j + 1) * 128], h2T[:, kc, tok], kc == 0, kc == KC - 1, r=[f"Wg{r}"], w=[f"pg{z}"])
        for kc in range(KC):
            P.mm(pl[z][:], Wl[r][:, kc, j * 128:(j + 1) * 128], h2T[:, kc, tok], kc == 0, kc == KC - 1, r=[f"Wl{r}"], w=[f"pl{z}"])
        P.op("dve", lambda e_, z=z, e=e, jj=jj: e_.tensor_scalar(Ta[z][:], pg[z][:], b1[:, e, jj:jj + 1], 7.0, ALU.add, ALU.min),
             r=[f"pg{z}", "b1"], w=[f"Ta{z}"])
        P.op("act", lambda e_, z=z: e_.activation(out=Tb[z][:], in_=Ta[z][:], func=AF.Sigmoid, scale=1.702), r=[f"Ta{z}"], w=[f"Tb{z}"])
        P.op("dve", lambda e_, z=z, e=e, jj=jj: e_.tensor_scalar(Tc[z][:], pl[z][:], b1[:, e, 8 + jj:9 + jj], -6.0, ALU.add, ALU.max),
             r=[f"pl{z}", "b1"], w=[f"Tc{z}"])
        P.op("pool", lambda e_, z=z: e_.tensor_tensor(Tb[z][:], Ta[z][:], Tb[z][:], ALU.mult), r=[f"Ta{z}", f"Tb{z}"], w=[f"Tb{z}"])

    def SA_tail(t, j):
        z = j % 2
        a = t % 3
        ga = t % 2
        P.op("dve", lambda e_, z=z: e_.scalar_tensor_tensor(Tc[z][:], Tc[z][:], 8.0, Tb[z][:], ALU.min, ALU.mult),
             r=[f"Tb{z}", f"Tc{z}"], w=[f"Tc{z}"])
        P.op("dve", lambda e_, z=z, a=a, j=j, ga=ga: e_.tensor_tensor(actT[a][:, j, :], Tc[z][:], gsb[ga][:], ALU.mult),
             r=[f"Tc{z}", f"gsb{ga}"], w=[f"act{a}.{j}"])

    def SB(t):
        u, tt = steps[t]
        r = u % 3
        tok = slice(tt * 512, (tt + 1) * 512)
        a = t % 3
        for fo in range(8):
            y = (t * 8 + fo) % 3
            for j in range(4):
                P.mm(po[y][:], W2[r][:, j, fo * 128:(fo + 1) * 128], actT[a][:, j, :], j == 0, j == 3,
                     r=[f"W2{r}", f"act{a}.{j}"], w=[f"po{y}"])
            P.op("dve", lambda e_, y=y, fo=fo, tok=tok: e_.scalar_tensor_tensor(
                x1T[:, fo, tok], po[y][:], G2(fo, b), x1T[:, fo, tok], ALU.mult, ALU.add),
                r=[f"po{y}", f"x2.{tt}.{fo}"], w=[f"x2.{tt}.{fo}"])

    n_st = len(steps)
    for t in range(n_st + 2):
        act_ = t < n_st
        if act_:
            SA_pre(t)
            SA_head(t, 0)
            SA_head(t, 1)
            SA_tail(t, 0)
        if t >= 2:
            SB(t - 2)
        if act_:
            SA_head(t, 2)
            SA_tail(t, 1)
            SA_head(t, 3)
            SA_tail(t, 2)
            SA_tail(t, 3)
    P.run()


def final_phase(nc, D, b, x2T, fg, ones, EPS_AP, outT):
    P = Prog(nc, f"fin{b}")
    sq = [P.sb([128, KC, 512], name=f"sq{i}") for i in range(2)]
    ot = [P.sb([128, KC, 512], name=f"ot{i}") for i in range(2)]
    rt = [P.sb([128, 512], name=f"rt{i}") for i in range(2)]
    pss = [P.ps(name=f"ss{i}") for i in range(2)]
    for tt in range(4):
        s = tt % 2
        tok = slice(tt * 512, (tt + 1) * 512)
        P.op("act", lambda e, s=s, tok=tok: e.activation(out=sq[s][:], in_=x2T[:, :, tok], func=AF.Square), r=(), w=[f"sq{s}"])
        for kc in range(KC):
            P.mm(pss[s][:], ones[:], sq[s][:, kc, :], kc == 0, kc == KC - 1, r=[f"sq{s}"], w=[f"ss{s}"])
        P.op("act", lambda e, s=s: e.activation(out=rt[s][:], in_=pss[s][:], func=AF.Ln, bias=EPS_AP[:], scale=1.0 / D_MODEL),
             r=[f"ss{s}"], w=[f"rt{s}"])
        P.op("act", lambda e, s=s: e.activation(out=rt[s][:], in_=rt[s][:], func=AF.Exp, scale=-0.5), r=[f"rt{s}"], w=[f"rt{s}"])
        for kc in range(KC):
            P.op("dve", lambda e, kc=kc, s=s, tok=tok: e.scalar_tensor_tensor(
                ot[s][:, kc, :], x2T[:, kc, tok], fg[:, kc:kc + 1], rt[s][:], ALU.mult, ALU.mult),
                r=[f"rt{s}"], w=[f"ot{s}"])
        P.dma("sp", outT[:, :, b * SEQ + tt * 512: b * SEQ + (tt + 1) * 512], ot[s][:], r=[f"ot{s}"], w=(), chan=f"out{s}")
    P.run()


def _consts():
    p = np.arange(128)
    c = {}
    c["c_ones"] = np.ones((128, 128), np.float32)
    c["c_tri"] = (p[:, None] >= p[None, :]).astype(np.float32)
    mD = np.zeros((128, 4, 512), np.float32)
    for i in range(4):
        for j in range(4):
            if j > i:
                mD[:, i, j * 128:(j + 1) * 128] = 1.0
            elif j == i:
                mD[:, i, j * 128:(j + 1) * 128] = (p[None, :] > p[:, None]).astype(np.float32)
    c["c_maskD"] = mD
    c["c_maskLE"] = (p[:, None] <= p[None, :]).astype(np.float32)
    c["c_ident"] = np.eye(128, dtype=np.float32)
    c["c_identb"] = np.eye(128, dtype=np.float32).astype(ml_dtypes.bfloat16)
    s4 = np.zeros((4, 4, 128), np.float32)
    for h in range(4):
        s4[h, h, :] = 1.0
    c["c_sel4"] = s4
    pid = np.full((128, 128), -1.0, np.float32)
    pid[:32, :] = np.arange(32, dtype=np.float32)[:, None]
    c["c_pid"] = pid
    return c


def _fm(v, nch):
    return np.ascontiguousarray(np.asarray(v, np.float32).reshape(nch, 128).T)


def make_in_maps(inp, cores=range(NCORES)):
    f = lambda a: np.ascontiguousarray(np.asarray(a, np.float32))
    shared = {
        "ada_w": f(inp["ada_w"][0]), "ada_bT": _fm(inp["ada_b"][0], 48),
        "n1g": _fm(inp["norm1_g"][0], 8), "n2g": _fm(inp["norm2_g"][0], 8), "fg": _fm(inp["final_g"], 8),
        "mlg": _fm(inp["ml_norm_g"][0], 8),
        "w_in": f(inp["w_in"][0]),
        "cwT": np.ascontiguousarray(np.asarray(inp["conv_w"][0], np.float32).reshape(4, 16, 128).transpose(2, 1, 0)),
        "cbT": _fm(inp["conv_b"][0], 16),
        "bi": f(inp["ml_b_i"][0]).reshape(4, 1), "bf": f(inp["ml_b_f"][0]).reshape(4, 1),
        "w_a": f(inp["w_branch_a"][0]), "w_b": f(inp["w_branch_b"][0]), "w_out": f(inp["w_out"][0]),
        "router_w": f(inp["router_w"][0]),
        "rb_rep": np.ascontiguousarray(np.broadcast_to(np.asarray(inp["router_b"][0], np.float32)[None, :], (128, NE))),
        "w1": f(inp["expert_w1"][0]),
        "b1T": np.ascontiguousarray(np.asarray(inp["expert_b1"][0], np.float32).reshape(NE, 16, 128).transpose(2, 0, 1)),
        "w2": f(inp["expert_w2"][0]), "b2": f(inp["expert_b2"][0]),
    }
    shared.update(_consts())
    x = np.asarray(inp["x"], np.float32)
    c = np.asarray(inp["c"], np.float32)
    maps = []
    for i in cores:
        xs = x[2 * i:2 * i + 2].reshape(NTOK, KC, 128)
        m = dict(shared)
        m["xT"] = np.ascontiguousarray(xs.transpose(2, 1, 0))
        m["cT"] = np.ascontiguousarray(c[2 * i:2 * i + 2].reshape(2, KC, 128).transpose(2, 1, 0))
        maps.append(m)
    return maps


def kernel(**inputs):
    nc = build()
    maps = make_in_maps(inputs)
    res = run_bass_kernel_spmd(nc, maps, core_ids=list(range(NCORES)))
    out = np.empty((16, SEQ, D_MODEL), np.float32)
    for i in range(NCORES):
        o = res.results[i]["outT"]
        out[2 * i:2 * i + 2] = o.transpose(2, 1, 0).reshape(2, SEQ, D_MODEL)
    return out
```

```python
import numpy as np
from contextlib import ExitStack
import ml_dtypes
import concourse.bass as bass
import concourse.mybir as mybir
from concourse.bass_utils import run_bass_kernel_spmd

F32 = mybir.dt.float32
BF16 = mybir.dt.bfloat16
F32R = mybir.dt.float32r
AF = mybir.ActivationFunctionType
ALU = mybir.AluOpType

NCORES = 8
D_MODEL = 1024
SEQ = 2048
NTOK = 2 * SEQ
NE = 32
EPS = 1e-6
KC = 8
COL_Q, COL_K, COL_V = 0, 1024, 2048
COL_MQ, COL_MK, COL_MV, COL_MO = 3072, 4096, 5120, 6144
COL_I, COL_F, COL_GA, COL_GB = 7168, 7172, 7176, 8200


class _Op:
    __slots__ = ("id", "eng", "fn", "deps", "chan", "chan_val", "sig")


class _SemPool:
    def __init__(self, nc):
        self.nc = nc
        self.esem = {e: nc.alloc_semaphore(f"eng_{e}") for e in ("pe", "act", "dve", "pool", "sp")}
        self.ecnt = {e: 0 for e in self.esem}
        self.csem = {}
        self.ccnt = {}

    def chan(self, c):
        if c not in self.csem:
            self.csem[c] = self.nc.alloc_semaphore(f"ch_{len(self.csem)}")
            self.ccnt[c] = 0
        return self.csem[c]


_POOLS = {}


class Prog:
    def __init__(self, nc, name):
        self.nc = nc
        self.name = name
        self.es = ExitStack()
        self.ops = []
        self.lastw = {}
        self.readers = {}
        self.chan_n = {}
        self.nsb = 0
        if id(nc) not in _POOLS:
            _POOLS.clear()
            _POOLS[id(nc)] = _SemPool(nc)
        self.pool = _POOLS[id(nc)]

    def sb(self, shape, dt=F32, name=None):
        self.nsb += 1
        return self.es.enter_context(self.nc.sbuf_tensor(f"{self.name}_s{self.nsb}_{name or ''}", list(shape), dt))

    def ps(self, shape=(128, 512), dt=F32, name=None):
        self.nsb += 1
        return self.es.enter_context(self.nc.psum_tensor(f"{self.name}_p{self.nsb}_{name or ''}", list(shape), dt))

    def op(self, eng, fn, r=(), w=(), chan=None):
        o = _Op()
        o.id = len(self.ops)
        o.eng = eng
        o.fn = fn
        o.chan = chan
        o.chan_val = 0
        o.sig = 0
        deps = {}
        for k in r:
            d = self.lastw.get(k)
            if d is not None:
                deps[d] = "raw"
        for k in w:
            d = self.lastw.get(k)
            if d is not None and d not in deps:
                deps[d] = "waw"
            for d in self.readers.get(k, {}).values():
                if d not in deps:
                    deps[d] = "war"
        o.deps = deps
        if chan is not None:
            self.pool.chan(chan)
            self.pool.ccnt[chan] += 1
            self.chan_n[chan] = self.pool.ccnt[chan]
            o.chan_val = 16 * self.chan_n[chan]
        self.ops.append(o)
        rk = (eng, chan)
        for k in r:
            self.readers.setdefault(k, {})[rk] = o.id
        for k in w:
            self.lastw[k] = o.id
            self.readers[k] = {}
        return o.id

    def mm(self, out, lhsT, rhs, start, stop, r, w, sgc=False):
        if sgc:
            return self.op("pe", lambda e: e.matmul(out, lhsT, rhs, start=start, stop=stop, skip_group_check=True), r, w)
        return self.op("pe", lambda e: e.matmul(out, lhsT, rhs, start=start, stop=stop), r, w)

    def dma(self, eng, out, in_, r, w, chan):
        return self.op(eng, lambda e: e.dma_start(out=out, in_=in_), r, w, chan=chan)

    def _skip(self, p, o, typ):
        return (p.chan is None and o.chan is None and p.eng == o.eng and p.eng == "pe")

    def run(self):
        nc = self.nc
        ops = self.ops
        engs = ("pe", "act", "dve", "pool", "sp")
        need = [False] * len(ops)
        for o in ops:
            for d, typ in o.deps.items():
                p = ops[d]
                if p.chan is not None or self._skip(p, o, typ):
                    continue
                need[d] = True
        cnt = self.pool.ecnt
        for o in ops:
            if o.chan is None and need[o.id]:
                cnt[o.eng] += 1
                o.sig = cnt[o.eng]
        with ExitStack() as es:
            esem = self.pool.esem
            csem = self.pool.csem
            by_eng = {e: [o for o in ops if o.eng == e] for e in engs}
            with nc.Block() as block:
                def emit(e, name):
                    waited = {}
                    for o in by_eng[name]:
                        want = {}
                        for d, typ in o.deps.items():
                            p = ops[d]
                            if p.chan is not None:
                                key, val = ("c", p.chan), p.chan_val
                            else:
                                if self._skip(p, o, typ):
                                    continue
                                key, val = ("e", p.eng), p.sig
                            if val > want.get(key, 0):
                                want[key] = val
                        for key, val in want.items():
                            if waited.get(key, 0) >= val:
                                continue
                            waited[key] = val
                            e.wait_ge(csem[key[1]] if key[0] == "c" else esem[key[1]], val)
                        ins = o.fn(e)
                        if o.chan is not None:
                            ins.then_inc(csem[o.chan], 16)
                        elif o.sig:
                            ins.then_inc(esem[name], 1)
                    if name == "sp":
                        for c, n in self.chan_n.items():
                            if waited.get(("c", c), 0) < 16 * n:
                                e.wait_ge(csem[c], 16 * n)

                @block.tensor
                def _(e):
                    emit(e, "pe")

                @block.scalar
                def _(e):
                    emit(e, "act")

                @block.vector
                def _(e):
                    emit(e, "dve")

                @block.gpsimd
                def _(e):
                    emit(e, "pool")

                @block.sync
                def _(e):
                    emit(e, "sp")
        self.es.close()


def _tap(nc, taps, name, ap, shape, dt=F32):
    if taps is None or name not in taps:
        return
    d = nc.dram_tensor("dbg_" + name, list(shape), dt, kind="ExternalOutput").ap()
    if id(nc) not in _POOLS:
        _POOLS.clear()
        _POOLS[id(nc)] = _SemPool(nc)
    pool = _POOLS[id(nc)]
    s = pool.chan("tap")
    pool.ccnt["tap"] += 1
    v = 16 * pool.ccnt["tap"]
    with nc.Block() as blk:
        @blk.sync
        def _(e):
            e.dma_start(out=d, in_=ap).then_inc(s, 16)
            e.wait_ge(s, v)


def build(upto=99, taps=None):
    nc = bass.Bass("TRN2", target_bir_lowering=False)
    D = {}

    def din(name, shape, dt=F32):
        D[name] = nc.dram_tensor(name, list(shape), dt, kind="ExternalInput").ap()

    din("xT", [128, KC, NTOK]); din("cT", [128, KC, 2]); din("ada_w", [1024, 6144]); din("ada_bT", [128, 48])
    din("n1g", [128, KC]); din("n2g", [128, KC]); din("fg", [128, KC]); din("mlg", [128, KC])
    din("w_in", [1024, 9224]); din("cwT", [128, 16, 4]); din("cbT", [128, 16]); din("bi", [4, 1]); din("bf", [4, 1])
    din("w_a", [1024, 1024]); din("w_b", [1024, 1024]); din("w_out", [1024, 1024])
    din("router_w", [1024, NE]); din("rb_rep", [128, NE])
    din("w1", [NE, 1024, 2048]); din("b1T", [128, NE, 16]); din("w2", [NE, 1024, 1024]); din("b2", [NE, 1024])
    din("c_ones", [128, 128]); din("c_tri", [128, 128]); din("c_maskD", [128, 4, 512]); din("c_maskLE", [128, 128])
    din("c_ident", [128, 128]); din("c_identb", [128, 128], BF16); din("c_sel4", [4, 4, 128]); din("c_pid", [128, 128])
    outT = nc.dram_tensor("outT", [128, KC, NTOK], F32, kind="ExternalOutput").ap()

    def wview(ap2d):
        return ap2d.rearrange("(kc p) n -> p kc n", p=128)

    with ExitStack() as top:
        def tsb(name, shape, dt=F32):
            return top.enter_context(nc.sbuf_tensor("g_" + name, list(shape), dt))

        modT = tsb("modT", [128, 48, 2])
        A1 = tsb("A1", [128, KC, 2]); A2 = tsb("A2", [128, KC, 2])
        ones = tsb("ones", [128, 128]); n1g = tsb("n1g", [128, KC]); n2g = tsb("n2g", [128, KC])
        fg = tsb("fg", [128, KC]); mlg = tsb("mlg", [128, KC])
        ident = tsb("ident", [128, 128])
        hT = tsb("hT", [128, KC, SEQ], BF16)
        Y = tsb("Y", [128, 2, KC, SEQ], BF16)
        yaT = Y[:, 0]
        ybT = Y[:, 1]
        x1T = Y[:].rearrange("p a k t -> p (a k t)").bitcast(F32).rearrange("p (k t) -> p k t", k=KC)

        P = Prog(nc, "p0")
        cT = P.sb([128, KC, 2], name="cT"); sc = P.sb([128, KC, 2], name="sc")
        abT = P.sb([128, 48], name="abT")
        wbuf = [P.sb([128, KC, 1024], name=f"adaw{i}") for i in range(2)]
        pm = [P.ps(name=f"pm{i}") for i in range(2)]
        for i, (t, nm) in enumerate([(ones, "c_ones"), (n1g, "n1g"), (n2g, "n2g"), (fg, "fg"), (mlg, "mlg"),
                                     (ident, "c_ident"), (cT, "cT"), (abT, "ada_bT")]):
            P.dma("sp", t[:], D[nm], r=(), w=[nm], chan="ld" + nm)
        P.op("act", lambda e: e.activation(out=sc[:], in_=cT[:], func=AF.Silu), r=["cT"], w=["sc"])
        adaw = wview(D["ada_w"])
        for m in range(6):
            wb = wbuf[m % 2]
            P.dma("sp", wb[:], adaw[:, :, m * 1024:(m + 1) * 1024], r=(), w=[f"adaw{m % 2}"], chan=f"adaw{m % 2}")
            pmm = pm[m % 2][:, 0:16].rearrange("p (j b) -> p j b", b=2)
            for jj in range(8):
                for kc in range(KC):
                    P.mm(pmm[:, jj, :], wb[:, kc, jj * 128:(jj + 1) * 128], sc[:, kc, :], kc == 0, kc == KC - 1,
                         r=[f"adaw{m % 2}", "sc"], w=[f"pm{m % 2}"])
            for b in range(2):
                P.op("dve", lambda e, m=m, b=b, pmm=pmm: e.tensor_tensor(
                    modT[:, m * 8:(m + 1) * 8, b], pmm[:, :, b], abT[:, m * 8:(m + 1) * 8], ALU.add),
                    r=[f"pm{m % 2}", "ada_bT"], w=["modT"])
        for b in range(2):
            P.op("dve", lambda e, b=b: e.scalar_tensor_tensor(A1[:, :, b], modT[:, 8:16, b], 1.0, n1g[:], ALU.add, ALU.mult),
                 r=["modT", "n1g"], w=["A1"])
            P.op("dve", lambda e, b=b: e.scalar_tensor_tensor(A2[:, :, b], modT[:, 32:40, b], 1.0, n2g[:], ALU.add, ALU.mult),
                 r=["modT", "n2g"], w=["A2"])
        P.run()
        _tap(nc, taps, "modT", modT[:], [128, 48, 2])
        _tap(nc, taps, "A1", A1[:], [128, KC, 2])

        def SH1(kc, b): return modT[:, 0 + kc, b:b + 1]
        def G1(kc, b): return modT[:, 16 + kc, b:b + 1]
        def SH2(kc, b): return modT[:, 24 + kc, b:b + 1]
        def G2(kc, b): return modT[:, 40 + kc, b:b + 1]

        def norm_phase(name, b, src_dram, src_sb, Aap, SHap, dst_bf, extra=None):
            P = Prog(nc, name)
            xt = [P.sb([128, KC, 512], name=f"xt{i}") for i in range(2)] if src_sb is None else None
            sq = [P.sb([128, KC, 512], name=f"sq{i}") for i in range(2)]
            rt = [P.sb([128, 512], name=f"rt{i}") for i in range(2)]
            rs = [P.sb([128, 512], name=f"rs{i}") for i in range(2)]
            tmp = [P.sb([128, 512], name=f"tmp{i}") for i in range(4)]
            pss = [P.ps(name=f"ss{i}") for i in range(2)]
            ctx = extra(P) if extra else None
            for tt in range(4):
                s = tt % 2
                tok = slice(tt * 512, (tt + 1) * 512)
                if src_sb is None:
                    P.dma("sp", xt[s][:], src_dram[:, :, b * SEQ + tt * 512: b * SEQ + (tt + 1) * 512], r=(), w=[f"xt{s}"], chan=f"xt{s}")
                    xin = xt[s]
                    xk = [f"xt{s}"]
                    xv = lambda kc, xin=xin: xin[:, kc, :]
                    xall = xin[:]
                else:
                    xk = [f"x1.{tt}"]
                    xv = lambda kc, tok=tok: src_sb[:, kc, tok]
                    xall = src_sb[:, :, tok]
                P.op("act", lambda e, s=s, xall=xall: e.activation(out=sq[s][:], in_=xall, func=AF.Square), r=xk, w=[f"sq{s}"])
                for kc in range(KC):
                    P.mm(pss[s][:], ones[:], sq[s][:, kc, :], kc == 0, kc == KC - 1, r=[f"sq{s}"], w=[f"ss{s}"])
                P.op("act", lambda e, s=s: e.activation(out=rt[s][:], in_=pss[s][:], func=AF.Ln, bias=EPS_AP[:], scale=1.0 / D_MODEL),
                     r=[f"ss{s}"], w=[f"rt{s}"])
                P.op("act", lambda e, s=s: e.activation(out=rs[s][:], in_=rt[s][:], func=AF.Exp, scale=-0.5), r=[f"rt{s}"], w=[f"rs{s}"])
                for kc in range(KC):
                    q = kc % 4
                    P.op("dve", lambda e, kc=kc, q=q, s=s, xv=xv: e.scalar_tensor_tensor(
                        tmp[q][:], xv(kc), Aap(kc, b), rs[s][:], ALU.mult, ALU.mult),
                        r=xk + [f"rs{s}"], w=[f"tmp{q}"])
                    if ctx is None:
                        P.op("act", lambda e, kc=kc, q=q, tok=tok: e.activation(
                            out=dst_bf[:, kc, tok], in_=tmp[q][:], func=AF.Identity, bias=SHap(kc, b), scale=1.0),
                            r=[f"tmp{q}"], w=[f"h.{tt}"])
                    else:
                        ctx["per_kc"](P, tt, kc, q, tmp[q], tok)
                if ctx is not None:
                    ctx["per_tile"](P, tt, tok)
            if ctx is not None and "final" in ctx:
                ctx["final"](P)
            P.run()

        EPS_AP = tsb("eps", [128, 1])
        P = Prog(nc, "pc")
        P.op("dve", lambda e: e.memset(EPS_AP[:], EPS), r=(), w=["eps"])
        P.run()

        for b in range(2):
            norm_phase(f"n1_{b}", b, D["xT"], None, lambda kc, b: A1[:, kc, b:b + 1], SH1, hT)
            if b == 0:
                _tap(nc, taps, "hT", hT[:], [128, KC, SEQ], BF16)
            if upto <= 1:
                continue
            scratch = Y[:, 0].rearrange("p k t -> p (k t)").bitcast(F32)
            mlstm_phase(nc, D, b, hT, ybT, scratch, wview, mlg, ones, ident, EPS_AP, taps)
            if b == 0:
                _tap(nc, taps, "ybT", ybT, [128, KC, SEQ], BF16)
            if upto <= 2:
                continue
            sb_attention(nc, D, b, hT, yaT, wview)
            if b == 0:
                _tap(nc, taps, "yaT", yaT, [128, KC, SEQ], BF16)
            if upto <= 3:
                continue
            with nc.sbuf_tensor(f"mergedT{b}", [128, KC, SEQ], BF16) as mergedT:
                merge_phase(nc, D, b, hT, yaT, ybT, mergedT, wview)
                if b == 0:
                    _tap(nc, taps, "mergedT", mergedT[:], [128, KC, SEQ], BF16)
                outproj_phase(nc, D, b, mergedT, x1T, G1, wview)
            if b == 0:
                _tap(nc, taps, "x1T", x1T, [128, KC, SEQ])
            if upto <= 4:
                continue
            with nc.sbuf_tensor(f"gT{b}", [128, SEQ], BF16) as gT:
                norm2_router_phase(nc, D, b, x1T, hT, gT, A2, SH2, G2, ones, ident, EPS_AP, wview)
                if b == 0:
                    _tap(nc, taps, "h2T", hT[:], [128, KC, SEQ], BF16)
                    _tap(nc, taps, "gT", gT[0:NE, :], [NE, SEQ], BF16)
                    _tap(nc, taps, "x1bT", x1T, [128, KC, SEQ])
                if upto <= 5:
                    continue
                moe_phase(nc, D, b, hT, x1T, gT, G2)
            if b == 0:
                _tap(nc, taps, "x2T", x1T, [128, KC, SEQ])
            final_phase(nc, D, b, x1T, fg, ones, EPS_AP, outT)
    return nc


def sb_attention(nc, D, b, hT, yaT, wview):
    P = Prog(nc, f"sb{b}")
    win = wview(D["w_in"])
    ones = P.sb([128, 128], name="ones"); tri = P.sb([128, 128], name="tri")
    maskD = P.sb([128, 4, 512], name="maskD")
    for t, nm in [(ones, "c_ones"), (tri, "c_tri"), (maskD, "c_maskD")]:
        P.dma("sp", t[:], D[nm], r=(), w=[nm], chan="ld" + nm)
    P.op("dve", lambda e: e.tensor_copy(trir[:], tri[:]), r=["c_tri"], w=["trir"])
    P.op("dve", lambda e: e.tensor_copy(onesr[:], ones[:]), r=["c_ones"], w=["onesr"])
    Wq = [P.sb([128, KC, 128], BF16, name=f"wq{i}") for i in range(2)]
    Wk = [P.sb([128, KC, 128], BF16, name=f"wk{i}") for i in range(2)]
    Wv = [P.sb([128, KC, 128], BF16, name=f"wv{i}") for i in range(2)]
    qT = [P.sb([128, SEQ], BF16, name=f"qT{i}") for i in range(2)]
    kTh_ = [[P.sb([128, SEQ], BF16, name=f"kT{i}_{hh}") for hh in range(2)] for i in range(2)]
    V = [P.sb([128, 16, 2, 128], BF16, name=f"V{i}") for i in range(2)]
    for i in range(2):
        P.op("pool", lambda e, i=i: e.memset(kTh_[i][0][64:128, :], 0.0), r=(), w=[f"kz{i}"])
        P.op("pool", lambda e, i=i: e.memset(kTh_[i][1][0:64, :], 0.0), r=(), w=[f"kz{i}"])
        P.op("pool", lambda e, i=i: e.memset(V[i][:], 0.0), r=(), w=[f"vz{i}"])
    et = [P.sb([128, 512], name=f"et{i}") for i in range(4)]
    spt = [P.sb([128, 512], F32R, name=f"spt{i}") for i in range(4)]
    ext = [P.sb([128, 512], name=f"ext{i}") for i in range(4)]
    AT = [P.sb([128, 512], BF16, name=f"AT{i}") for i in range(4)]
    R = [P.sb([128, 512], F32R, name=f"R{i}") for i in range(2)]
    trir = P.sb([128, 128], F32R, name="trir"); onesr = P.sb([128, 128], F32R, name="onesr")
    psz = [P.ps(name=f"z{i}") for i in range(2)]
    pscs = [P.ps(name=f"cs{i}") for i in range(2)]
    psy = [P.ps(name=f"y{i}") for i in range(2)]
    pspj = [P.ps(name=f"pj{i}") for i in range(2)]
    npj = [0]
    it = 0

    def proj_chunks(hp):
        s = hp % 2
        ch = []

        def dmas():
            for (W, col, nm) in [(Wq, COL_Q, "wq"), (Wk, COL_K, "wk"), (Wv, COL_V, "wv")]:
                P.dma("pool", W[s][:], win[:, :, col + hp * 128: col + (hp + 1) * 128], r=(), w=[f"{nm}{s}"], chan=f"{nm}{s}")
        ch.append(dmas)
        for tt in range(4):
            for nm in ("q", "k"):
                def c_(tt=tt, nm=nm):
                    tok = slice(tt * 512, (tt + 1) * 512)
                    W = Wq if nm == "q" else Wk
                    pj = npj[0] % 2
                    npj[0] += 1
                    for kc in range(KC):
                        P.mm(pspj[pj][:], W[s][:, kc, :], hT[:, kc, tok], kc == 0, kc == KC - 1,
                             r=[f"w{nm}{s}", f"h.{tt}"], w=[f"pj{pj}"])
                    if nm == "q":
                        P.op("dve", lambda e, pj=pj, o_=qT[s][:, tok]: e.tensor_scalar(o_, pspj[pj][:], 0.125, None, ALU.mult),
                             r=[f"pj{pj}"], w=[f"qT{s}.{tt}"])
                    else:
                        P.op("dve", lambda e, pj=pj, o_=kTh_[s][0][0:64, tok]: e.tensor_copy(o_, pspj[pj][0:64, :]),
                             r=[f"pj{pj}", f"kz{s}"], w=[f"kT{s}.{tt}"])
                        P.op("dve", lambda e, pj=pj, o_=kTh_[s][1][64:128, tok]: e.tensor_copy(o_, pspj[pj][64:128, :]),
                             r=[f"pj{pj}", f"kz{s}"], w=[f"kT{s}.{tt}"])
                ch.append(c_)
        for g4 in range(4):
            def c_(g4=g4):
                pj = npj[0] % 2
                npj[0] += 1
                pv = pspj[pj][:].rearrange("p (j c) -> p j c", c=128)
                for j in range(4):
                    blk = g4 * 4 + j
                    for kc in range(KC):
                        P.mm(pv[:, j, :], hT[:, kc, blk * 128:(blk + 1) * 128], Wv[s][:, kc, :], kc == 0, kc == KC - 1,
                             r=[f"wv{s}", f"h.{g4}"], w=[f"pj{pj}"])
                P.op("dve", lambda e, o_=V[s][:, g4 * 4:(g4 + 1) * 4, 0, 0:64], pv=pv: e.tensor_copy(o_, pv[:, :, 0:64]),
                     r=[f"pj{pj}", f"vz{s}"], w=[f"V{s}.{g4}"])
                P.op("dve", lambda e, o_=V[s][:, g4 * 4:(g4 + 1) * 4, 1, 64:128], pv=pv: e.tensor_copy(o_, pv[:, :, 64:128]),
                     r=[f"pj{pj}", f"vz{s}"], w=[f"V{s}.{g4}"])
            ch.append(c_)
        return ch

    for c_ in proj_chunks(0):
        c_()
    for hp in range(8):
        s = hp % 2
        nxt = proj_chunks(hp + 1) if hp + 1 < 8 else []
        iters = []
        for qc in range(4):
            nkb = 4 * qc + 4
            for idx, kb in enumerate(range(nkb - 1, -1, -1)):
                for hh in range(2):
                    iters.append((qc, idx, kb, hh))
        base = it
        it += len(iters)
        triS = maskD[:, 0, 0:128]

        def prm(t, iters=iters, base=base):
            qc, idx, kb, hh = iters[t]
            g = base + t
            c0 = 128 * max(0, kb - 4 * qc)
            return qc, idx, kb, hh, g % 4, g % 2, c0

        def SA_mm(t, s=s):
            qc, idx, kb, hh, u, z, c0 = prm(t)
            P.mm(psz[z][:, c0:], kTh_[s][hh][:, kb * 128:(kb + 1) * 128], qT[s][:, qc * 512 + c0:(qc + 1) * 512], True, True,
                 r=[f"kT{s}.{kb // 4}", f"qT{s}.{qc}"], w=[f"z{z}"])

        def SA_exp(t):
            qc, idx, kb, hh, u, z, c0 = prm(t)
            P.op("act", lambda e, u=u, z=z, c0=c0: e.activation(out=et[u][:, c0:], in_=psz[z][:, c0:], func=AF.Exp), r=[f"z{z}"], w=[f"et{u}"])

        def SA_ln(t):
            qc, idx, kb, hh, u, z, c0 = prm(t)
            P.op("act", lambda e, u=u, c0=c0: e.activation(out=spt[u][:, c0:], in_=et[u][:, c0:], func=AF.Ln, bias=1.0, scale=1.0),
                 r=[f"et{u}"], w=[f"spt{u}"])

        def SA_mask(t):
            qc, idx, kb, hh, u, z, c0 = prm(t)
            if kb >= 4 * qc:
                P.op("dve", lambda e, u=u, c0=c0: e.tensor_tensor(spt[u][:, c0:c0 + 128], spt[u][:, c0:c0 + 128], triS, ALU.mult),
                     r=[f"spt{u}", "c_maskD"], w=[f"spt{u}"])
                P.op("dve", lambda e, u=u, c0=c0: e.tensor_tensor(et[u][:, c0:c0 + 128], et[u][:, c0:c0 + 128], triS, ALU.mult),
                     r=[f"et{u}", "c_maskD"], w=[f"et{u}"])

        def SB_mm(t):
            qc, idx, kb, hh, u, z, c0 = prm(t)
            P.mm(pscs[z][:, c0:], trir[:], spt[u][:, c0:], True, idx == 0, r=[f"spt{u}", "trir"], w=[f"cs{z}"])
            if idx > 0:
                P.mm(pscs[z][:, c0:], onesr[:], R[hh][:, c0:], False, True, r=[f"R{hh}", "onesr"], w=[f"cs{z}"])

        def SB_pool(t):
            qc, idx, kb, hh, u, z, c0 = prm(t)
            if kb > 0:
                if idx == 0:
                    if c0 > 0:
                        P.op("dve", lambda e, hh=hh, c0=c0: e.tensor_copy(R[hh][:, 0:c0], maskD[:, 3, 0:c0]), r=["c_maskD"], w=[f"R{hh}"])
                    P.op("dve", lambda e, u=u, hh=hh, c0=c0: e.tensor_copy(R[hh][:, c0:], spt[u][:, c0:]), r=[f"spt{u}"], w=[f"R{hh}"])
                else:
                    P.op("dve", lambda e, u=u, hh=hh, c0=c0: e.tensor_tensor(R[hh][:, c0:], R[hh][:, c0:], spt[u][:, c0:], ALU.add),
                         r=[f"R{hh}", f"spt{u}"], w=[f"R{hh}"])

        def SB_exp(t):
            qc, idx, kb, hh, u, z, c0 = prm(t)
            P.op("act", lambda e, u=u, z=z, c0=c0: e.activation(out=ext[u][:, c0:], in_=pscs[z][:, c0:], func=AF.Exp, scale=-1.0),
                 r=[f"cs{z}"], w=[f"ext{u}"])

        def SB_mult(t):
            qc, idx, kb, hh, u, z, c0 = prm(t)
            P.op("dve", lambda e, u=u, c0=c0: e.tensor_tensor(AT[u][:, c0:], et[u][:, c0:], ext[u][:, c0:], ALU.mult),
                 r=[f"et{u}", f"ext{u}"], w=[f"AT{u}"])

        def SC(t, s=s, hp=hp):
            qc, idx, kb, hh, u, z, c0 = prm(t)
            yb = (hp * 4 + qc) % 2
            P.mm(psy[yb][:, c0:], V[s][:, kb, hh, :], AT[u][:, c0:], idx == 0 and hh == 0, kb == 0 and hh == 1,
                 r=[f"V{s}.{kb // 4}", f"AT{u}"], w=[f"y{yb}.0", f"y{yb}.1"], sgc=True)
            if kb == 0 and hh == 1:
                P.op("dve", lambda e, yb=yb, o_=yaT[:, hp, qc * 512:(qc + 1) * 512]: e.tensor_copy(o_, psy[yb][:]),
                     r=[f"y{yb}.0", f"y{yb}.1"], w=[f"ya.{qc}"])

        n_pairs = len(iters) // 2
        for p in range(n_pairs + 2):
            if nxt and p % 3 == 0:
                nxt.pop(0)()
            if p < n_pairs:
                for f in (SA_mm, SA_exp, SA_ln, SA_mask):
                    f(2 * p)
                    f(2 * p + 1)
            if 0 <= p - 1 < n_pairs:
                for f in (SB_mm, SB_pool, SB_exp, SB_mult):
                    f(2 * (p - 1))
                    f(2 * (p - 1) + 1)
            if 0 <= p - 2 < n_pairs:
                SC(2 * (p - 2))
                SC(2 * (p - 2) + 1)
        while nxt:
            nxt.pop(0)()
    P.run()


def mlstm_phase(nc, D, b, hT, ybT, scratch, wview, mlg, ones, ident, EPS_AP, taps=None):
    win = wview(D["w_in"])
    NUMv = scratch[:, 0:4096].rearrange("p (a t) -> p a t", a=2)
    XP = scratch[:, 4096:6147]
    with ExitStack() as mes:
        def msb(name, shape, dt=F32):
            return mes.enter_context(nc.sbuf_tensor(f"ml{b}_{name}", list(shape), dt))
        Mrow = msb("Mrow", [4, SEQ]); WIrow = msb("WIrow", [4, SEQ]); EMRrow = msb("EMRrow", [4, SEQ])
        GT = msb("GT", [128, 16, 4]); WST = msb("WST", [128, 16, 4]); DEC = msb("DEC", [4, 16])
        sel4 = msb("sel4", [4, 4, 128]); maskLE = msb("maskLE", [128, 128]); identb = msb("identb", [128, 128], BF16)
        cwT = msb("cwT", [128, 16, 4]); cbT = msb("cbT", [128, 16])
        P = Prog(nc, f"mla{b}")
        for t, nm in [(sel4, "c_sel4"), (maskLE, "c_maskLE"), (identb, "c_identb"), (cwT, "cwT"), (cbT, "cbT")]:
            P.dma("sp", t[:], D[nm], r=(), w=[nm], chan="ld" + nm)
        bi = P.sb([4, 1], name="bi"); bfv = P.sb([4, 1], name="bf"); nbf = P.sb([4, 1], name="nbf")
        P.dma("sp", bi[:], D["bi"], r=(), w=["bi"], chan="ldbi")
        P.dma("sp", bfv[:], D["bf"], r=(), w=["bf"], chan="ldbf")
        Wif32 = P.sb([128, KC, 8], name="Wif32")
        Wi = P.sb([128, KC, 4], BF16, name="Wi"); Wf = P.sb([128, KC, 4], BF16, name="Wf")
        P.dma("sp", Wif32[:], win[:, :, COL_I:COL_I + 8], r=(), w=["Wif32"], chan="ldWif")
        P.op("dve", lambda e: e.tensor_copy(Wi[:], Wif32[:, :, 0:4]), r=["Wif32"], w=["Wi"])
        P.op("dve", lambda e: e.tensor_copy(Wf[:], Wif32[:, :, 4:8]), r=["Wif32"], w=["Wf"])
        Grow = P.sb([4, SEQ], name="Grow"); SP = P.sb([4, SEQ], name="SP"); CS = P.sb([4, SEQ], name="CS")
        WSrow = P.sb([4, SEQ], name="WSrow"); Mprev = P.sb([4, 16], name="Mprev")
        psI = [P.ps(name=f"psI{i}") for i in range(2)]
        psF = [P.ps(name=f"psF{i}") for i in range(2)]
        psT = P.ps(name="psT"); psT2 = P.ps(name="psT2")
        P.op("dve", lambda e: e.tensor_scalar(nbf[:], bfv[:], -1.0, None, ALU.mult), r=["bf"], w=["nbf"])
        for tt in range(4):
            s = tt % 2
            tok = slice(tt * 512, (tt + 1) * 512)
            for kc in range(KC):
                P.mm(psI[s][0:4, :], Wi[:, kc, :], hT[:, kc, tok], kc == 0, kc == KC - 1, r=["Wi"], w=[f"psI{s}"])
            for kc in range(KC):
                P.mm(psF[s][0:4, :], Wf[:, kc, :], hT[:, kc, tok], kc == 0, kc == KC - 1, r=["Wf"], w=[f"psF{s}"])
            P.op("dve", lambda e, s=s, tok=tok: e.tensor_scalar(Grow[:, tok], psI[s][0:4, :], bi[:, 0:1], None, ALU.add),
                 r=[f"psI{s}", "bi"], w=["Grow"])
            P.op("act", lambda e, s=s, tok=tok: e.activation(out=SP[:, tok], in_=psF[s][0:4, :], func=AF.Exp, bias=nbf[:, 0:1], scale=-1.0),
                 r=[f"psF{s}", "nbf"], w=["SP"])
        P.op("act", lambda e: e.activation(out=SP[:], in_=SP[:], func=AF.Ln, bias=1.0, scale=1.0), r=["SP"], w=["SP"])
        P.op("dve", lambda e: e.tensor_tensor_scan(CS[:], SP[:], SP[:], 0.0, ALU.add, ALU.max), r=["SP"], w=["CS"])
        P.op("dve", lambda e: e.tensor_tensor(Grow[:], Grow[:], CS[:], ALU.add), r=["Grow", "CS"], w=["Grow"])
        P.op("dve", lambda e: e.tensor_tensor_scan(Mrow[:], Grow[:], Grow[:], 0.0, ALU.max, ALU.max), r=["Grow"], w=["Mrow"])
        M3 = Mrow[:].rearrange("p (c t) -> p c t", t=128)
        G3 = Grow[:].rearrange("p (c t) -> p c t", t=128)
        WI3 = WIrow[:].rearrange("p (c t) -> p c t", t=128)
        WS3 = WSrow[:].rearrange("p (c t) -> p c t", t=128)
        P.op("dve", lambda e: e.memset(Mprev[:, 0:1], 0.0), r=(), w=["Mprev0"])
        P.op("dve", lambda e: e.tensor_copy(Mprev[:, 1:16], M3[:, 0:15, 127]), r=["Mrow"], w=["Mprev"])
        for c in range(16):
            P.op("dve", lambda e, c=c: e.tensor_scalar(WI3[:, c, :], M3[:, c, :], Mprev[:, c:c + 1], None, ALU.subtract),
                 r=["Mrow", "Mprev", "Mprev0"], w=["WIrow"])
            P.op("dve", lambda e, c=c: e.tensor_scalar(WS3[:, c, :], G3[:, c, :], M3[:, c, 127:128], None, ALU.subtract),
                 r=["Mrow", "Grow"], w=["WSrow"])
        P.op("act", lambda e: e.activation(out=WIrow[:], in_=WIrow[:], func=AF.Exp, scale=-1.0), r=["WIrow"], w=["WIrow"])
        P.op("act", lambda e: e.activation(out=WSrow[:], in_=WSrow[:], func=AF.Exp), r=["WSrow"], w=["WSrow"])
        P.op("dve", lambda e: e.tensor_tensor(EMRrow[:], CS[:], Mrow[:], ALU.subtract), r=["CS", "Mrow"], w=["EMRrow"])
        P.op("act", lambda e: e.activation(out=EMRrow[:], in_=EMRrow[:], func=AF.Exp), r=["EMRrow"], w=["EMRrow"])
        P.op("dve", lambda e: e.tensor_tensor(DEC[:], Mprev[:], M3[:, :, 127], ALU.subtract), r=["Mprev", "Mprev0", "Mrow"], w=["DEC"])
        P.op("act", lambda e: e.activation(out=DEC[:], in_=DEC[:], func=AF.Exp), r=["DEC"], w=["DEC"])
        pT = psT[:, 0:64].rearrange("p (c h) -> p c h", h=4)
        pT2 = psT2[:, 0:64].rearrange("p (c h) -> p c h", h=4)
        for blk in range(16):
            P.op("pe", lambda e, blk=blk: e.transpose(pT[:, blk, :], Grow[0:4, blk * 128:(blk + 1) * 128], ident[0:4, 0:4]),
                 r=["Grow"], w=["psT"])
            P.op("pe", lambda e, blk=blk: e.transpose(pT2[:, blk, :], WSrow[0:4, blk * 128:(blk + 1) * 128], ident[0:4, 0:4]),
                 r=["WSrow"], w=["psT2"])
        P.op("dve", lambda e: e.tensor_copy(GT[:], pT), r=["psT"], w=["GT"])
        P.op("dve", lambda e: e.tensor_copy(WST[:], pT2), r=["psT2"], w=["WST"])
        P.run()
        _tap(nc, taps, f"Mrow{b}", Mrow[:], [4, SEQ]); _tap(nc, taps, f"WIrow{b}", WIrow[:], [4, SEQ])
        _tap(nc, taps, f"EMRrow{b}", EMRrow[:], [4, SEQ]); _tap(nc, taps, f"GT{b}", GT[:], [128, 16, 4])
        _tap(nc, taps, f"WST{b}", WST[:], [128, 16, 4]); _tap(nc, taps, f"DEC{b}", DEC[:], [4, 16])

        import os
        ML_STAGE = float(os.environ.get("ML_STAGE", "9"))
        for h in range(4 if ML_STAGE > 0 else 0):
            P = Prog(nc, f"mlh{b}_{h}")
            try:
              _ml_head(P, nc, D, b, h, hT, ybT, scratch, NUMv, XP, win, mlg, ones, EPS_AP, Mrow, WIrow, EMRrow, GT, WST, DEC,
                       sel4, maskLE, identb, cwT, cbT, ML_STAGE)
            except _StopStage:
              pass
            P.run()


class _StopStage(Exception):
    pass


def _ml_head(P, nc, D, b, h, hT, ybT, scratch, NUMv, XP, win, mlg, ones, EPS_AP, Mrow, WIrow, EMRrow, GT, WST, DEC,
             sel4, maskLE, identb, cwT, cbT, ML_STAGE):
    def chk(k):
        if ML_STAGE < k:
            raise _StopStage()
    if True:
        if True:
            Wb = [P.sb([128, KC, 256], BF16, name=f"Wb{i}") for i in range(2)]
            ACC = [NUMv[:, 0, :], NUMv[:, 1, :]]
            qTh = P.sb([128, 2, SEQ], BF16, name="qTh"); kTh = P.sb([128, 2, SEQ], BF16, name="kTh")
            QW = P.sb([128, 2, SEQ], BF16, name="QW")
            KW = P.sb([128, 16, 256], BF16, name="KW"); VA = P.sb([128, 16, 258], BF16, name="VA")
            WT = P.sb([128, 16, 128], BF16, name="WT")
            DEN = P.sb([1, SEQ], name="DEN")
            C = P.sb([128, 2, 258], name="C"); Cbf = P.sb([128, 2, 258], BF16, name="Cbf")
            DECsb = P.sb([128, 16], name="DECsb")
            PT = [P.sb([128, 128], BF16, name=f"PT{i}") for i in range(2)]
            B = [P.ps(name=f"B{i}") for i in range(8)]
            Bb = [bk[:].bitcast(BF16) for bk in B]
            nb_ = [0]

            def nbank():
                nb_[0] += 1
                return nb_[0] % 8
            wslot = [0]

            def loadW(col):
                sl = wslot[0] % 2
                wslot[0] += 1
                P.dma("pool", Wb[sl][:], win[:, :, col + h * 256: col + (h + 1) * 256], r=(), w=[f"Wb{sl}"], chan=f"Wb{sl}")
                return sl
            XPb = P.sb([128, SEQ + 3], name="XPb")
            XPs = [XP, XPb[:]]
            P.op("dve", lambda e: e.memset(XP[:, 0:3], 0.0), r=(), w=["XP0"])
            P.op("dve", lambda e: e.memset(XPb[:, 0:3], 0.0), r=(), w=["XP0"])
            P.op("dve", lambda e: e.memset(VA[:, :, 256:257], 1.0), r=(), w=["VA1"])
            for wi, (col, dstT, cbase) in enumerate([(COL_MQ, qTh, 0), (COL_MK, kTh, 8)]):
                sl = loadW(col)
                chk(1.1)
                for dch in range(2):
                    cidx = cbase + h * 2 + dch
                    xi = (wi * 2 + dch) % 2
                    XPc = XPs[xi]
                    for tt in range(4):
                        tok = slice(tt * 512, (tt + 1) * 512)
                        bk = nbank()
                        for kc in range(KC):
                            P.mm(B[bk][:], Wb[sl][:, kc, dch * 128:(dch + 1) * 128], hT[:, kc, tok], kc == 0, kc == KC - 1,
                                 r=[f"Wb{sl}"], w=[f"B{bk}"])
                        P.op("act", lambda e, bk=bk, tt=tt, XPc=XPc: e.activation(out=XPc[:, 3 + tt * 512: 3 + (tt + 1) * 512], in_=B[bk][:], func=AF.Copy),
                             r=[f"B{bk}"], w=[f"XP{xi}.{tt}"])
                    chk(1.2)
                    a = ACC[dch]
                    xk = [f"XP{xi}.{t_}" for t_ in range(4)] + ["XP0"]
                    P.op("dve", lambda e, a=a, cidx=cidx, XPc=XPc: e.tensor_scalar(a, XPc[:, 0:SEQ], cwT[:, cidx, 0:1], cbT[:, cidx:cidx + 1], ALU.mult, ALU.add),
                         r=xk, w=[f"N{dch}"])
                    for tap in range(1, 4):
                        P.op("dve", lambda e, a=a, cidx=cidx, tap=tap, XPc=XPc: e.scalar_tensor_tensor(
                            a, XPc[:, tap:tap + SEQ], cwT[:, cidx, tap:tap + 1], a, ALU.mult, ALU.add),
                            r=xk + [f"N{dch}"], w=[f"N{dch}"])
                    chk(1.3)
                    if wi == 0:
                        P.op("act", lambda e, a=a, o_=qTh[:, dch, :]: e.activation(out=o_, in_=a, func=AF.Silu), r=[f"N{dch}"], w=[f"qTh{dch}"])
                    else:
                        P.op("act", lambda e, a=a: e.activation(out=a, in_=a, func=AF.Silu), r=[f"N{dch}"], w=[f"N{dch}"])
                        P.op("dve", lambda e, a=a, o_=kTh[:, dch, :]: e.tensor_scalar(o_, a, 1.0 / 16.0, None, ALU.mult),
                             r=[f"N{dch}"], w=[f"kTh{dch}"])
            chk(2)
            sl = loadW(COL_MV)
            for sb2 in range(8):
                bk = nbank()
                pv = B[bk][:].rearrange("p (j c) -> p j c", c=256)
                for j in range(2):
                    blk = sb2 * 2 + j
                    for kc in range(KC):
                        P.mm(pv[:, j, :], hT[:, kc, blk * 128:(blk + 1) * 128], Wb[sl][:, kc, :], kc == 0, kc == KC - 1,
                             r=[f"Wb{sl}"], w=[f"B{bk}"])
                P.op("act", lambda e, pv=pv, o_=VA[:, sb2 * 2:sb2 * 2 + 2, 0:256]: e.activation(out=o_, in_=pv, func=AF.Copy),
                     r=[f"B{bk}"], w=[f"VA.{sb2}"])
            slo = loadW(COL_MO)
            chk(3)
            TD = ACC[1]
            TD3 = TD.rearrange("p (c t) -> p c t", t=128)
            for tt in range(4):
                tok = slice(tt * 512, (tt + 1) * 512)
                bk = nbank()
                P.mm(B[bk][:], sel4[0:4, h, :], Mrow[0:4, tok], True, True, r=(), w=[f"B{bk}"])
                for j in range(4):
                    c = tt * 4 + j
                    P.op("dve", lambda e, bk=bk, j=j, c=c: e.tensor_scalar(
                        TD3[:, c, :], B[bk][:, j * 128:(j + 1) * 128], GT[:, c, h:h + 1], 0.0, ALU.subtract, ALU.max),
                        r=[f"B{bk}", "kTh1"], w=["N1"])
                bk = nbank()
                P.mm(B[bk][:], sel4[0:4, h, :], WIrow[0:4, tok], True, True, r=(), w=[f"B{bk}"])
                for dch in range(2):
                    P.op("dve", lambda e, bk=bk, o_=QW[:, dch, tok], i_=qTh[:, dch, tok]: e.tensor_tensor(o_, i_, B[bk][:], ALU.mult),
                         r=[f"B{bk}", f"qTh{dch}"], w=[f"QW{dch}"])
            P.op("act", lambda e: e.activation(out=WT[:].rearrange("p c t -> p (c t)"), in_=TD, func=AF.Exp, scale=-1.0), r=["N1"], w=["WT"])
            for c in range(16):
                P.op("pool", lambda e, c=c: e.tensor_tensor(WT[:, c, :], WT[:, c, :], maskLE[:], ALU.mult), r=["WT"], w=["WT"])
            bk = nbank()
            P.mm(B[bk][:, 0:16], sel4[0:4, h, :], DEC[0:4, :], True, True, r=(), w=[f"B{bk}"])
            P.op("act", lambda e, bk=bk: e.activation(out=DECsb[:], in_=B[bk][:, 0:16], func=AF.Copy), r=[f"B{bk}"], w=["DECsb"])
            for sbk in range(16):
                bk = nbank()
                for dch in range(2):
                    P.op("pe", lambda e, bk=bk, dch=dch, sbk=sbk: e.transpose(
                        Bb[bk][:, dch * 128:(dch + 1) * 128], kTh[:, dch, sbk * 128:(sbk + 1) * 128], identb[:]),
                        r=[f"kTh{dch}"], w=[f"B{bk}"])
                P.op("dve", lambda e, bk=bk, sbk=sbk: e.tensor_scalar(KW[:, sbk, :], Bb[bk][:, 0:256], WST[:, sbk, h:h + 1], None, ALU.mult),
                     r=[f"B{bk}"], w=[f"KW.{sbk}"])
            chk(4)
            bS = [0, 1]; bN = [2, 3]; bD = 4; bC = [5, 6]
            def s_pt(c):
                blk = slice(c * 128, (c + 1) * 128)
                s = c % 2
                for dch in range(2):
                    P.mm(B[bS[s]][:, 0:128], kTh[:, dch, blk], qTh[:, dch, blk], dch == 0, dch == 1,
                         r=[f"kTh{dch}", f"qTh{dch}"], w=[f"B{bS[s]}"])
                P.op("dve", lambda e, s=s, c=c: e.tensor_tensor(PT[s][:], B[bS[s]][:, 0:128], WT[:, c, :], ALU.mult),
                     r=[f"B{bS[s]}", "WT"], w=[f"PT{s}"])
            s_pt(0)
            for c in range(16):
                blk = slice(c * 128, (c + 1) * 128)
                cs4 = slice((c % 4) * 128, (c % 4 + 1) * 128)
                s = c % 2
                if c + 1 < 16:
                    s_pt(c + 1)
                for ech in range(3):
                    if ech < 2:
                        outp = B[bN[ech]][:, cs4]; wk = f"B{bN[ech]}"
                        cs_ = slice(ech * 128, (ech + 1) * 128)
                    else:
                        outp = B[bD][0:1, cs4]; wk = f"B{bD}"
                        cs_ = slice(256, 257)
                    if c > 0:
                        for dch in range(2):
                            P.mm(outp, Cbf[:, dch, cs_], QW[:, dch, blk], dch == 0, False, r=[f"Cbf{dch}", f"QW{dch}"], w=[wk])
                    P.mm(outp, VA[:, c, cs_], PT[s][:], c == 0, True, r=[f"VA.{c // 2}", "VA1", f"PT{s}"], w=[wk])
                if c % 4 == 3:
                    t4 = slice((c // 4) * 512, (c // 4 + 1) * 512)
                    for ech in range(2):
                        P.op("act", lambda e, ech=ech, t4=t4: e.activation(out=NUMv[:, ech, t4], in_=B[bN[ech]][:], func=AF.Copy),
                             r=[f"B{bN[ech]}"], w=[f"NUM{ech}.{c // 4}"])
                    P.op("dve", lambda e, t4=t4: e.tensor_copy(DEN[0:1, t4], B[bD][0:1, :]), r=[f"B{bD}"], w=[f"DEN.{c // 4}"])
                if c < 15:
                    for dch in range(2):
                        P.mm(B[bC[dch]][:, 0:257], KW[:, c, dch * 128:(dch + 1) * 128], VA[:, c, 0:257], True, True,
                             r=[f"KW.{c}", f"VA.{c // 2}", "VA1"], w=[f"B{bC[dch]}"])
                        if c == 0:
                            P.op("dve", lambda e, dch=dch: e.tensor_copy(C[:, dch, 0:257], B[bC[dch]][:, 0:257]),
                                 r=[f"B{bC[dch]}"], w=[f"C{dch}"])
                        else:
                            P.op("dve", lambda e, dch=dch, c=c: e.scalar_tensor_tensor(
                                C[:, dch, 0:257], C[:, dch, 0:257], DECsb[:, c:c + 1], B[bC[dch]][:, 0:257], ALU.mult, ALU.add),
                                r=[f"B{bC[dch]}", f"C{dch}", "DECsb"], w=[f"C{dch}"])
                        P.op("act", lambda e, dch=dch: e.activation(out=Cbf[:, dch, 0:257], in_=C[:, dch, 0:257], func=AF.Copy),
                             r=[f"C{dch}"], w=[f"Cbf{dch}"])
            chk(5)
            T = [scratch[:, 4096 + i * 512: 4096 + (i + 1) * 512] for i in range(4)] + \
                [scratch[:, 6148 + i * 512: 6148 + (i + 1) * 512] for i in range(3)]
            T.append(P.sb([128, 512], name="T7")[:]); T.append(P.sb([128, 512], name="T8")[:])
            for tt in range(4):
                tok = slice(tt * 512, (tt + 1) * 512)
                bA = nbank()
                P.mm(B[bA][:], ones[0:1, :], DEN[0:1, tok], True, True, r=[f"DEN.{tt}"], w=[f"B{bA}"])
                bB = nbank()
                P.mm(B[bB][:], sel4[0:4, h, :], EMRrow[0:4, tok], True, True, r=(), w=[f"B{bB}"])
                P.op("act", lambda e, bB=bB: e.activation(out=T[0], in_=B[bB][:], func=AF.Copy), r=[f"B{bB}"], w=["T0"])
                P.op("act", lambda e, bA=bA: e.activation(out=T[1], in_=B[bA][:], func=AF.Abs), r=[f"B{bA}"], w=["T1"])
                P.op("dve", lambda e: e.tensor_tensor(T[1], T[1], T[0], ALU.max), r=["T1", "T0"], w=["T1"])
                P.op("act", lambda e: e.activation(out=T[1], in_=T[1], func=AF.Ln), r=["T1"], w=["T1"])
                P.op("act", lambda e: e.activation(out=T[1], in_=T[1], func=AF.Exp, scale=-1.0), r=["T1"], w=["T1"])
                for ech in range(2):
                    P.op("dve", lambda e, ech=ech, tok=tok: e.tensor_tensor(NUMv[:, ech, tok], NUMv[:, ech, tok], T[1], ALU.mult),
                         r=["T1", f"NUM{ech}.{tt}"], w=[f"NUM{ech}.{tt}"])
                    P.op("act", lambda e, ech=ech, tok=tok: e.activation(out=T[2 + ech], in_=NUMv[:, ech, tok], func=AF.Square),
                         r=[f"NUM{ech}.{tt}"], w=[f"T{2 + ech}"])
                bC2 = nbank()
                for ech in range(2):
                    P.mm(B[bC2][:], ones[:], T[2 + ech], ech == 0, ech == 1, r=[f"T{2 + ech}"], w=[f"B{bC2}"])
                P.op("act", lambda e, bC2=bC2: e.activation(out=T[4], in_=B[bC2][:], func=AF.Ln, bias=EPS_AP[:], scale=1.0 / 256.0),
                     r=[f"B{bC2}"], w=["T4"])
                P.op("act", lambda e: e.activation(out=T[4], in_=T[4], func=AF.Exp, scale=-0.5), r=["T4"], w=["T4"])
                for ech in range(2):
                    bo = nbank()
                    for kc in range(KC):
                        P.mm(B[bo][:], Wb[slo][:, kc, ech * 128:(ech + 1) * 128], hT[:, kc, tok], kc == 0, kc == KC - 1,
                             r=[f"Wb{slo}"], w=[f"B{bo}"])
                    P.op("act", lambda e, bo=bo, ech=ech: e.activation(out=T[5 + ech], in_=B[bo][:], func=AF.Sigmoid), r=[f"B{bo}"], w=[f"T{5 + ech}"])
                    P.op("dve", lambda e, ech=ech, tok=tok: e.scalar_tensor_tensor(
                        T[7 + ech], NUMv[:, ech, tok], mlg[:, h * 2 + ech: h * 2 + ech + 1], T[4], ALU.mult, ALU.mult),
                        r=[f"NUM{ech}.{tt}", "T4"], w=[f"T{7 + ech}"])
                    P.op("pool", lambda e, ech=ech, o_=ybT[:, h * 2 + ech, tok]: e.tensor_tensor(o_, T[7 + ech], T[5 + ech], ALU.mult),
                         r=[f"T{7 + ech}", f"T{5 + ech}"], w=[f"yb.{tt}"])


def merge_phase(nc, D, b, hT, yaT, ybT, mergedT, wview):
    P = Prog(nc, f"mg{b}")
    win = wview(D["w_in"]); wa = wview(D["w_a"]); wb = wview(D["w_b"])
    W = {nm: [P.sb([128, KC, 128], BF16, name=f"{nm}{i}") for i in range(2)] for nm in ("wa", "wb", "wga", "wgb")}
    sa = [P.sb([128, 512], name=f"sa{i}") for i in range(2)]
    sg = [P.sb([128, 512], name=f"sg{i}") for i in range(2)]
    B = [P.ps(name=f"B{i}") for i in range(8)]
    it = 0
    for fo in range(8):
        s = fo % 2
        fsl = slice(fo * 128, (fo + 1) * 128)
        P.dma("pool", W["wa"][s][:], wa[:, :, fsl], r=(), w=[f"wa{s}"], chan=f"mwa{s}")
        P.dma("pool", W["wb"][s][:], wb[:, :, fsl], r=(), w=[f"wb{s}"], chan=f"mwb{s}")
        P.dma("pool", W["wga"][s][:], win[:, :, COL_GA + fo * 128: COL_GA + (fo + 1) * 128], r=(), w=[f"wga{s}"], chan=f"mwga{s}")
        P.dma("pool", W["wgb"][s][:], win[:, :, COL_GB + fo * 128: COL_GB + (fo + 1) * 128], r=(), w=[f"wgb{s}"], chan=f"mwgb{s}")
        for tt in range(4):
            tok = slice(tt * 512, (tt + 1) * 512)
            g = it % 2
            it += 1
            bA, bB, bGA, bGB = 4 * g, 4 * g + 1, 4 * g + 2, 4 * g + 3
            for (bk, nm, src) in [(bA, "wa", yaT), (bB, "wb", ybT), (bGA, "wga", hT), (bGB, "wgb", hT)]:
                for kc in range(KC):
                    P.mm(B[bk][:], W[nm][s][:, kc, :], src[:, kc, tok], kc == 0, kc == KC - 1, r=[f"{nm}{s}"], w=[f"B{bk}"])
            P.op("act", lambda e, g=g, bGA=bGA: e.activation(out=sa[g][:], in_=B[bGA][:], func=AF.Sigmoid), r=[f"B{bGA}"], w=[f"sa{g}"])
            P.op("act", lambda e, g=g, bGB=bGB: e.activation(out=sg[g][:], in_=B[bGB][:], func=AF.Sigmoid), r=[f"B{bGB}"], w=[f"sg{g}"])
            P.op("dve", lambda e, g=g, bA=bA: e.tensor_tensor(sa[g][:], sa[g][:], B[bA][:], ALU.mult), r=[f"sa{g}", f"B{bA}"], w=[f"sa{g}"])
            P.op("dve", lambda e, g=g, bB=bB: e.tensor_tensor(sg[g][:], sg[g][:], B[bB][:], ALU.mult), r=[f"sg{g}", f"B{bB}"], w=[f"sg{g}"])
            P.op("pool", lambda e, g=g, o_=mergedT[:, fo, tok]: e.tensor_tensor(o_, sa[g][:], sg[g][:], ALU.add),
                 r=[f"sa{g}", f"sg{g}"], w=[f"mg.{tt}"])
    P.run()


def outproj_phase(nc, D, b, mergedT, x1T, G1, wview):
    P = Prog(nc, f"op{b}")
    wo = P.sb([128, KC, 1024], BF16, name="wo")
    P.dma("pool", wo[:], wview(D["w_out"]), r=(), w=["wo"], chan="ldwo")
    xt = [P.sb([128, KC, 512], name=f"xt{i}") for i in range(2)]
    B = [P.ps(name=f"B{i}") for i in range(4)]
    it = 0
    for tt in range(4):
        s = tt % 2
        tok = slice(tt * 512, (tt + 1) * 512)
        P.dma("sp", xt[s][:], D["xT"][:, :, b * SEQ + tt * 512: b * SEQ + (tt + 1) * 512], r=(), w=[f"xt{s}"], chan=f"oxt{s}")
        for fo in range(8):
            bk = it % 4
            it += 1
            for kc in range(KC):
                P.mm(B[bk][:], wo[:, kc, fo * 128:(fo + 1) * 128], mergedT[:, kc, tok], kc == 0, kc == KC - 1, r=["wo"], w=[f"B{bk}"])
            P.op("dve", lambda e, bk=bk, fo=fo, s=s, tok=tok: e.scalar_tensor_tensor(
                x1T[:, fo, tok], B[bk][:], G1(fo, b), xt[s][:, fo, :], ALU.mult, ALU.add),
                r=[f"B{bk}", f"xt{s}"], w=[f"x1.{tt}"])
    P.run()


def norm2_router_phase(nc, D, b, x1T, h2T, gT, A2, SH2, G2, ones, ident, EPS_AP, wview):
    P = Prog(nc, f"n2_{b}")
    rw = P.sb([128, KC, NE], name="rw"); rb = P.sb([128, NE], name="rb"); b2n = P.sb([NE, 1024], name="b2n")
    gTb = gT
    gT = P.sb([NE, SEQ], name="gTf")
    P.op("pool", lambda e: e.memset(gTb[:], 0.0), r=(), w=["gTbz"])
    P.dma("sp", rw[:], wview(D["router_w"]), r=(), w=["rw"], chan="ldrw")
    P.dma("sp", rb[:], D["rb_rep"], r=(), w=["rb"], chan="ldrb")
    P.dma("sp", b2n[:], D["b2"], r=(), w=["b2n"], chan="ldb2")
    sq = [P.sb([128, KC, 512], name=f"sq{i}") for i in range(2)]
    h2f = [P.sb([128, KC, 512], name=f"h2f{i}") for i in range(2)]
    rt = [P.sb([128, 512], name=f"rt{i}") for i in range(2)]
    tmp = [P.sb([128, 512], name=f"tmp{i}") for i in range(4)]
    lg = P.sb([128, 4, NE], name="lg"); m8 = P.sb([128, 4, 8], name="m8"); negm = P.sb([128, 4], name="negm")
    ex = P.sb([128, 4, NE], name="ex"); msk = P.sb([128, 4, NE], name="msk"); ssum = P.sb([128, 4], name="ssum")
    gates = P.sb([128, 4, NE], name="gates")
    pss = [P.ps(name=f"ss{i}") for i in range(2)]
    psL = [P.ps(name=f"psL{i}") for i in range(2)]
    psG = [P.ps(name=f"psG{i}") for i in range(2)]
    psB = [P.ps(name=f"psB{i}") for i in range(2)]
    for tt in range(4):
        s = tt % 2
        tok = slice(tt * 512, (tt + 1) * 512)
        xk = [f"x1.{tt}"]
        P.op("act", lambda e, s=s, tok=tok: e.activation(out=sq[s][:], in_=x1T[:, :, tok], func=AF.Square), r=xk, w=[f"sq{s}"])
        for kc in range(KC):
            P.mm(pss[s][:], ones[:], sq[s][:, kc, :], kc == 0, kc == KC - 1, r=[f"sq{s}"], w=[f"ss{s}"])
        P.op("act", lambda e, s=s: e.activation(out=rt[s][:], in_=pss[s][:], func=AF.Ln, bias=EPS_AP[:], scale=1.0 / D_MODEL),
             r=[f"ss{s}"], w=[f"rt{s}"])
        P.op("act", lambda e, s=s: e.activation(out=rt[s][:], in_=rt[s][:], func=AF.Exp, scale=-0.5), r=[f"rt{s}"], w=[f"rt{s}"])
        for kc in range(KC):
            q = kc % 4
            P.op("dve", lambda e, kc=kc, q=q, s=s, tok=tok: e.scalar_tensor_tensor(
                tmp[q][:], x1T[:, kc, tok], A2[:, kc, b:b + 1], rt[s][:], ALU.mult, ALU.mult),
                r=xk + [f"rt{s}"], w=[f"tmp{q}"])
            P.op("act", lambda e, kc=kc, q=q, s=s: e.activation(
                out=h2f[s][:, kc, :], in_=tmp[q][:], func=AF.Identity, bias=SH2(kc, b), scale=1.0),
                r=[f"tmp{q}"], w=[f"h2f{s}.{kc}"])
            P.op("act", lambda e, kc=kc, q=q, tok=tok: e.activation(
                out=h2T[:, kc, tok], in_=tmp[q][:], func=AF.Identity, bias=SH2(kc, b), scale=1.0),
                r=[f"tmp{q}"], w=[f"h2.{tt}"])
        pl4 = psL[s][:, 0:4 * NE].rearrange("p (j n) -> p j n", n=NE)
        for blk in range(4):
            for kc in range(KC):
                P.mm(pl4[:, blk, :], h2f[s][:, kc, blk * 128:(blk + 1) * 128], rw[:, kc, :], kc == 0, kc == KC - 1,
                     r=[f"h2f{s}.{kc}", "rw"], w=[f"psL{s}"])
        for blk in range(4):
            P.op("dve", lambda e, blk=blk, pl4=pl4: e.tensor_tensor(lg[:, blk, :], pl4[:, blk, :], rb[:], ALU.add),
                 r=[f"psL{s}", "rb"], w=[f"lg{blk}"])
            P.op("dve", lambda e, blk=blk: e.max(m8[:, blk, :], lg[:, blk, :]), r=[f"lg{blk}"], w=[f"m8{blk}"])
            P.op("dve", lambda e, blk=blk: e.tensor_scalar(negm[:, blk:blk + 1], m8[:, blk, 0:1], -1.0, None, ALU.mult),
                 r=[f"m8{blk}"], w=[f"negm{blk}"])
            P.op("act", lambda e, blk=blk: e.activation(out=ex[:, blk, :], in_=lg[:, blk, :], func=AF.Exp, bias=negm[:, blk:blk + 1], scale=1.0),
                 r=[f"lg{blk}", f"negm{blk}"], w=[f"ex{blk}"])
            P.op("dve", lambda e, blk=blk: e.tensor_scalar(msk[:, blk, :], lg[:, blk, :], m8[:, blk, 3:4], None, ALU.is_ge),
                 r=[f"lg{blk}", f"m8{blk}"], w=[f"msk{blk}"])
            P.op("dve", lambda e, blk=blk: e.tensor_tensor(ex[:, blk, :], ex[:, blk, :], msk[:, blk, :], ALU.mult),
                 r=[f"ex{blk}", f"msk{blk}"], w=[f"ex{blk}"])
            P.op("dve", lambda e, blk=blk: e.reduce_sum(ssum[:, blk:blk + 1], ex[:, blk, :], mybir.AxisListType.X),
                 r=[f"ex{blk}"], w=[f"ssum{blk}"])
            P.op("dve", lambda e, blk=blk: e.reciprocal(ssum[:, blk:blk + 1], ssum[:, blk:blk + 1]), r=[f"ssum{blk}"], w=[f"ssum{blk}"])
            P.op("dve", lambda e, blk=blk: e.tensor_scalar(gates[:, blk, :], ex[:, blk, :], ssum[:, blk:blk + 1], None, ALU.mult),
                 r=[f"ex{blk}", f"ssum{blk}"], w=[f"gates{blk}"])
            P.op("pe", lambda e, blk=blk, s=s: e.transpose(psG[s][0:NE, blk * 128:(blk + 1) * 128], gates[:, blk, :], ident[:]),
                 r=[f"gates{blk}"], w=[f"psG{s}"])
        P.op("act", lambda e, s=s, tok=tok: e.activation(out=gT[0:NE, tok], in_=psG[s][0:NE, :], func=AF.Copy), r=[f"psG{s}"], w=[f"gT.{tt}"])
        P.op("dve", lambda e, tok=tok: e.tensor_copy(gTb[0:NE, tok], gT[0:NE, tok]), r=[f"gT.{tt}", "gTbz"], w=[f"gTb.{tt}"])
        for fo in range(8):
            bb = fo % 2
            P.mm(psB[bb][:], b2n[0:NE, fo * 128:(fo + 1) * 128], gT[0:NE, tok], True, True, r=["b2n", f"gT.{tt}"], w=[f"psB{bb}"])
            P.op("dve", lambda e, bb=bb, fo=fo, tok=tok: e.scalar_tensor_tensor(
                x1T[:, fo, tok], psB[bb][:], G2(fo, b), x1T[:, fo, tok], ALU.mult, ALU.add),
                r=[f"psB{bb}", f"x1.{tt}"], w=[f"x1.{tt}"])
    P.run()


def moe_phase(nc, D, b, h2T, x1T, gT, G2):
    P = Prog(nc, f"moe{b}")
    w1 = D["w1"].rearrange("e (kc p) n -> e p kc n", p=128)
    w2 = D["w2"].rearrange("e (kc p) n -> e p kc n", p=128)
    b1 = P.sb([128, NE, 16], name="b1"); pid = P.sb([128, 128], name="pid")
    P.dma("sp", b1[:], D["b1T"], r=(), w=["b1"], chan="ldb1")
    P.dma("sp", pid[:], D["c_pid"], r=(), w=["pid"], chan="ldpid")
    P.op("dve", lambda e_: e_.tensor_scalar(b1[:, :, 8:16], b1[:, :, 8:16], 1.0, None, ALU.add), r=["b1"], w=["b1"])
    sel = [P.sb([128, 128], BF16, name=f"sel{i}") for i in range(2)]
    Wg = [P.sb([128, KC, 512], BF16, name=f"Wg{i}") for i in range(3)]
    Wl = [P.sb([128, KC, 512], BF16, name=f"Wl{i}") for i in range(3)]
    W2 = [P.sb([128, 4, 1024], BF16, name=f"W2{i}") for i in range(3)]
    actT = [P.sb([128, 4, 512], BF16, name=f"act{i}") for i in range(3)]
    gsb = [P.sb([128, 512], BF16, name=f"gsb{i}") for i in range(2)]
    Ta = [P.sb([128, 512], name=f"Ta{i}") for i in range(2)]
    Tb = [P.sb([128, 512], name=f"Tb{i}") for i in range(2)]
    Tc = [P.sb([128, 512], name=f"Tc{i}") for i in range(2)]
    pg = [P.ps(name=f"pg{i}") for i in range(2)]
    pl = [P.ps(name=f"pl{i}") for i in range(2)]
    po = [P.ps(name=f"po{i}") for i in range(3)]
    pgt = [P.ps(name=f"pgt{i}") for i in range(1)]
    units = [(e, hf) for e in range(NE) for hf in range(2)]

    def load(u):
        e, hf = units[u]
        r = u % 3
        P.dma("pool", Wg[r][:], w1[e][:, :, hf * 512:(hf + 1) * 512], r=(), w=[f"Wg{r}"], chan=f"Wg{r}")
        P.dma("pool", Wl[r][:], w1[e][:, :, 1024 + hf * 512: 1024 + (hf + 1) * 512], r=(), w=[f"Wl{r}"], chan=f"Wl{r}")
        P.dma("pool", W2[r][:], w2[e][:, hf * 4:(hf + 1) * 4, :], r=(), w=[f"W2{r}"], chan=f"W2{r}")
    load(0)
    steps = [(u, tt) for u in range(len(units)) for tt in range(4)]

    def SA_pre(t):
        u, tt = steps[t]
        e, hf = units[u]
        if tt == 0:
            if u + 1 < len(units):
                load(u + 1)
            if hf == 0:
                P.op("dve", lambda e_, e=e: e_.tensor_scalar(sel[e % 2][:], pid[:], float(e), None, ALU.is_equal), r=["pid"], w=[f"sel{e % 2}"])
        tok = slice(tt * 512, (tt + 1) * 512)
        ga = t % 2
        P.mm(pgt[0][:], sel[e % 2][:], gT[:, tok], True, True, r=[f"sel{e % 2}"], w=["pgt0"])
        P.op("act", lambda e_, ga=ga: e_.activation(out=gsb[ga][:], in_=pgt[0][:], func=AF.Copy), r=["pgt0"], w=[f"gsb{ga}"])

    def SA_head(t, j):
        u, tt = steps[t]
        e, hf = units[u]
        r = u % 3
        tok = slice(tt * 512, (tt + 1) * 512)
        jj = hf * 4 + j
        z = j % 2
        for kc in range(KC):
            P.mm(pg[z][:], Wg[r][:, kc, j * 128:(j + 1) * 128], h2T[:, kc, tok], kc == 0, kc == KC - 1, r=[f"Wg{r}"], w=[f"pg{z}"])
        for kc in range(KC):
            P.mm(pl[z][:], Wl[r][:, kc, j * 128:(j + 1) * 128], h2T[:, kc, tok], kc == 0, kc == KC - 1, r=[f"Wl{r}"], w=[f"pl{z}"])
        P.op("dve", lambda e_, z=z, e=e, jj=jj: e_.tensor_scalar(Ta[z][:], pg[z][:], b1[:, e, jj:jj + 1], 7.0, ALU.add, ALU.min),
             r=[f"pg{z}", "b1"], w=[f"Ta{z}"])
        P.op("act", lambda e_, z=z: e_.activation(out=Tb[z][:], in_=Ta[z][:], func=AF.Sigmoid, scale=1.702), r=[f"Ta{z}"], w=[f"Tb{z}"])
        P.op("dve", lambda e_, z=z, e=e, jj=jj: e_.tensor_scalar(Tc[z][:], pl[z][:], b1[:, e, 8 + jj:9 + jj], -6.0, ALU.add, ALU.max),
             r=[f"pl{z}", "b1"], w=[f"Tc{z}"])
        P.op("pool", lambda e_, z=z: e_.tensor_tensor(Tb[z][:], Ta[z][:], Tb[z][:], ALU.mult), r=[f"Ta{z}", f"Tb{z}"], w=[f"Tb{z}"])

    def SA_tail(t, j):
        z = j % 2
        a = t % 3
        ga = t % 2
        P.op("dve", lambda e_, z=z: e_.scalar_tensor_tensor(Tc[z][:], Tc[z][:], 8.0, Tb[z][:], ALU.min, ALU.mult),
             r=[f"Tb{z}", f"Tc{z}"], w=[f"Tc{z}"])
        P.op("dve", lambda e_, z=z, a=a, j=j, ga=ga: e_.tensor_tensor(actT[a][:, j, :], Tc[z][:], gsb[ga][:], ALU.mult),
             r=[f"Tc{z}", f"gsb{ga}"], w=[f"act{a}.{j}"])

    def SB(t):
        u, tt = steps[t]
        r = u % 3
        tok = slice(tt * 512, (tt + 1) * 512)
        a = t % 3
        for fo in range(8):
            y = (t * 8 + fo) % 3
            for j in range(4):
                P.mm(po[y][:], W2[r][:, j, fo * 128:(fo + 1) * 128], actT[a][:, j, :], j == 0, j == 3,
                     r=[f"W2{r}", f"act{a}.{j}"], w=[f"po{y}"])
            P.op("dve", lambda e_, y=y, fo=fo, tok=tok: e_.scalar_tensor_tensor(
                x1T[:, fo, tok], po[y][:], G2(fo, b), x1T[:, fo, tok], ALU.mult, ALU.add),
                r=[f"po{y}", f"x2.{tt}.{fo}"], w=[f"x2.{tt}.{fo}"])

    n_st = len(steps)
    for t in range(n_st + 2):
        act_ = t < n_st
        if act_:
            SA_pre(t)
            SA_head(t, 0)
            SA_head(t, 1)
            SA_tail(t, 0)
        if t >= 2:
            SB(t - 2)
        if act_:
            SA_head(t, 2)
            SA_tail(t, 1)
            SA_head(t, 3)
            SA_tail(t, 2)
            SA_tail(t, 3)
    P.run()


def final_phase(nc, D, b, x2T, fg, ones, EPS_AP, outT):
    P = Prog(nc, f"fin{b}")
    sq = [P.sb([128, KC, 512], name=f"sq{i}") for i in range(2)]
    ot = [P.sb([128, KC, 512], name=f"ot{i}") for i in range(2)]
    rt = [P.sb([128, 512], name=f"rt{i}") for i in range(2)]
    pss = [P.ps(name=f"ss{i}") for i in range(2)]
    for tt in range(4):
        s = tt % 2
        tok = slice(tt * 512, (tt + 1) * 512)
        P.op("act", lambda e, s=s, tok=tok: e.activation(out=sq[s][:], in_=x2T[:, :, tok], func=AF.Square), r=(), w=[f"sq{s}"])
        for kc in range(KC):
            P.mm(pss[s][:], ones[:], sq[s][:, kc, :], kc == 0, kc == KC - 1, r=[f"sq{s}"], w=[f"ss{s}"])
        P.op("act", lambda e, s=s: e.activation(out=rt[s][:], in_=pss[s][:], func=AF.Ln, bias=EPS_AP[:], scale=1.0 / D_MODEL),
             r=[f"ss{s}"], w=[f"rt{s}"])
        P.op("act", lambda e, s=s: e.activation(out=rt[s][:], in_=rt[s][:], func=AF.Exp, scale=-0.5), r=[f"rt{s}"], w=[f"rt{s}"])
        for kc in range(KC):
            P.op("dve", lambda e, kc=kc, s=s, tok=tok: e.scalar_tensor_tensor(
                ot[s][:, kc, :], x2T[:, kc, tok], fg[:, kc:kc + 1], rt[s][:], ALU.mult, ALU.mult),
                r=[f"rt{s}"], w=[f"ot{s}"])
        P.dma("sp", outT[:, :, b * SEQ + tt * 512: b * SEQ + (tt + 1) * 512], ot[s][:], r=[f"ot{s}"], w=(), chan=f"out{s}")
    P.run()


def _consts():
    p = np.arange(128)
    c = {}
    c["c_ones"] = np.ones((128, 128), np.float32)
    c["c_tri"] = (p[:, None] >= p[None, :]).astype(np.float32)
    mD = np.zeros((128, 4, 512), np.float32)
    for i in range(4):
        for j in range(4):
            if j > i:
                mD[:, i, j * 128:(j + 1) * 128] = 1.0
            elif j == i:
                mD[:, i, j * 128:(j + 1) * 128] = (p[None, :] > p[:, None]).astype(np.float32)
    c["c_maskD"] = mD
    c["c_maskLE"] = (p[:, None] <= p[None, :]).astype(np.float32)
    c["c_ident"] = np.eye(128, dtype=np.float32)
    c["c_identb"] = np.eye(128, dtype=np.float32).astype(ml_dtypes.bfloat16)
    s4 = np.zeros((4, 4, 128), np.float32)
    for h in range(4):
        s4[h, h, :] = 1.0
    c["c_sel4"] = s4
    pid = np.full((128, 128), -1.0, np.float32)
    pid[:32, :] = np.arange(32, dtype=np.float32)[:, None]
    c["c_pid"] = pid
    return c


def _fm(v, nch):
    return np.ascontiguousarray(np.asarray(v, np.float32).reshape(nch, 128).T)


def make_in_maps(inp, cores=range(NCORES)):
    f = lambda a: np.ascontiguousarray(np.asarray(a, np.float32))
    shared = {
        "ada_w": f(inp["ada_w"][0]), "ada_bT": _fm(inp["ada_b"][0], 48),
        "n1g": _fm(inp["norm1_g"][0], 8), "n2g": _fm(inp["norm2_g"][0], 8), "fg": _fm(inp["final_g"], 8),
        "mlg": _fm(inp["ml_norm_g"][0], 8),
        "w_in": f(inp["w_in"][0]),
        "cwT": np.ascontiguousarray(np.asarray(inp["conv_w"][0], np.float32).reshape(4, 16, 128).transpose(2, 1, 0)),
        "cbT": _fm(inp["conv_b"][0], 16),
        "bi": f(inp["ml_b_i"][0]).reshape(4, 1), "bf": f(inp["ml_b_f"][0]).reshape(4, 1),
        "w_a": f(inp["w_branch_a"][0]), "w_b": f(inp["w_branch_b"][0]), "w_out": f(inp["w_out"][0]),
        "router_w": f(inp["router_w"][0]),
        "rb_rep": np.ascontiguousarray(np.broadcast_to(np.asarray(inp["router_b"][0], np.float32)[None, :], (128, NE))),
        "w1": f(inp["expert_w1"][0]),
        "b1T": np.ascontiguousarray(np.asarray(inp["expert_b1"][0], np.float32).reshape(NE, 16, 128).transpose(2, 0, 1)),
        "w2": f(inp["expert_w2"][0]), "b2": f(inp["expert_b2"][0]),
    }
    shared.update(_consts())
    x = np.asarray(inp["x"], np.float32)
    c = np.asarray(inp["c"], np.float32)
    maps = []
    for i in cores:
        xs = x[2 * i:2 * i + 2].reshape(NTOK, KC, 128)
        m = dict(shared)
        m["xT"] = np.ascontiguousarray(xs.transpose(2, 1, 0))
        m["cT"] = np.ascontiguousarray(c[2 * i:2 * i + 2].reshape(2, KC, 128).transpose(2, 1, 0))
        maps.append(m)
    return maps


def kernel(**inputs):
    nc = build()
    maps = make_in_maps(inputs)
    res = run_bass_kernel_spmd(nc, maps, core_ids=list(range(NCORES)))
    out = np.empty((16, SEQ, D_MODEL), np.float32)
    for i in range(NCORES):
        o = res.results[i]["outT"]
        out[2 * i:2 * i + 2] = o.transpose(2, 1, 0).reshape(2, SEQ, D_MODEL)
    return out
```

```python
import numpy as np
from contextlib import ExitStack
import ml_dtypes
import concourse.bass as bass
import concourse.mybir as mybir
from concourse.bass_utils import run_bass_kernel_spmd

F32 = mybir.dt.float32
BF16 = mybir.dt.bfloat16
F32R = mybir.dt.float32r
AF = mybir.ActivationFunctionType
ALU = mybir.AluOpType

NCORES = 8
D_MODEL = 1024
SEQ = 2048
NTOK = 2 * SEQ
NE = 32
EPS = 1e-6
KC = 8
COL_Q, COL_K, COL_V = 0, 1024, 2048
COL_MQ, COL_MK, COL_MV, COL_MO = 3072, 4096, 5120, 6144
COL_I, COL_F, COL_GA, COL_GB = 7168, 7172, 7176, 8200


class _Op:
    __slots__ = ("id", "eng", "fn", "deps", "chan", "chan_val", "sig")


class _SemPool:
    def __init__(self, nc):
        self.nc = nc
        self.esem = {e: nc.alloc_semaphore(f"eng_{e}") for e in ("pe", "act", "dve", "pool", "sp")}
        self.ecnt = {e: 0 for e in self.esem}
        self.csem = {}
        self.ccnt = {}

    def chan(self, c):
        if c not in self.csem:
            self.csem[c] = self.nc.alloc_semaphore(f"ch_{len(self.csem)}")
            self.ccnt[c] = 0
        return self.csem[c]


_POOLS = {}


class Prog:
    def __init__(self, nc, name):
        self.nc = nc
        self.name = name
        self.es = ExitStack()
        self.ops = []
        self.lastw = {}
        self.readers = {}
        self.chan_n = {}
        self.nsb = 0
        if id(nc) not in _POOLS:
            _POOLS.clear()
            _POOLS[id(nc)] = _SemPool(nc)
        self.pool = _POOLS[id(nc)]

    def sb(self, shape, dt=F32, name=None):
        self.nsb += 1
        return self.es.enter_context(self.nc.sbuf_tensor(f"{self.name}_s{self.nsb}_{name or ''}", list(shape), dt))

    def ps(self, shape=(128, 512), dt=F32, name=None):
        self.nsb += 1
        return self.es.enter_context(self.nc.psum_tensor(f"{self.name}_p{self.nsb}_{name or ''}", list(shape), dt))

    def op(self, eng, fn, r=(), w=(), chan=None):
        o = _Op()
        o.id = len(self.ops)
        o.eng = eng
        o.fn = fn
        o.chan = chan
        o.chan_val = 0
        o.sig = 0
        deps = {}
        for k in r:
            d = self.lastw.get(k)
            if d is not None:
                deps[d] = "raw"
        for k in w:
            d = self.lastw.get(k)
            if d is not None and d not in deps:
                deps[d] = "waw"
            for d in self.readers.get(k, {}).values():
                if d not in deps:
                    deps[d] = "war"
        o.deps = deps
        if chan is not None:
            self.pool.chan(chan)
            self.pool.ccnt[chan] += 1
            self.chan_n[chan] = self.pool.ccnt[chan]
            o.chan_val = 16 * self.chan_n[chan]
        self.ops.append(o)
        rk = (eng, chan)
        for k in r:
            self.readers.setdefault(k, {})[rk] = o.id
        for k in w:
            self.lastw[k] = o.id
            self.readers[k] = {}
        return o.id

    def mm(self, out, lhsT, rhs, start, stop, r, w, sgc=False):
        if sgc:
            return self.op("pe", lambda e: e.matmul(out, lhsT, rhs, start=start, stop=stop, skip_group_check=True), r, w)
        return self.op("pe", lambda e: e.matmul(out, lhsT, rhs, start=start, stop=stop), r, w)

    def dma(self, eng, out, in_, r, w, chan):
        return self.op(eng, lambda e: e.dma_start(out=out, in_=in_), r, w, chan=chan)

    def _skip(self, p, o, typ):
        return (p.chan is None and o.chan is None and p.eng == o.eng and p.eng == "pe")

    def run(self):
        nc = self.nc
        ops = self.ops
        engs = ("pe", "act", "dve", "pool", "sp")
        need = [False] * len(ops)
        for o in ops:
            for d, typ in o.deps.items():
                p = ops[d]
                if p.chan is not None or self._skip(p, o, typ):
                    continue
                need[d] = True
        cnt = self.pool.ecnt
        for o in ops:
            if o.chan is None and need[o.id]:
                cnt[o.eng] += 1
                o.sig = cnt[o.eng]
        with ExitStack() as es:
            esem = self.pool.esem
            csem = self.pool.csem
            by_eng = {e: [o for o in ops if o.eng == e] for e in engs}
            with nc.Block() as block:
                def emit(e, name):
                    waited = {}
                    for o in by_eng[name]:
                        want = {}
                        for d, typ in o.deps.items():
                            p = ops[d]
                            if p.chan is not None:
                                key, val = ("c", p.chan), p.chan_val
                            else:
                                if self._skip(p, o, typ):
                                    continue
                                key, val = ("e", p.eng), p.sig
                            if val > want.get(key, 0):
                                want[key] = val
                        for key, val in want.items():
                            if waited.get(key, 0) >= val:
                                continue
                            waited[key] = val
                            e.wait_ge(csem[key[1]] if key[0] == "c" else esem[key[1]], val)
                        ins = o.fn(e)
                        if o.chan is not None:
                            ins.then_inc(csem[o.chan], 16)
                        elif o.sig:
                            ins.then_inc(esem[name], 1)
                    if name == "sp":
                        for c, n in self.chan_n.items():
                            if waited.get(("c", c), 0) < 16 * n:
                                e.wait_ge(csem[c], 16 * n)

                @block.tensor
                def _(e):
                    emit(e, "pe")

                @block.scalar
                def _(e):
                    emit(e, "act")

                @block.vector
                def _(e):
                    emit(e, "dve")

                @block.gpsimd
                def _(e):
                    emit(e, "pool")

                @block.sync
                def _(e):
                    emit(e, "sp")
        self.es.close()


def _tap(nc, taps, name, ap, shape, dt=F32):
    if taps is None or name not in taps:
        return
    d = nc.dram_tensor("dbg_" + name, list(shape), dt, kind="ExternalOutput").ap()
    if id(nc) not in _POOLS:
        _POOLS.clear()
        _POOLS[id(nc)] = _SemPool(nc)
    pool = _POOLS[id(nc)]
    s = pool.chan("tap")
    pool.ccnt["tap"] += 1
    v = 16 * pool.ccnt["tap"]
    with nc.Block() as blk:
        @blk.sync
        def _(e):
            e.dma_start(out=d, in_=ap).then_inc(s, 16)
            e.wait_ge(s, v)


def build(upto=99, taps=None):
    nc = bass.Bass("TRN2", target_bir_lowering=False)
    D = {}

    def din(name, shape, dt=F32):
        D[name] = nc.dram_tensor(name, list(shape), dt, kind="ExternalInput").ap()

    din("xT", [128, KC, NTOK]); din("cT", [128, KC, 2]); din("ada_w", [1024, 6144]); din("ada_bT", [128, 48])
    din("n1g", [128, KC]); din("n2g", [128, KC]); din("fg", [128, KC]); din("mlg", [128, KC])
    din("w_in", [1024, 9224]); din("cwT", [128, 16, 4]); din("cbT", [128, 16]); din("bi", [4, 1]); din("bf", [4, 1])
    din("w_a", [1024, 1024]); din("w_b", [1024, 1024]); din("w_out", [1024, 1024])
    din("router_w", [1024, NE]); din("rb_rep", [128, NE])
    din("w1", [NE, 1024, 2048]); din("b1T", [128, NE, 16]); din("w2", [NE, 1024, 1024]); din("b2", [NE, 1024])
    din("c_ones", [128, 128]); din("c_tri", [128, 128]); din("c_maskD", [128, 4, 512]); din("c_maskLE", [128, 128])
    din("c_ident", [128, 128]); din("c_identb", [128, 128], BF16); din("c_sel4", [4, 4, 128]); din("c_pid", [128, 128])
    outT = nc.dram_tensor("outT", [128, KC, NTOK], F32, kind="ExternalOutput").ap()

    def wview(ap2d):
        return ap2d.rearrange("(kc p) n -> p kc n", p=128)

    with ExitStack() as top:
        def tsb(name, shape, dt=F32):
            return top.enter_context(nc.sbuf_tensor("g_" + name, list(shape), dt))

        modT = tsb("modT", [128, 48, 2])
        A1 = tsb("A1", [128, KC, 2]); A2 = tsb("A2", [128, KC, 2])
        ones = tsb("ones", [128, 128]); n1g = tsb("n1g", [128, KC]); n2g = tsb("n2g", [128, KC])
        fg = tsb("fg", [128, KC]); mlg = tsb("mlg", [128, KC])
        ident = tsb("ident", [128, 128])
        hT = tsb("hT", [128, KC, SEQ], BF16)
        Y = tsb("Y", [128, 2, KC, SEQ], BF16)
        yaT = Y[:, 0]
        ybT = Y[:, 1]
        x1T = Y[:].rearrange("p a k t -> p (a k t)").bitcast(F32).rearrange("p (k t) -> p k t", k=KC)

        P = Prog(nc, "p0")
        cT = P.sb([128, KC, 2], name="cT"); sc = P.sb([128, KC, 2], name="sc")
        abT = P.sb([128, 48], name="abT")
        wbuf = [P.sb([128, KC, 1024], name=f"adaw{i}") for i in range(2)]
        pm = [P.ps(name=f"pm{i}") for i in range(2)]
        for i, (t, nm) in enumerate([(ones, "c_ones"), (n1g, "n1g"), (n2g, "n2g"), (fg, "fg"), (mlg, "mlg"),
                                     (ident, "c_ident"), (cT, "cT"), (abT, "ada_bT")]):
            P.dma("sp", t[:], D[nm], r=(), w=[nm], chan="ld" + nm)
        P.op("act", lambda e: e.activation(out=sc[:], in_=cT[:], func=AF.Silu), r=["cT"], w=["sc"])
        adaw = wview(D["ada_w"])
        for m in range(6):
            wb = wbuf[m % 2]
            P.dma("sp", wb[:], adaw[:, :, m * 1024:(m + 1) * 1024], r=(), w=[f"adaw{m % 2}"], chan=f"adaw{m % 2}")
            pmm = pm[m % 2][:, 0:16].rearrange("p (j b) -> p j b", b=2)
            for jj in range(8):
                for kc in range(KC):
                    P.mm(pmm[:, jj, :], wb[:, kc, jj * 128:(jj + 1) * 128], sc[:, kc, :], kc == 0, kc == KC - 1,
                         r=[f"adaw{m % 2}", "sc"], w=[f"pm{m % 2}"])
            for b in range(2):
                P.op("dve", lambda e, m=m, b=b, pmm=pmm: e.tensor_tensor(
                    modT[:, m * 8:(m + 1) * 8, b], pmm[:, :, b], abT[:, m * 8:(m + 1) * 8], ALU.add),
                    r=[f"pm{m % 2}", "ada_bT"], w=["modT"])
        for b in range(2):
            P.op("dve", lambda e, b=b: e.scalar_tensor_tensor(A1[:, :, b], modT[:, 8:16, b], 1.0, n1g[:], ALU.add, ALU.mult),
                 r=["modT", "n1g"], w=["A1"])
            P.op("dve", lambda e, b=b: e.scalar_tensor_tensor(A2[:, :, b], modT[:, 32:40, b], 1.0, n2g[:], ALU.add, ALU.mult),
                 r=["modT", "n2g"], w=["A2"])
        P.run()
        _tap(nc, taps, "modT", modT[:], [128, 48, 2])
        _tap(nc, taps, "A1", A1[:], [128, KC, 2])

        def SH1(kc, b): return modT[:, 0 + kc, b:b + 1]
        def G1(kc, b): return modT[:, 16 + kc, b:b + 1]
        def SH2(kc, b): return modT[:, 24 + kc, b:b + 1]
        def G2(kc, b): return modT[:, 40 + kc, b:b + 1]

        def norm_phase(name, b, src_dram, src_sb, Aap, SHap, dst_bf, extra=None):
            P = Prog(nc, name)
            xt = [P.sb([128, KC, 512], name=f"xt{i}") for i in range(2)] if src_sb is None else None
            sq = [P.sb([128, KC, 512], name=f"sq{i}") for i in range(2)]
            rt = [P.sb([128, 512], name=f"rt{i}") for i in range(2)]
            rs = [P.sb([128, 512], name=f"rs{i}") for i in range(2)]
            tmp = [P.sb([128, 512], name=f"tmp{i}") for i in range(4)]
            pss = [P.ps(name=f"ss{i}") for i in range(2)]
            ctx = extra(P) if extra else None
            for tt in range(4):
                s = tt % 2
                tok = slice(tt * 512, (tt + 1) * 512)
                if src_sb is None:
                    P.dma("sp", xt[s][:], src_dram[:, :, b * SEQ + tt * 512: b * SEQ + (tt + 1) * 512], r=(), w=[f"xt{s}"], chan=f"xt{s}")
                    xin = xt[s]
                    xk = [f"xt{s}"]
                    xv = lambda kc, xin=xin: xin[:, kc, :]
                    xall = xin[:]
                else:
                    xk = [f"x1.{tt}"]
                    xv = lambda kc, tok=tok: src_sb[:, kc, tok]
                    xall = src_sb[:, :, tok]
                P.op("act", lambda e, s=s, xall=xall: e.activation(out=sq[s][:], in_=xall, func=AF.Square), r=xk, w=[f"sq{s}"])
                for kc in range(KC):
                    P.mm(pss[s][:], ones[:], sq[s][:, kc, :], kc == 0, kc == KC - 1, r=[f"sq{s}"], w=[f"ss{s}"])
                P.op("act", lambda e, s=s: e.activation(out=rt[s][:], in_=pss[s][:], func=AF.Ln, bias=EPS_AP[:], scale=1.0 / D_MODEL),
                     r=[f"ss{s}"], w=[f"rt{s}"])
                P.op("act", lambda e, s=s: e.activation(out=rs[s][:], in_=rt[s][:], func=AF.Exp, scale=-0.5), r=[f"rt{s}"], w=[f"rs{s}"])
                for kc in range(KC):
                    q = kc % 4
                    P.op("dve", lambda e, kc=kc, q=q, s=s, xv=xv: e.scalar_tensor_tensor(
                        tmp[q][:], xv(kc), Aap(kc, b), rs[s][:], ALU.mult, ALU.mult),
                        r=xk + [f"rs{s}"], w=[f"tmp{q}"])
                    if ctx is None:
                        P.op("act", lambda e, kc=kc, q=q, tok=tok: e.activation(
                            out=dst_bf[:, kc, tok], in_=tmp[q][:], func=AF.Identity, bias=SHap(kc, b), scale=1.0),
                            r=[f"tmp{q}"], w=[f"h.{tt}"])
                    else:
                        ctx["per_kc"](P, tt, kc, q, tmp[q], tok)
                if ctx is not None:
                    ctx["per_tile"](P, tt, tok)
            if ctx is not None and "final" in ctx:
                ctx["final"](P)
            P.run()

        EPS_AP = tsb("eps", [128, 1])
        P = Prog(nc, "pc")
        P.op("dve", lambda e: e.memset(EPS_AP[:], EPS), r=(), w=["eps"])
        P.run()

        for b in range(2):
            norm_phase(f"n1_{b}", b, D["xT"], None, lambda kc, b: A1[:, kc, b:b + 1], SH1, hT)
            if b == 0:
                _tap(nc, taps, "hT", hT[:], [128, KC, SEQ], BF16)
            if upto <= 1:
                continue
            scratch = Y[:, 0].rearrange("p k t -> p (k t)").bitcast(F32)
            mlstm_phase(nc, D, b, hT, ybT, scratch, wview, mlg, ones, ident, EPS_AP, taps)
            if b == 0:
                _tap(nc, taps, "ybT", ybT, [128, KC, SEQ], BF16)
            if upto <= 2:
                continue
            sb_attention(nc, D, b, hT, yaT, wview)
            if b == 0:
                _tap(nc, taps, "yaT", yaT, [128, KC, SEQ], BF16)
            if upto <= 3:
                continue
            with nc.sbuf_tensor(f"mergedT{b}", [128, KC, SEQ], BF16) as mergedT:
                merge_phase(nc, D, b, hT, yaT, ybT, mergedT, wview)
                if b == 0:
                    _tap(nc, taps, "mergedT", mergedT[:], [128, KC, SEQ], BF16)
                outproj_phase(nc, D, b, mergedT, x1T, G1, wview)
            if b == 0:
                _tap(nc, taps, "x1T", x1T, [128, KC, SEQ])
            if upto <= 4:
                continue
            with nc.sbuf_tensor(f"gT{b}", [128, SEQ], BF16) as gT:
                norm2_router_phase(nc, D, b, x1T, hT, gT, A2, SH2, G2, ones, ident, EPS_AP, wview)
                if b == 0:
                    _tap(nc, taps, "h2T", hT[:], [128, KC, SEQ], BF16)
                    _tap(nc, taps, "gT", gT[0:NE, :], [NE, SEQ], BF16)
                    _tap(nc, taps, "x1bT", x1T, [128, KC, SEQ])
                if upto <= 5:
                    continue
                moe_phase(nc, D, b, hT, x1T, gT, G2)
            if b == 0:
                _tap(nc, taps, "x2T", x1T, [128, KC, SEQ])
            final_phase(nc, D, b, x1T, fg, ones, EPS_AP, outT)
    return nc


def sb_attention(nc, D, b, hT, yaT, wview):
    P = Prog(nc, f"sb{b}")
    win = wview(D["w_in"])
    ones = P.sb([128, 128], name="ones"); tri = P.sb([128, 128], name="tri")
    maskD = P.sb([128, 4, 512], name="maskD")
    for t, nm in [(ones, "c_ones"), (tri, "c_tri"), (maskD, "c_maskD")]:
        P.dma("sp", t[:], D[nm], r=(), w=[nm], chan="ld" + nm)
    P.op("dve", lambda e: e.tensor_copy(trir[:], tri[:]), r=["c_tri"], w=["trir"])
    P.op("dve", lambda e: e.tensor_copy(onesr[:], ones[:]), r=["c_ones"], w=["onesr"])
    Wq = [P.sb([128, KC, 128], BF16, name=f"wq{i}") for i in range(2)]
    Wk = [P.sb([128, KC, 128], BF16, name=f"wk{i}") for i in range(2)]
    Wv = [P.sb([128, KC, 128], BF16, name=f"wv{i}") for i in range(2)]
    qT = [P.sb([128, SEQ], BF16, name=f"qT{i}") for i in range(2)]
    kTh_ = [[P.sb([128, SEQ], BF16, name=f"kT{i}_{hh}") for hh in range(2)] for i in range(2)]
    V = [P.sb([128, 16, 2, 128], BF16, name=f"V{i}") for i in range(2)]
    for i in range(2):
        P.op("pool", lambda e, i=i: e.memset(kTh_[i][0][64:128, :], 0.0), r=(), w=[f"kz{i}"])
        P.op("pool", lambda e, i=i: e.memset(kTh_[i][1][0:64, :], 0.0), r=(), w=[f"kz{i}"])
        P.op("pool", lambda e, i=i: e.memset(V[i][:], 0.0), r=(), w=[f"vz{i}"])
    et = [P.sb([128, 512], name=f"et{i}") for i in range(4)]
    spt = [P.sb([128, 512], F32R, name=f"spt{i}") for i in range(4)]
    ext = [P.sb([128, 512], name=f"ext{i}") for i in range(4)]
    AT = [P.sb([128, 512], BF16, name=f"AT{i}") for i in range(4)]
    R = [P.sb([128, 512], F32R, name=f"R{i}") for i in range(2)]
    trir = P.sb([128, 128], F32R, name="trir"); onesr = P.sb([128, 128], F32R, name="onesr")
    psz = [P.ps(name=f"z{i}") for i in range(2)]
    pscs = [P.ps(name=f"cs{i}") for i in range(2)]
    psy = [P.ps(name=f"y{i}") for i in range(2)]
    pspj = [P.ps(name=f"pj{i}") for i in range(2)]
    npj = [0]
    it = 0

    def proj_chunks(hp):
        s = hp % 2
        ch = []

        def dmas():
            for (W, col, nm) in [(Wq, COL_Q, "wq"), (Wk, COL_K, "wk"), (Wv, COL_V, "wv")]:
                P.dma("pool", W[s][:], win[:, :, col + hp * 128: col + (hp + 1) * 128], r=(), w=[f"{nm}{s}"], chan=f"{nm}{s}")
        ch.append(dmas)
        for tt in range(4):
            for nm in ("q", "k"):
                def c_(tt=tt, nm=nm):
                    tok = slice(tt * 512, (tt + 1) * 512)
                    W = Wq if nm == "q" else Wk
                    pj = npj[0] % 2
                    npj[0] += 1
                    for kc in range(KC):
                        P.mm(pspj[pj][:], W[s][:, kc, :], hT[:, kc, tok], kc == 0, kc == KC - 1,
                             r=[f"w{nm}{s}", f"h.{tt}"], w=[f"pj{pj}"])
                    if nm == "q":
                        P.op("dve", lambda e, pj=pj, o_=qT[s][:, tok]: e.tensor_scalar(o_, pspj[pj][:], 0.125, None, ALU.mult),
                             r=[f"pj{pj}"], w=[f"qT{s}.{tt}"])
                    else:
                        P.op("dve", lambda e, pj=pj, o_=kTh_[s][0][0:64, tok]: e.tensor_copy(o_, pspj[pj][0:64, :]),
                             r=[f"pj{pj}", f"kz{s}"], w=[f"kT{s}.{tt}"])
                        P.op("dve", lambda e, pj=pj, o_=kTh_[s][1][64:128, tok]: e.tensor_copy(o_, pspj[pj][64:128, :]),
                             r=[f"pj{pj}", f"kz{s}"], w=[f"kT{s}.{tt}"])
                ch.append(c_)
        for g4 in range(4):
            def c_(g4=g4):
                pj = npj[0] % 2
                npj[0] += 1
                pv = pspj[pj][:].rearrange("p (j c) -> p j c", c=128)
                for j in range(4):
                    blk = g4 * 4 + j
                    for kc in range(KC):
                        P.mm(pv[:, j, :], hT[:, kc, blk * 128:(blk + 1) * 128], Wv[s][:, kc, :], kc == 0, kc == KC - 1,
                             r=[f"wv{s}", f"h.{g4}"], w=[f"pj{pj}"])
                P.op("dve", lambda e, o_=V[s][:, g4 * 4:(g4 + 1) * 4, 0, 0:64], pv=pv: e.tensor_copy(o_, pv[:, :, 0:64]),
                     r=[f"pj{pj}", f"vz{s}"], w=[f"V{s}.{g4}"])
                P.op("dve", lambda e, o_=V[s][:, g4 * 4:(g4 + 1) * 4, 1, 64:128], pv=pv: e.tensor_copy(o_, pv[:, :, 64:128]),
                     r=[f"pj{pj}", f"vz{s}"], w=[f"V{s}.{g4}"])
            ch.append(c_)
        return ch

    for c_ in proj_chunks(0):
        c_()
    for hp in range(8):
        s = hp % 2
        nxt = proj_chunks(hp + 1) if hp + 1 < 8 else []
        iters = []
        for qc in range(4):
            nkb = 4 * qc + 4
            for idx, kb in enumerate(range(nkb - 1, -1, -1)):
                for hh in range(2):
                    iters.append((qc, idx, kb, hh))
        base = it
        it += len(iters)
        triS = maskD[:, 0, 0:128]

        def prm(t, iters=iters, base=base):
            qc, idx, kb, hh = iters[t]
            g = base + t
            c0 = 128 * max(0, kb - 4 * qc)
            return qc, idx, kb, hh, g % 4, g % 2, c0

        def SA_mm(t, s=s):
            qc, idx, kb, hh, u, z, c0 = prm(t)
            P.mm(psz[z][:, c0:], kTh_[s][hh][:, kb * 128:(kb + 1) * 128], qT[s][:, qc * 512 + c0:(qc + 1) * 512], True, True,
                 r=[f"kT{s}.{kb // 4}", f"qT{s}.{qc}"], w=[f"z{z}"])

        def SA_exp(t):
            qc, idx, kb, hh, u, z, c0 = prm(t)
            P.op("act", lambda e, u=u, z=z, c0=c0: e.activation(out=et[u][:, c0:], in_=psz[z][:, c0:], func=AF.Exp), r=[f"z{z}"], w=[f"et{u}"])

        def SA_ln(t):
            qc, idx, kb, hh, u, z, c0 = prm(t)
            P.op("act", lambda e, u=u, c0=c0: e.activation(out=spt[u][:, c0:], in_=et[u][:, c0:], func=AF.Ln, bias=1.0, scale=1.0),
                 r=[f"et{u}"], w=[f"spt{u}"])

        def SA_mask(t):
            qc, idx, kb, hh, u, z, c0 = prm(t)
            if kb >= 4 * qc:
                P.op("dve", lambda e, u=u, c0=c0: e.tensor_tensor(spt[u][:, c0:c0 + 128], spt[u][:, c0:c0 + 128], triS, ALU.mult),
                     r=[f"spt{u}", "c_maskD"], w=[f"spt{u}"])
                P.op("dve", lambda e, u=u, c0=c0: e.tensor_tensor(et[u][:, c0:c0 + 128], et[u][:, c0:c0 + 128], triS, ALU.mult),
                     r=[f"et{u}", "c_maskD"], w=[f"et{u}"])

        def SB_mm(t):
            qc, idx, kb, hh, u, z, c0 = prm(t)
            P.mm(pscs[z][:, c0:], trir[:], spt[u][:, c0:], True, idx == 0, r=[f"spt{u}", "trir"], w=[f"cs{z}"])
            if idx > 0:
                P.mm(pscs[z][:, c0:], onesr[:], R[hh][:, c0:], False, True, r=[f"R{hh}", "onesr"], w=[f"cs{z}"])

        def SB_pool(t):
            qc, idx, kb, hh, u, z, c0 = prm(t)
            if kb > 0:
                if idx == 0:
                    if c0 > 0:
                        P.op("dve", lambda e, hh=hh, c0=c0: e.tensor_copy(R[hh][:, 0:c0], maskD[:, 3, 0:c0]), r=["c_maskD"], w=[f"R{hh}"])
                    P.op("dve", lambda e, u=u, hh=hh, c0=c0: e.tensor_copy(R[hh][:, c0:], spt[u][:, c0:]), r=[f"spt{u}"], w=[f"R{hh}"])
                else:
                    P.op("dve", lambda e, u=u, hh=hh, c0=c0: e.tensor_tensor(R[hh][:, c0:], R[hh][:, c0:], spt[u][:, c0:], ALU.add),
                         r=[f"R{hh}", f"spt{u}"], w=[f"R{hh}"])

        def SB_exp(t):
            qc, idx, kb, hh, u, z, c0 = prm(t)
            P.op("act", lambda e, u=u, z=z, c0=c0: e.activation(out=ext[u][:, c0:], in_=pscs[z][:, c0:], func=AF.Exp, scale=-1.0),
                 r=[f"cs{z}"], w=[f"ext{u}"])

        def SB_mult(t):
            qc, idx, kb, hh, u, z, c0 = prm(t)
            P.op("dve", lambda e, u=u, c0=c0: e.tensor_tensor(AT[u][:, c0:], et[u][:, c0:], ext[u][:, c0:], ALU.mult),
                 r=[f"et{u}", f"ext{u}"], w=[f"AT{u}"])

        def SC(t, s=s, hp=hp):
            qc, idx, kb, hh, u, z, c0 = prm(t)
            yb = (hp * 4 + qc) % 2
            P.mm(psy[yb][:, c0:], V[s][:, kb, hh, :], AT[u][:, c0:], idx == 0 and hh == 0, kb == 0 and hh == 1,
                 r=[f"V{s}.{kb // 4}", f"AT{u}"], w=[f"y{yb}.0", f"y{yb}.1"], sgc=True)
            if kb == 0 and hh == 1:
                P.op("dve", lambda e, yb=yb, o_=yaT[:, hp, qc * 512:(qc + 1) * 512]: e.tensor_copy(o_, psy[yb][:]),
                     r=[f"y{yb}.0", f"y{yb}.1"], w=[f"ya.{qc}"])

        n_pairs = len(iters) // 2
        for p in range(n_pairs + 2):
            if nxt and p % 3 == 0:
                nxt.pop(0)()
            if p < n_pairs:
                for f in (SA_mm, SA_exp, SA_ln, SA_mask):
                    f(2 * p)
                    f(2 * p + 1)
            if 0 <= p - 1 < n_pairs:
                for f in (SB_mm, SB_pool, SB_exp, SB_mult):
                    f(2 * (p - 1))
                    f(2 * (p - 1) + 1)
            if 0 <= p - 2 < n_pairs:
                SC(2 * (p - 2))
                SC(2 * (p - 2) + 1)
        while nxt:
            nxt.pop(0)()
    P.run()


def mlstm_phase(nc, D, b, hT, ybT, scratch, wview, mlg, ones, ident, EPS_AP, taps=None):
    win = wview(D["w_in"])
    NUMv = scratch[:, 0:4096].rearrange("p (a t) -> p a t", a=2)
    XP = scratch[:, 4096:6147]
    with ExitStack() as mes:
        def msb(name, shape, dt=F32):
            return mes.enter_context(nc.sbuf_tensor(f"ml{b}_{name}", list(shape), dt))
        Mrow = msb("Mrow", [4, SEQ]); WIrow = msb("WIrow", [4, SEQ]); EMRrow = msb("EMRrow", [4, SEQ])
        GT = msb("GT", [128, 16, 4]); WST = msb("WST", [128, 16, 4]); DEC = msb("DEC", [4, 16])
        sel4 = msb("sel4", [4, 4, 128]); maskLE = msb("maskLE", [128, 128]); identb = msb("identb", [128, 128], BF16)
        cwT = msb("cwT", [128, 16, 4]); cbT = msb("cbT", [128, 16])
        P = Prog(nc, f"mla{b}")
        for t, nm in [(sel4, "c_sel4"), (maskLE, "c_maskLE"), (identb, "c_identb"), (cwT, "cwT"), (cbT, "cbT")]:
            P.dma("sp", t[:], D[nm], r=(), w=[nm], chan="ld" + nm)
        bi = P.sb([4, 1], name="bi"); bfv = P.sb([4, 1], name="bf"); nbf = P.sb([4, 1], name="nbf")
        P.dma("sp", bi[:], D["bi"], r=(), w=["bi"], chan="ldbi")
        P.dma("sp", bfv[:], D["bf"], r=(), w=["bf"], chan="ldbf")
        Wif32 = P.sb([128, KC, 8], name="Wif32")
        Wi = P.sb([128, KC, 4], BF16, name="Wi"); Wf = P.sb([128, KC, 4], BF16, name="Wf")
        P.dma("sp", Wif32[:], win[:, :, COL_I:COL_I + 8], r=(), w=["Wif32"], chan="ldWif")
        P.op("dve", lambda e: e.tensor_copy(Wi[:], Wif32[:, :, 0:4]), r=["Wif32"], w=["Wi"])
        P.op("dve", lambda e: e.tensor_copy(Wf[:], Wif32[:, :, 4:8]), r=["Wif32"], w=["Wf"])
        Grow = P.sb([4, SEQ], name="Grow"); SP = P.sb([4, SEQ], name="SP"); CS = P.sb([4, SEQ], name="CS")
        WSrow = P.sb([4, SEQ], name="WSrow"); Mprev = P.sb([4, 16], name="Mprev")
        psI = [P.ps(name=f"psI{i}") for i in range(2)]
        psF = [P.ps(name=f"psF{i}") for i in range(2)]
        psT = P.ps(name="psT"); psT2 = P.ps(name="psT2")
        P.op("dve", lambda e: e.tensor_scalar(nbf[:], bfv[:], -1.0, None, ALU.mult), r=["bf"], w=["nbf"])
        for tt in range(4):
            s = tt % 2
            tok = slice(tt * 512, (tt + 1) * 512)
            for kc in range(KC):
                P.mm(psI[s][0:4, :], Wi[:, kc, :], hT[:, kc, tok], kc == 0, kc == KC - 1, r=["Wi"], w=[f"psI{s}"])
            for kc in range(KC):
                P.mm(psF[s][0:4, :], Wf[:, kc, :], hT[:, kc, tok], kc == 0, kc == KC - 1, r=["Wf"], w=[f"psF{s}"])
            P.op("dve", lambda e, s=s, tok=tok: e.tensor_scalar(Grow[:, tok], psI[s][0:4, :], bi[:, 0:1], None, ALU.add),
                 r=[f"psI{s}", "bi"], w=["Grow"])
            P.op("act", lambda e, s=s, tok=tok: e.activation(out=SP[:, tok], in_=psF[s][0:4, :], func=AF.Exp, bias=nbf[:, 0:1], scale=-1.0),
                 r=[f"psF{s}", "nbf"], w=["SP"])
        P.op("act", lambda e: e.activation(out=SP[:], in_=SP[:], func=AF.Ln, bias=1.0, scale=1.0), r=["SP"], w=["SP"])
        P.op("dve", lambda e: e.tensor_tensor_scan(CS[:], SP[:], SP[:], 0.0, ALU.add, ALU.max), r=["SP"], w=["CS"])
        P.op("dve", lambda e: e.tensor_tensor(Grow[:], Grow[:], CS[:], ALU.add), r=["Grow", "CS"], w=["Grow"])
        P.op("dve", lambda e: e.tensor_tensor_scan(Mrow[:], Grow[:], Grow[:], 0.0, ALU.max, ALU.max), r=["Grow"], w=["Mrow"])
        M3 = Mrow[:].rearrange("p (c t) -> p c t", t=128)
        G3 = Grow[:].rearrange("p (c t) -> p c t", t=128)
        WI3 = WIrow[:].rearrange("p (c t) -> p c t", t=128)
        WS3 = WSrow[:].rearrange("p (c t) -> p c t", t=128)
        P.op("dve", lambda e: e.memset(Mprev[:, 0:1], 0.0), r=(), w=["Mprev0"])
        P.op("dve", lambda e: e.tensor_copy(Mprev[:, 1:16], M3[:, 0:15, 127]), r=["Mrow"], w=["Mprev"])
        for c in range(16):
            P.op("dve", lambda e, c=c: e.tensor_scalar(WI3[:, c, :], M3[:, c, :], Mprev[:, c:c + 1], None, ALU.subtract),
                 r=["Mrow", "Mprev", "Mprev0"], w=["WIrow"])
            P.op("dve", lambda e, c=c: e.tensor_scalar(WS3[:, c, :], G3[:, c, :], M3[:, c, 127:128], None, ALU.subtract),
                 r=["Mrow", "Grow"], w=["WSrow"])
        P.op("act", lambda e: e.activation(out=WIrow[:], in_=WIrow[:], func=AF.Exp, scale=-1.0), r=["WIrow"], w=["WIrow"])
        P.op("act", lambda e: e.activation(out=WSrow[:], in_=WSrow[:], func=AF.Exp), r=["WSrow"], w=["WSrow"])
        P.op("dve", lambda e: e.tensor_tensor(EMRrow[:], CS[:], Mrow[:], ALU.subtract), r=["CS", "Mrow"], w=["EMRrow"])
        P.op("act", lambda e: e.activation(out=EMRrow[:], in_=EMRrow[:], func=AF.Exp), r=["EMRrow"], w=["EMRrow"])
        P.op("dve", lambda e: e.tensor_tensor(DEC[:], Mprev[:], M3[:, :, 127], ALU.subtract), r=["Mprev", "Mprev0", "Mrow"], w=["DEC"])
        P.op("act", lambda e: e.activation(out=DEC[:], in_=DEC[:], func=AF.Exp), r=["DEC"], w=["DEC"])
        pT = psT[:, 0:64].rearrange("p (c h) -> p c h", h=4)
        pT2 = psT2[:, 0:64].rearrange("p (c h) -> p c h", h=4)
        for blk in range(16):
            P.op("pe", lambda e, blk=blk: e.transpose(pT[:, blk, :], Grow[0:4, blk * 128:(blk + 1) * 128], ident[0:4, 0:4]),
                 r=["Grow"], w=["psT"])
            P.op("pe", lambda e, blk=blk: e.transpose(pT2[:, blk, :], WSrow[0:4, blk * 128:(blk + 1) * 128], ident[0:4, 0:4]),
                 r=["WSrow"], w=["psT2"])
        P.op("dve", lambda e: e.tensor_copy(GT[:], pT), r=["psT"], w=["GT"])
        P.op("dve", lambda e: e.tensor_copy(WST[:], pT2), r=["psT2"], w=["WST"])
        P.run()
        _tap(nc, taps, f"Mrow{b}", Mrow[:], [4, SEQ]); _tap(nc, taps, f"WIrow{b}", WIrow[:], [4, SEQ])
        _tap(nc, taps, f"EMRrow{b}", EMRrow[:], [4, SEQ]); _tap(nc, taps, f"GT{b}", GT[:], [128, 16, 4])
        _tap(nc, taps, f"WST{b}", WST[:], [128, 16, 4]); _tap(nc, taps, f"DEC{b}", DEC[:], [4, 16])

        import os
        ML_STAGE = float(os.environ.get("ML_STAGE", "9"))
        for h in range(4 if ML_STAGE > 0 else 0):
            P = Prog(nc, f"mlh{b}_{h}")
            try:
              _ml_head(P, nc, D, b, h, hT, ybT, scratch, NUMv, XP, win, mlg, ones, EPS_AP, Mrow, WIrow, EMRrow, GT, WST, DEC,
                       sel4, maskLE, identb, cwT, cbT, ML_STAGE)
            except _StopStage:
              pass
            P.run()


class _StopStage(Exception):
    pass


def _ml_head(P, nc, D, b, h, hT, ybT, scratch, NUMv, XP, win, mlg, ones, EPS_AP, Mrow, WIrow, EMRrow, GT, WST, DEC,
             sel4, maskLE, identb, cwT, cbT, ML_STAGE):
    def chk(k):
        if ML_STAGE < k:
            raise _StopStage()
    if True:
        if True:
            Wb = [P.sb([128, KC, 256], BF16, name=f"Wb{i}") for i in range(2)]
            ACC = [NUMv[:, 0, :], NUMv[:, 1, :]]
            qTh = P.sb([128, 2, SEQ], BF16, name="qTh"); kTh = P.sb([128, 2, SEQ], BF16, name="kTh")
            QW = P.sb([128, 2, SEQ], BF16, name="QW")
            KW = P.sb([128, 16, 256], BF16, name="KW"); VA = P.sb([128, 16, 258], BF16, name="VA")
            WT = P.sb([128, 16, 128], BF16, name="WT")
            DEN = P.sb([1, SEQ], name="DEN")
            C = P.sb([128, 2, 258], name="C"); Cbf = P.sb([128, 2, 258], BF16, name="Cbf")
            DECsb = P.sb([128, 16], name="DECsb")
            PT = [P.sb([128, 128], BF16, name=f"PT{i}") for i in range(2)]
            B = [P.ps(name=f"B{i}") for i in range(8)]
            Bb = [bk[:].bitcast(BF16) for bk in B]
            nb_ = [0]

            def nbank():
                nb_[0] += 1
                return nb_[0] % 8
            wslot = [0]

            def loadW(col):
                sl = wslot[0] % 2
                wslot[0] += 1
                P.dma("pool", Wb[sl][:], win[:, :, col + h * 256: col + (h + 1) * 256], r=(), w=[f"Wb{sl}"], chan=f"Wb{sl}")
                return sl
            XPb = P.sb([128, SEQ + 3], name="XPb")
            XPs = [XP, XPb[:]]
            P.op("dve", lambda e: e.memset(XP[:, 0:3], 0.0), r=(), w=["XP0"])
            P.op("dve", lambda e: e.memset(XPb[:, 0:3], 0.0), r=(), w=["XP0"])
            P.op("dve", lambda e: e.memset(VA[:, :, 256:257], 1.0), r=(), w=["VA1"])
            for wi, (col, dstT, cbase) in enumerate([(COL_MQ, qTh, 0), (COL_MK, kTh, 8)]):
                sl = loadW(col)
                chk(1.1)
                for dch in range(2):
                    cidx = cbase + h * 2 + dch
                    xi = (wi * 2 + dch) % 2
                    XPc = XPs[xi]
                    for tt in range(4):
                        tok = slice(tt * 512, (tt + 1) * 512)
                        bk = nbank()
                        for kc in range(KC):
                            P.mm(B[bk][:], Wb[sl][:, kc, dch * 128:(dch + 1) * 128], hT[:, kc, tok], kc == 0, kc == KC - 1,
                                 r=[f"Wb{sl}"], w=[f"B{bk}"])
                        P.op("act", lambda e, bk=bk, tt=tt, XPc=XPc: e.activation(out=XPc[:, 3 + tt * 512: 3 + (tt + 1) * 512], in_=B[bk][:], func=AF.Copy),
                             r=[f"B{bk}"], w=[f"XP{xi}.{tt}"])
                    chk(1.2)
                    a = ACC[dch]
                    xk = [f"XP{xi}.{t_}" for t_ in range(4)] + ["XP0"]
                    P.op("dve", lambda e, a=a, cidx=cidx, XPc=XPc: e.tensor_scalar(a, XPc[:, 0:SEQ], cwT[:, cidx, 0:1], cbT[:, cidx:cidx + 1], ALU.mult, ALU.add),
                         r=xk, w=[f"N{dch}"])
                    for tap in range(1, 4):
                        P.op("dve", lambda e, a=a, cidx=cidx, tap=tap, XPc=XPc: e.scalar_tensor_tensor(
                            a, XPc[:, tap:tap + SEQ], cwT[:, cidx, tap:tap + 1], a, ALU.mult, ALU.add),
                            r=xk + [f"N{dch}"], w=[f"N{dch}"])
                    chk(1.3)
                    if wi == 0:
                        P.op("act", lambda e, a=a, o_=qTh[:, dch, :]: e.activation(out=o_, in_=a, func=AF.Silu), r=[f"N{dch}"], w=[f"qTh{dch}"])
                    else:
                        P.op("act", lambda e, a=a: e.activation(out=a, in_=a, func=AF.Silu), r=[f"N{dch}"], w=[f"N{dch}"])
                        P.op("dve", lambda e, a=a, o_=kTh[:, dch, :]: e.tensor_scalar(o_, a, 1.0 / 16.0, None, ALU.mult),
                             r=[f"N{dch}"], w=[f"kTh{dch}"])
            chk(2)
            sl = loadW(COL_MV)
            for sb2 in range(8):
                bk = nbank()
                pv = B[bk][:].rearrange("p (j c) -> p j c", c=256)
                for j in range(2):
                    blk = sb2 * 2 + j
                    for kc in range(KC):
                        P.mm(pv[:, j, :], hT[:, kc, blk * 128:(blk + 1) * 128], Wb[sl][:, kc, :], kc == 0, kc == KC - 1,
                             r=[f"Wb{sl}"], w=[f"B{bk}"])
                P.op("act", lambda e, pv=pv, o_=VA[:, sb2 * 2:sb2 * 2 + 2, 0:256]: e.activation(out=o_, in_=pv, func=AF.Copy),
                     r=[f"B{bk}"], w=[f"VA.{sb2}"])
            slo = loadW(COL_MO)
            chk(3)
            TD = ACC[1]
            TD3 = TD.rearrange("p (c t) -> p c t", t=128)
            for tt in range(4):
                tok = slice(tt * 512, (tt + 1) * 512)
                bk = nbank()
                P.mm(B[bk][:], sel4[0:4, h, :], Mrow[0:4, tok], True, True, r=(), w=[f"B{bk}"])
                for j in range(4):
                    c = tt * 4 + j
                    P.op("dve", lambda e, bk=bk, j=j, c=c: e.tensor_scalar(
                        TD3[:, c, :], B[bk][:, j * 128:(j + 1) * 128], GT[:, c, h:h + 1], 0.0, ALU.subtract, ALU.max),
                        r=[f"B{bk}", "kTh1"], w=["N1"])
                bk = nbank()
                P.mm(B[bk][:], sel4[0:4, h, :], WIrow[0:4, tok], True, True, r=(), w=[f"B{bk}"])
                for dch in range(2):
                    P.op("dve", lambda e, bk=bk, o_=QW[:, dch, tok], i_=qTh[:, dch, tok]: e.tensor_tensor(o_, i_, B[bk][:], ALU.mult),
                         r=[f"B{bk}", f"qTh{dch}"], w=[f"QW{dch}"])
            P.op("act", lambda e: e.activation(out=WT[:].rearrange("p c t -> p (c t)"), in_=TD, func=AF.Exp, scale=-1.0), r=["N1"], w=["WT"])
            for c in range(16):
                P.op("pool", lambda e, c=c: e.tensor_tensor(WT[:, c, :], WT[:, c, :], maskLE[:], ALU.mult), r=["WT"], w=["WT"])
            bk = nbank()
            P.mm(B[bk][:, 0:16], sel4[0:4, h, :], DEC[0:4, :], True, True, r=(), w=[f"B{bk}"])
            P.op("act", lambda e, bk=bk: e.activation(out=DECsb[:], in_=B[bk][:, 0:16], func=AF.Copy), r=[f"B{bk}"], w=["DECsb"])
            for sbk in range(16):
                bk = nbank()
                for dch in range(2):
                    P.op("pe", lambda e, bk=bk, dch=dch, sbk=sbk: e.transpose(
                        Bb[bk][:, dch * 128:(dch + 1) * 128], kTh[:, dch, sbk * 128:(sbk + 1) * 128], identb[:]),
                        r=[f"kTh{dch}"], w=[f"B{bk}"])
                P.op("dve", lambda e, bk=bk, sbk=sbk: e.tensor_scalar(KW[:, sbk, :], Bb[bk][:, 0:256], WST[:, sbk, h:h + 1], None, ALU.mult),
                     r=[f"B{bk}"], w=[f"KW.{sbk}"])
            chk(4)
            bS = [0, 1]; bN = [2, 3]; bD = 4; bC = [5, 6]
            def s_pt(c):
                blk = slice(c * 128, (c + 1) * 128)
                s = c % 2
                for dch in range(2):
                    P.mm(B[bS[s]][:, 0:128], kTh[:, dch, blk], qTh[:, dch, blk], dch == 0, dch == 1,
                         r=[f"kTh{dch}", f"qTh{dch}"], w=[f"B{bS[s]}"])
                P.op("dve", lambda e, s=s, c=c: e.tensor_tensor(PT[s][:], B[bS[s]][:, 0:128], WT[:, c, :], ALU.mult),
                     r=[f"B{bS[s]}", "WT"], w=[f"PT{s}"])
            s_pt(0)
            for c in range(16):
                blk = slice(c * 128, (c + 1) * 128)
                cs4 = slice((c % 4) * 128, (c % 4 + 1) * 128)
                s = c % 2
                if c + 1 < 16:
                    s_pt(c + 1)
                for ech in range(3):
                    if ech < 2:
                        outp = B[bN[ech]][:, cs4]; wk = f"B{bN[ech]}"
                        cs_ = slice(ech * 128, (ech + 1) * 128)
                    else:
                        outp = B[bD][0:1, cs4]; wk = f"B{bD}"
                        cs_ = slice(256, 257)
                    if c > 0:
                        for dch in range(2):
                            P.mm(outp, Cbf[:, dch, cs_], QW[:, dch, blk], dch == 0, False, r=[f"Cbf{dch}", f"QW{dch}"], w=[wk])
                    P.mm(outp, VA[:, c, cs_], PT[s][:], c == 0, True, r=[f"VA.{c // 2}", "VA1", f"PT{s}"], w=[wk])
                if c % 4 == 3:
                    t4 = slice((c // 4) * 512, (c // 4 + 1) * 512)
                    for ech in range(2):
                        P.op("act", lambda e, ech=ech, t4=t4: e.activation(out=NUMv[:, ech, t4], in_=B[bN[ech]][:], func=AF.Copy),
                             r=[f"B{bN[ech]}"], w=[f"NUM{ech}.{c // 4}"])
                    P.op("dve", lambda e, t4=t4: e.tensor_copy(DEN[0:1, t4], B[bD][0:1, :]), r=[f"B{bD}"], w=[f"DEN.{c // 4}"])
                if c < 15:
                    for dch in range(2):
                        P.mm(B[bC[dch]][:, 0:257], KW[:, c, dch * 128:(dch + 1) * 128], VA[:, c, 0:257], True, True,
                             r=[f"KW.{c}", f"VA.{c // 2}", "VA1"], w=[f"B{bC[dch]}"])
                        if c == 0:
                            P.op("dve", lambda e, dch=dch: e.tensor_copy(C[:, dch, 0:257], B[bC[dch]][:, 0:257]),
                                 r=[f"B{bC[dch]}"], w=[f"C{dch}"])
                        else:
                            P.op("dve", lambda e, dch=dch, c=c: e.scalar_tensor_tensor(
                                C[:, dch, 0:257], C[:, dch, 0:257], DECsb[:, c:c + 1], B[bC[dch]][:, 0:257], ALU.mult, ALU.add),
                                r=[f"B{bC[dch]}", f"C{dch}", "DECsb"], w=[f"C{dch}"])
                        P.op("act", lambda e, dch=dch: e.activation(out=Cbf[:, dch, 0:257], in_=C[:, dch, 0:257], func=AF.Copy),
                             r=[f"C{dch}"], w=[f"Cbf{dch}"])
            chk(5)
            T = [scratch[:, 4096 + i * 512: 4096 + (i + 1) * 512] for i in range(4)] + \
                [scratch[:, 6148 + i * 512: 6148 + (i + 1) * 512] for i in range(3)]
            T.append(P.sb([128, 512], name="T7")[:]); T.append(P.sb([128, 512], name="T8")[:])
            for tt in range(4):
                tok = slice(tt * 512, (tt + 1) * 512)
                bA = nbank()
                P.mm(B[bA][:], ones[0:1, :], DEN[0:1, tok], True, True, r=[f"DEN.{tt}"], w=[f"B{bA}"])
                bB = nbank()
                P.mm(B[bB][:], sel4[0:4, h, :], EMRrow[0:4, tok], True, True, r=(), w=[f"B{bB}"])
                P.op("act", lambda e, bB=bB: e.activation(out=T[0], in_=B[bB][:], func=AF.Copy), r=[f"B{bB}"], w=["T0"])
                P.op("act", lambda e, bA=bA: e.activation(out=T[1], in_=B[bA][:], func=AF.Abs), r=[f"B{bA}"], w=["T1"])
                P.op("dve", lambda e: e.tensor_tensor(T[1], T[1], T[0], ALU.max), r=["T1", "T0"], w=["T1"])
                P.op("act", lambda e: e.activation(out=T[1], in_=T[1], func=AF.Ln), r=["T1"], w=["T1"])
                P.op("act", lambda e: e.activation(out=T[1], in_=T[1], func=AF.Exp, scale=-1.0), r=["T1"], w=["T1"])
                for ech in range(2):
                    P.op("dve", lambda e, ech=ech, tok=tok: e.tensor_tensor(NUMv[:, ech, tok], NUMv[:, ech, tok], T[1], ALU.mult),
                         r=["T1", f"NUM{ech}.{tt}"], w=[f"NUM{ech}.{tt}"])
                    P.op("act", lambda e, ech=ech, tok=tok: e.activation(out=T[2 + ech], in_=NUMv[:, ech, tok], func=AF.Square),
                         r=[f"NUM{ech}.{tt}"], w=[f"T{2 + ech}"])
                bC2 = nbank()
                for ech in range(2):
                    P.mm(B[bC2][:], ones[:], T[2 + ech], ech == 0, ech == 1, r=[f"T{2 + ech}"], w=[f"B{bC2}"])
                P.op("act", lambda e, bC2=bC2: e.activation(out=T[4], in_=B[bC2][:], func=AF.Ln, bias=EPS_AP[:], scale=1.0 / 256.0),
                     r=[f"B{bC2}"], w=["T4"])
                P.op("act", lambda e: e.activation(out=T[4], in_=T[4], func=AF.Exp, scale=-0.5), r=["T4"], w=["T4"])
                for ech in range(2):
                    bo = nbank()
                    for kc in range(KC):
                        P.mm(B[bo][:], Wb[slo][:, kc, ech * 128:(ech + 1) * 128], hT[:, kc, tok], kc == 0, kc == KC - 1,
                             r=[f"Wb{slo}"], w=[f"B{bo}"])
                    P.op("act", lambda e, bo=bo, ech=ech: e.activation(out=T[5 + ech], in_=B[bo][:], func=AF.Sigmoid), r=[f"B{bo}"], w=[f"T{5 + ech}"])
                    P.op("dve", lambda e, ech=ech, tok=tok: e.scalar_tensor_tensor(
                        T[7 + ech], NUMv[:, ech, tok], mlg[:, h * 2 + ech: h * 2 + ech + 1], T[4], ALU.mult, ALU.mult),
                        r=[f"NUM{ech}.{tt}", "T4"], w=[f"T{7 + ech}"])
                    P.op("pool", lambda e, ech=ech, o_=ybT[:, h * 2 + ech, tok]: e.tensor_tensor(o_, T[7 + ech], T[5 + ech], ALU.mult),
                         r=[f"T{7 + ech}", f"T{5 + ech}"], w=[f"yb.{tt}"])


def merge_phase(nc, D, b, hT, yaT, ybT, mergedT, wview):
    P = Prog(nc, f"mg{b}")
    win = wview(D["w_in"]); wa = wview(D["w_a"]); wb = wview(D["w_b"])
    W = {nm: [P.sb([128, KC, 128], BF16, name=f"{nm}{i}") for i in range(2)] for nm in ("wa", "wb", "wga", "wgb")}
    sa = [P.sb([128, 512], name=f"sa{i}") for i in range(2)]
    sg = [P.sb([128, 512], name=f"sg{i}") for i in range(2)]
    B = [P.ps(name=f"B{i}") for i in range(8)]
    it = 0
    for fo in range(8):
        s = fo % 2
        fsl = slice(fo * 128, (fo + 1) * 128)
        P.dma("pool", W["wa"][s][:], wa[:, :, fsl], r=(), w=[f"wa{s}"], chan=f"mwa{s}")
        P.dma("pool", W["wb"][s][:], wb[:, :, fsl], r=(), w=[f"wb{s}"], chan=f"mwb{s}")
        P.dma("pool", W["wga"][s][:], win[:, :, COL_GA + fo * 128: COL_GA + (fo + 1) * 128], r=(), w=[f"wga{s}"], chan=f"mwga{s}")
        P.dma("pool", W["wgb"][s][:], win[:, :, COL_GB + fo * 128: COL_GB + (fo + 1) * 128], r=(), w=[f"wgb{s}"], chan=f"mwgb{s}")
        for tt in range(4):
            tok = slice(tt * 512, (tt + 1) * 512)
            g = it % 2
            it += 1
            bA, bB, bGA, bGB = 4 * g, 4 * g + 1, 4 * g + 2, 4 * g + 3
            for (bk, nm, src) in [(bA, "wa", yaT), (bB, "wb", ybT), (bGA, "wga", hT), (bGB, "wgb", hT)]:
                for kc in range(KC):
                    P.mm(B[bk][:], W[nm][s][:, kc, :], src[:, kc, tok], kc == 0, kc == KC - 1, r=[f"{nm}{s}"], w=[f"B{bk}"])
            P.op("act", lambda e, g=g, bGA=bGA: e.activation(out=sa[g][:], in_=B[bGA][:], func=AF.Sigmoid), r=[f"B{bGA}"], w=[f"sa{g}"])
            P.op("act", lambda e, g=g, bGB=bGB: e.activation(out=sg[g][:], in_=B[bGB][:], func=AF.Sigmoid), r=[f"B{bGB}"], w=[f"sg{g}"])
            P.op("dve", lambda e, g=g, bA=bA: e.tensor_tensor(sa[g][:], sa[g][:], B[bA][:], ALU.mult), r=[f"sa{g}", f"B{bA}"], w=[f"sa{g}"])
            P.op("dve", lambda e, g=g, bB=bB: e.tensor_tensor(sg[g][:], sg[g][:], B[bB][:], ALU.mult), r=[f"sg{g}", f"B{bB}"], w=[f"sg{g}"])
            P.op("pool", lambda e, g=g, o_=mergedT[:, fo, tok]: e.tensor_tensor(o_, sa[g][:], sg[g][:], ALU.add),
                 r=[f"sa{g}", f"sg{g}"], w=[f"mg.{tt}"])
    P.run()


def outproj_phase(nc, D, b, mergedT, x1T, G1, wview):
    P = Prog(nc, f"op{b}")
    wo = P.sb([128, KC, 1024], BF16, name="wo")
    P.dma("pool", wo[:], wview(D["w_out"]), r=(), w=["wo"], chan="ldwo")
    xt = [P.sb([128, KC, 512], name=f"xt{i}") for i in range(2)]
    B = [P.ps(name=f"B{i}") for i in range(4)]
    it = 0
    for tt in range(4):
        s = tt % 2
        tok = slice(tt * 512, (tt + 1) * 512)
        P.dma("sp", xt[s][:], D["xT"][:, :, b * SEQ + tt * 512: b * SEQ + (tt + 1) * 512], r=(), w=[f"xt{s}"], chan=f"oxt{s}")
        for fo in range(8):
            bk = it % 4
            it += 1
            for kc in range(KC):
                P.mm(B[bk][:], wo[:, kc, fo * 128:(fo + 1) * 128], mergedT[:, kc, tok], kc == 0, kc == KC - 1, r=["wo"], w=[f"B{bk}"])
            P.op("dve", lambda e, bk=bk, fo=fo, s=s, tok=tok: e.scalar_tensor_tensor(
                x1T[:, fo, tok], B[bk][:], G1(fo, b), xt[s][:, fo, :], ALU.mult, ALU.add),
                r=[f"B{bk}", f"xt{s}"], w=[f"x1.{tt}"])
    P.run()


def norm2_router_phase(nc, D, b, x1T, h2T, gT, A2, SH2, G2, ones, ident, EPS_AP, wview):
    P = Prog(nc, f"n2_{b}")
    rw = P.sb([128, KC, NE], name="rw"); rb = P.sb([128, NE], name="rb"); b2n = P.sb([NE, 1024], name="b2n")
    gTb = gT
    gT = P.sb([NE, SEQ], name="gTf")
    P.op("pool", lambda e: e.memset(gTb[:], 0.0), r=(), w=["gTbz"])
    P.dma("sp", rw[:], wview(D["router_w"]), r=(), w=["rw"], chan="ldrw")
    P.dma("sp", rb[:], D["rb_rep"], r=(), w=["rb"], chan="ldrb")
    P.dma("sp", b2n[:], D["b2"], r=(), w=["b2n"], chan="ldb2")
    sq = [P.sb([128, KC, 512], name=f"sq{i}") for i in range(2)]
    h2f = [P.sb([128, KC, 512], name=f"h2f{i}") for i in range(2)]
    rt = [P.sb([128, 512], name=f"rt{i}") for i in range(2)]
    tmp = [P.sb([128, 512], name=f"tmp{i}") for i in range(4)]
    lg = P.sb([128, 4, NE], name="lg"); m8 = P.sb([128, 4, 8], name="m8"); negm = P.sb([128, 4], name="negm")
    ex = P.sb([128, 4, NE], name="ex"); msk = P.sb([128, 4, NE], name="msk"); ssum = P.sb([128, 4], name="ssum")
    gates = P.sb([128, 4, NE], name="gates")
    pss = [P.ps(name=f"ss{i}") for i in range(2)]
    psL = [P.ps(name=f"psL{i}") for i in range(2)]
    psG = [P.ps(name=f"psG{i}") for i in range(2)]
    psB = [P.ps(name=f"psB{i}") for i in range(2)]
    for tt in range(4):
        s = tt % 2
        tok = slice(tt * 512, (tt + 1) * 512)
        xk = [f"x1.{tt}"]
        P.op("act", lambda e, s=s, tok=tok: e.activation(out=sq[s][:], in_=x1T[:, :, tok], func=AF.Square), r=xk, w=[f"sq{s}"])
        for kc in range(KC):
            P.mm(pss[s][:], ones[:], sq[s][:, kc, :], kc == 0, kc == KC - 1, r=[f"sq{s}"], w=[f"ss{s}"])
        P.op("act", lambda e, s=s: e.activation(out=rt[s][:], in_=pss[s][:], func=AF.Ln, bias=EPS_AP[:], scale=1.0 / D_MODEL),
             r=[f"ss{s}"], w=[f"rt{s}"])
        P.op("act", lambda e, s=s: e.activation(out=rt[s][:], in_=rt[s][:], func=AF.Exp, scale=-0.5), r=[f"rt{s}"], w=[f"rt{s}"])
        for kc in range(KC):
            q = kc % 4
            P.op("dve", lambda e, kc=kc, q=q, s=s, tok=tok: e.scalar_tensor_tensor(
                tmp[q][:], x1T[:, kc, tok], A2[:, kc, b:b + 1], rt[s][:], ALU.mult, ALU.mult),
                r=xk + [f"rt{s}"], w=[f"tmp{q}"])
            P.op("act", lambda e, kc=kc, q=q, s=s: e.activation(
                out=h2f[s][:, kc, :], in_=tmp[q][:], func=AF.Identity, bias=SH2(kc, b), scale=1.0),
                r=[f"tmp{q}"], w=[f"h2f{s}.{kc}"])
            P.op("pool", lambda e, kc=kc, s=s, tok=tok: e.tensor_copy(h2T[:, kc, tok], h2f[s][:, kc, :]),
                 r=[f"h2f{s}.{kc}"], w=[f"h2.{tt}"])
        pl4 = psL[s][:, 0:4 * NE].rearrange("p (j n) -> p j n", n=NE)
        for blk in range(4):
            for kc in range(KC):
                P.mm(pl4[:, blk, :], h2f[s][:, kc, blk * 128:(blk + 1) * 128], rw[:, kc, :], kc == 0, kc == KC - 1,
                     r=[f"h2f{s}.{kc}", "rw"], w=[f"psL{s}"])
        for blk in range(4):
            P.op("dve", lambda e, blk=blk, pl4=pl4: e.tensor_tensor(lg[:, blk, :], pl4[:, blk, :], rb[:], ALU.add),
                 r=[f"psL{s}", "rb"], w=[f"lg{blk}"])
            P.op("dve", lambda e, blk=blk: e.max(m8[:, blk, :], lg[:, blk, :]), r=[f"lg{blk}"], w=[f"m8{blk}"])
            P.op("dve", lambda e, blk=blk: e.tensor_scalar(negm[:, blk:blk + 1], m8[:, blk, 0:1], -1.0, None, ALU.mult),
                 r=[f"m8{blk}"], w=[f"negm{blk}"])
            P.op("act", lambda e, blk=blk: e.activation(out=ex[:, blk, :], in_=lg[:, blk, :], func=AF.Exp, bias=negm[:, blk:blk + 1], scale=1.0),
                 r=[f"lg{blk}", f"negm{blk}"], w=[f"ex{blk}"])
            P.op("dve", lambda e, blk=blk: e.tensor_scalar(msk[:, blk, :], lg[:, blk, :], m8[:, blk, 3:4], None, ALU.is_ge),
                 r=[f"lg{blk}", f"m8{blk}"], w=[f"msk{blk}"])
            P.op("dve", lambda e, blk=blk: e.tensor_tensor(ex[:, blk, :], ex[:, blk, :], msk[:, blk, :], ALU.mult),
                 r=[f"ex{blk}", f"msk{blk}"], w=[f"ex{blk}"])
            P.op("dve", lambda e, blk=blk: e.reduce_sum(ssum[:, blk:blk + 1], ex[:, blk, :], mybir.AxisListType.X),
                 r=[f"ex{blk}"], w=[f"ssum{blk}"])
            P.op("dve", lambda e, blk=blk: e.reciprocal(ssum[:, blk:blk + 1], ssum[:, blk:blk + 1]), r=[f"ssum{blk}"], w=[f"ssum{blk}"])
            P.op("dve", lambda e, blk=blk: e.tensor_scalar(gates[:, blk, :], ex[:, blk, :], ssum[:, blk:blk + 1], None, ALU.mult),
                 r=[f"ex{blk}", f"ssum{blk}"], w=[f"gates{blk}"])
            P.op("pe", lambda e, blk=blk, s=s: e.transpose(psG[s][0:NE, blk * 128:(blk + 1) * 128], gates[:, blk, :], ident[:]),
                 r=[f"gates{blk}"], w=[f"psG{s}"])
        P.op("act", lambda e, s=s, tok=tok: e.activation(out=gT[0:NE, tok], in_=psG[s][0:NE, :], func=AF.Copy), r=[f"psG{s}"], w=[f"gT.{tt}"])
        P.op("dve", lambda e, tok=tok: e.tensor_copy(gTb[0:NE, tok], gT[0:NE, tok]), r=[f"gT.{tt}", "gTbz"], w=[f"gTb.{tt}"])
        for fo in range(8):
            bb = fo % 2
            P.mm(psB[bb][:], b2n[0:NE, fo * 128:(fo + 1) * 128], gT[0:NE, tok], True, True, r=["b2n", f"gT.{tt}"], w=[f"psB{bb}"])
            P.op("dve", lambda e, bb=bb, fo=fo, tok=tok: e.scalar_tensor_tensor(
                x1T[:, fo, tok], psB[bb][:], G2(fo, b), x1T[:, fo, tok], ALU.mult, ALU.add),
                r=[f"psB{bb}", f"x1.{tt}"], w=[f"x1.{tt}"])
    P.run()


def moe_phase(nc, D, b, h2T, x1T, gT, G2):
    P = Prog(nc, f"moe{b}")
    w1 = D["w1"].rearrange("e (kc p) n -> e p kc n", p=128)
    w2 = D["w2"].rearrange("e (kc p) n -> e p kc n", p=128)
    b1 = P.sb([128, NE, 16], name="b1"); pid = P.sb([128, 128], name="pid")
    P.dma("sp", b1[:], D["b1T"], r=(), w=["b1"], chan="ldb1")
    P.dma("sp", pid[:], D["c_pid"], r=(), w=["pid"], chan="ldpid")
    P.op("dve", lambda e_: e_.tensor_scalar(b1[:, :, 8:16], b1[:, :, 8:16], 1.0, None, ALU.add), r=["b1"], w=["b1"])
    sel = [P.sb([128, 128], BF16, name=f"sel{i}") for i in range(2)]
    Wg = [P.sb([128, KC, 512], BF16, name=f"Wg{i}") for i in range(3)]
    Wl = [P.sb([128, KC, 512], BF16, name=f"Wl{i}") for i in range(3)]
    W2 = [P.sb([128, 4, 1024], BF16, name=f"W2{i}") for i in range(3)]
    actT = [P.sb([128, 4, 512], BF16, name=f"act{i}") for i in range(3)]
    gsb = [P.sb([128, 512], BF16, name=f"gsb{i}") for i in range(2)]
    Ta = [P.sb([128, 512], name=f"Ta{i}") for i in range(2)]
    Tb = [P.sb([128, 512], name=f"Tb{i}") for i in range(2)]
    Tc = [P.sb([128, 512], name=f"Tc{i}") for i in range(2)]
    pg = [P.ps(name=f"pg{i}") for i in range(2)]
    pl = [P.ps(name=f"pl{i}") for i in range(2)]
    po = [P.ps(name=f"po{i}") for i in range(3)]
    pgt = [P.ps(name=f"pgt{i}") for i in range(1)]
    units = [(e, hf) for e in range(NE) for hf in range(2)]

    def load(u):
        e, hf = units[u]
        r = u % 3
        P.dma("pool", Wg[r][:], w1[e][:, :, hf * 512:(hf + 1) * 512], r=(), w=[f"Wg{r}"], chan=f"Wg{r}")
        P.dma("pool", Wl[r][:], w1[e][:, :, 1024 + hf * 512: 1024 + (hf + 1) * 512], r=(), w=[f"Wl{r}"], chan=f"Wl{r}")
        P.dma("pool", W2[r][:], w2[e][:, hf * 4:(hf + 1) * 4, :], r=(), w=[f"W2{r}"], chan=f"W2{r}")
    load(0)
    steps = [(u, tt) for u in range(len(units)) for tt in range(4)]

    def SA_pre(t):
        u, tt = steps[t]
        e, hf = units[u]
        if tt == 0:
            if u + 1 < len(units):
                load(u + 1)
            if hf == 0:
                P.op("dve", lambda e_, e=e: e_.tensor_scalar(sel[e % 2][:], pid[:], float(e), None, ALU.is_equal), r=["pid"], w=[f"sel{e % 2}"])
        tok = slice(tt * 512, (tt + 1) * 512)
        ga = t % 2
        P.mm(pgt[0][:], sel[e % 2][:], gT[:, tok], True, True, r=[f"sel{e % 2}"], w=["pgt0"])
        P.op("act", lambda e_, ga=ga: e_.activation(out=gsb[ga][:], in_=pgt[0][:], func=AF.Copy, scale=1.0 / 1.702), r=["pgt0"], w=[f"gsb{ga}"])

    def SA_head(t, j):
        u, tt = steps[t]
        e, hf = units[u]
        r = u % 3
        tok = slice(tt * 512, (tt + 1) * 512)
        jj = hf * 4 + j
        z = j % 2
        for kc in range(KC):
            P.mm(pg[z][:], Wg[r][:, kc, j * 128:(j + 1) * 128], h2T[:, kc, tok], kc == 0, kc == KC - 1, r=[f"Wg{r}"], w=[f"pg{z}"])
        for kc in range(KC):
            P.mm(pl[z][:], Wl[r][:, kc, j * 128:(j + 1) * 128], h2T[:, kc, tok], kc == 0, kc == KC - 1, r=[f"Wl{r}"], w=[f"pl{z}"])
        P.op("dve", lambda e_, z=z, e=e, jj=jj: e_.tensor_scalar(Ta[z][:], pg[z][:], b1[:, e, jj:jj + 1], 7.0, ALU.add, ALU.min),
             r=[f"pg{z}", "b1"], w=[f"Ta{z}"])
        P.op("act", lambda e_, z=z: e_.activation(out=Tb[z][:], in_=Ta[z][:], func=AF.Silu, scale=1.702), r=[f"Ta{z}"], w=[f"Tb{z}"])
        P.op("dve", lambda e_, z=z, e=e, jj=jj: e_.tensor_scalar(Tc[z][:], pl[z][:], b1[:, e, 8 + jj:9 + jj], -6.0, ALU.add, ALU.max),
             r=[f"pl{z}", "b1"], w=[f"Tc{z}"])

    def SA_tail(t, j):
        z = j % 2
        a = t % 3
        ga = t % 2
        P.op("dve", lambda e_, z=z: e_.scalar_tensor_tensor(Tc[z][:], Tc[z][:], 8.0, Tb[z][:], ALU.min, ALU.mult),
             r=[f"Tb{z}", f"Tc{z}"], w=[f"Tc{z}"])
        P.op("dve", lambda e_, z=z, a=a, j=j, ga=ga: e_.tensor_tensor(actT[a][:, j, :], Tc[z][:], gsb[ga][:], ALU.mult),
             r=[f"Tc{z}", f"gsb{ga}"], w=[f"act{a}.{j}"])

    def SB(t):
        u, tt = steps[t]
        r = u % 3
        tok = slice(tt * 512, (tt + 1) * 512)
        a = t % 3
        for fo in range(8):
            y = (t * 8 + fo) % 3
            for j in range(4):
                P.mm(po[y][:], W2[r][:, j, fo * 128:(fo + 1) * 128], actT[a][:, j, :], j == 0, j == 3,
                     r=[f"W2{r}", f"act{a}.{j}"], w=[f"po{y}"])
            P.op("dve", lambda e_, y=y, fo=fo, tok=tok: e_.scalar_tensor_tensor(
                x1T[:, fo, tok], po[y][:], G2(fo, b), x1T[:, fo, tok], ALU.mult, ALU.add),
                r=[f"po{y}", f"x2.{tt}.{fo}"], w=[f"x2.{tt}.{fo}"])

    n_st = len(steps)
    for t in range(n_st + 2):
        act_ = t < n_st
        if act_:
            SA_pre(t)
            SA_head(t, 0)
            SA_head(t, 1)
            SA_tail(t, 0)
        if t >= 2:
            SB(t - 2)
        if act_:
            SA_head(t, 2)
            SA_tail(t, 1)
            SA_head(t, 3)
            SA_tail(t, 2)
            SA_tail(t, 3)
    P.run()


def final_phase(nc, D, b, x2T, fg, ones, EPS_AP, outT):
    P = Prog(nc, f"fin{b}")
    sq = [P.sb([128, KC, 512], name=f"sq{i}") for i in range(2)]
    ot = [P.sb([128, KC, 512], name=f"ot{i}") for i in range(2)]
    rt = [P.sb([128, 512], name=f"rt{i}") for i in range(2)]
    pss = [P.ps(name=f"ss{i}") for i in range(2)]
    for tt in range(4):
        s = tt % 2
        tok = slice(tt * 512, (tt + 1) * 512)
        P.op("act", lambda e, s=s, tok=tok: e.activation(out=sq[s][:], in_=x2T[:, :, tok], func=AF.Square), r=(), w=[f"sq{s}"])
        for kc in range(KC):
            P.mm(pss[s][:], ones[:], sq[s][:, kc, :], kc == 0, kc == KC - 1, r=[f"sq{s}"], w=[f"ss{s}"])
        P.op("act", lambda e, s=s: e.activation(out=rt[s][:], in_=pss[s][:], func=AF.Ln, bias=EPS_AP[:], scale=1.0 / D_MODEL),
             r=[f"ss{s}"], w=[f"rt{s}"])
        P.op("act", lambda e, s=s: e.activation(out=rt[s][:], in_=rt[s][:], func=AF.Exp, scale=-0.5), r=[f"rt{s}"], w=[f"rt{s}"])
        for kc in range(KC):
            P.op("dve", lambda e, kc=kc, s=s, tok=tok: e.scalar_tensor_tensor(
                ot[s][:, kc, :], x2T[:, kc, tok], fg[:, kc:kc + 1], rt[s][:], ALU.mult, ALU.mult),
                r=[f"rt{s}"], w=[f"ot{s}"])
        P.dma("sp", outT[:, :, b * SEQ + tt * 512: b * SEQ + (tt + 1) * 512], ot[s][:], r=[f"ot{s}"], w=(), chan=f"out{s}")
    P.run()


def _consts():
    p = np.arange(128)
    c = {}
    c["c_ones"] = np.ones((128, 128), np.float32)
    c["c_tri"] = (p[:, None] >= p[None, :]).astype(np.float32)
    mD = np.zeros((128, 4, 512), np.float32)
    for i in range(4):
        for j in range(4):
            if j > i:
                mD[:, i, j * 128:(j + 1) * 128] = 1.0
            elif j == i:
                mD[:, i, j * 128:(j + 1) * 128] = (p[None, :] > p[:, None]).astype(np.float32)
    c["c_maskD"] = mD
    c["c_maskLE"] = (p[:, None] <= p[None, :]).astype(np.float32)
    c["c_ident"] = np.eye(128, dtype=np.float32)
    c["c_identb"] = np.eye(128, dtype=np.float32).astype(ml_dtypes.bfloat16)
    s4 = np.zeros((4, 4, 128), np.float32)
    for h in range(4):
        s4[h, h, :] = 1.0
    c["c_sel4"] = s4
    pid = np.full((128, 128), -1.0, np.float32)
    pid[:32, :] = np.arange(32, dtype=np.float32)[:, None]
    c["c_pid"] = pid
    return c


def _fm(v, nch):
    return np.ascontiguousarray(np.asarray(v, np.float32).reshape(nch, 128).T)


def make_in_maps(inp, cores=range(NCORES)):
    f = lambda a: np.ascontiguousarray(np.asarray(a, np.float32))
    shared = {
        "ada_w": f(inp["ada_w"][0]), "ada_bT": _fm(inp["ada_b"][0], 48),
        "n1g": _fm(inp["norm1_g"][0], 8), "n2g": _fm(inp["norm2_g"][0], 8), "fg": _fm(inp["final_g"], 8),
        "mlg": _fm(inp["ml_norm_g"][0], 8),
        "w_in": f(inp["w_in"][0]),
        "cwT": np.ascontiguousarray(np.asarray(inp["conv_w"][0], np.float32).reshape(4, 16, 128).transpose(2, 1, 0)),
        "cbT": _fm(inp["conv_b"][0], 16),
        "bi": f(inp["ml_b_i"][0]).reshape(4, 1), "bf": f(inp["ml_b_f"][0]).reshape(4, 1),
        "w_a": f(inp["w_branch_a"][0]), "w_b": f(inp["w_branch_b"][0]), "w_out": f(inp["w_out"][0]),
        "router_w": f(inp["router_w"][0]),
        "rb_rep": np.ascontiguousarray(np.broadcast_to(np.asarray(inp["router_b"][0], np.float32)[None, :], (128, NE))),
        "w1": f(inp["expert_w1"][0]),
        "b1T": np.ascontiguousarray(np.asarray(inp["expert_b1"][0], np.float32).reshape(NE, 16, 128).transpose(2, 0, 1)),
        "w2": f(inp["expert_w2"][0]), "b2": f(inp["expert_b2"][0]),
    }
    shared.update(_consts())
    x = np.asarray(inp["x"], np.float32)
    c = np.asarray(inp["c"], np.float32)
    maps = []
    for i in cores:
        xs = x[2 * i:2 * i + 2].reshape(NTOK, KC, 128)
        m = dict(shared)
        m["xT"] = np.ascontiguousarray(xs.transpose(2, 1, 0))
        m["cT"] = np.ascontiguousarray(c[2 * i:2 * i + 2].reshape(2, KC, 128).transpose(2, 1, 0))
        maps.append(m)
    return maps


def kernel(**inputs):
    nc = build()
    maps = make_in_maps(inputs)
    res = run_bass_kernel_spmd(nc, maps, core_ids=list(range(NCORES)))
    out = np.empty((16, SEQ, D_MODEL), np.float32)
    for i in range(NCORES):
        o = res.results[i]["outT"]
        out[2 * i:2 * i + 2] = o.transpose(2, 1, 0).reshape(2, SEQ, D_MODEL)
    return out
```

```python
import numpy as np
from contextlib import ExitStack
import ml_dtypes
import concourse.bass as bass
import concourse.mybir as mybir
from concourse.bass_utils import run_bass_kernel_spmd

F32 = mybir.dt.float32
BF16 = mybir.dt.bfloat16
F32R = mybir.dt.float32r
AF = mybir.ActivationFunctionType
ALU = mybir.AluOpType

NCORES = 8
D_MODEL = 1024
SEQ = 2048
NTOK = 2 * SEQ
NE = 32
EPS = 1e-6
KC = 8
COL_Q, COL_K, COL_V = 0, 1024, 2048
COL_MQ, COL_MK, COL_MV, COL_MO = 3072, 4096, 5120, 6144
COL_I, COL_F, COL_GA, COL_GB = 7168, 7172, 7176, 8200


class _Op:
    __slots__ = ("id", "eng", "fn", "deps", "chan", "chan_val", "sig")


class _SemPool:
    def __init__(self, nc):
        self.nc = nc
        self.esem = {e: nc.alloc_semaphore(f"eng_{e}") for e in ("pe", "act", "dve", "pool", "sp")}
        self.ecnt = {e: 0 for e in self.esem}
        self.csem = {}
        self.ccnt = {}

    def chan(self, c):
        if c not in self.csem:
            self.csem[c] = self.nc.alloc_semaphore(f"ch_{len(self.csem)}")
            self.ccnt[c] = 0
        return self.csem[c]


_POOLS = {}


class Prog:
    def __init__(self, nc, name):
        self.nc = nc
        self.name = name
        self.es = ExitStack()
        self.ops = []
        self.lastw = {}
        self.readers = {}
        self.chan_n = {}
        self.nsb = 0
        if id(nc) not in _POOLS:
            _POOLS.clear()
            _POOLS[id(nc)] = _SemPool(nc)
        self.pool = _POOLS[id(nc)]

    def sb(self, shape, dt=F32, name=None):
        self.nsb += 1
        return self.es.enter_context(self.nc.sbuf_tensor(f"{self.name}_s{self.nsb}_{name or ''}", list(shape), dt))

    def ps(self, shape=(128, 512), dt=F32, name=None):
        self.nsb += 1
        return self.es.enter_context(self.nc.psum_tensor(f"{self.name}_p{self.nsb}_{name or ''}", list(shape), dt))

    def op(self, eng, fn, r=(), w=(), chan=None):
        o = _Op()
        o.id = len(self.ops)
        o.eng = eng
        o.fn = fn
        o.chan = chan
        o.chan_val = 0
        o.sig = 0
        deps = {}
        for k in r:
            d = self.lastw.get(k)
            if d is not None:
                deps[d] = "raw"
        for k in w:
            d = self.lastw.get(k)
            if d is not None and d not in deps:
                deps[d] = "waw"
            for d in self.readers.get(k, {}).values():
                if d not in deps:
                    deps[d] = "war"
        o.deps = deps
        if chan is not None:
            self.pool.chan(chan)
            self.pool.ccnt[chan] += 1
            self.chan_n[chan] = self.pool.ccnt[chan]
            o.chan_val = 16 * self.chan_n[chan]
        self.ops.append(o)
        rk = (eng, chan)
        for k in r:
            self.readers.setdefault(k, {})[rk] = o.id
        for k in w:
            self.lastw[k] = o.id
            self.readers[k] = {}
        return o.id

    def mm(self, out, lhsT, rhs, start, stop, r, w, sgc=False):
        if sgc:
            return self.op("pe", lambda e: e.matmul(out, lhsT, rhs, start=start, stop=stop, skip_group_check=True), r, w)
        return self.op("pe", lambda e: e.matmul(out, lhsT, rhs, start=start, stop=stop), r, w)

    def dma(self, eng, out, in_, r, w, chan):
        return self.op(eng, lambda e: e.dma_start(out=out, in_=in_), r, w, chan=chan)

    def _skip(self, p, o, typ):
        return (p.chan is None and o.chan is None and p.eng == o.eng and p.eng == "pe")

    def run(self):
        nc = self.nc
        ops = self.ops
        engs = ("pe", "act", "dve", "pool", "sp")
        need = [False] * len(ops)
        for o in ops:
            for d, typ in o.deps.items():
                p = ops[d]
                if p.chan is not None or self._skip(p, o, typ):
                    continue
                need[d] = True
        cnt = self.pool.ecnt
        for o in ops:
            if o.chan is None and need[o.id]:
                cnt[o.eng] += 1
                o.sig = cnt[o.eng]
        with ExitStack() as es:
            esem = self.pool.esem
            csem = self.pool.csem
            by_eng = {e: [o for o in ops if o.eng == e] for e in engs}
            with nc.Block() as block:
                def emit(e, name):
                    waited = {}
                    for o in by_eng[name]:
                        want = {}
                        for d, typ in o.deps.items():
                            p = ops[d]
                            if p.chan is not None:
                                key, val = ("c", p.chan), p.chan_val
                            else:
                                if self._skip(p, o, typ):
                                    continue
                                key, val = ("e", p.eng), p.sig
                            if val > want.get(key, 0):
                                want[key] = val
                        for key, val in want.items():
                            if waited.get(key, 0) >= val:
                                continue
                            waited[key] = val
                            e.wait_ge(csem[key[1]] if key[0] == "c" else esem[key[1]], val)
                        ins = o.fn(e)
                        if o.chan is not None:
                            ins.then_inc(csem[o.chan], 16)
                        elif o.sig:
                            ins.then_inc(esem[name], 1)
                    if name == "sp":
                        for c, n in self.chan_n.items():
                            if waited.get(("c", c), 0) < 16 * n:
                                e.wait_ge(csem[c], 16 * n)

                @block.tensor
                def _(e):
                    emit(e, "pe")

                @block.scalar
                def _(e):
                    emit(e, "act")

                @block.vector
                def _(e):
                    emit(e, "dve")

                @block.gpsimd
                def _(e):
                    emit(e, "pool")

                @block.sync
                def _(e):
                    emit(e, "sp")
        self.es.close()


def _tap(nc, taps, name, ap, shape, dt=F32):
    if taps is None or name not in taps:
        return
    d = nc.dram_tensor("dbg_" + name, list(shape), dt, kind="ExternalOutput").ap()
    if id(nc) not in _POOLS:
        _POOLS.clear()
        _POOLS[id(nc)] = _SemPool(nc)
    pool = _POOLS[id(nc)]
    s = pool.chan("tap")
    pool.ccnt["tap"] += 1
    v = 16 * pool.ccnt["tap"]
    with nc.Block() as blk:
        @blk.sync
        def _(e):
            e.dma_start(out=d, in_=ap).then_inc(s, 16)
            e.wait_ge(s, v)


def build(upto=99, taps=None):
    nc = bass.Bass("TRN2", target_bir_lowering=False)
    D = {}

    def din(name, shape, dt=F32):
        D[name] = nc.dram_tensor(name, list(shape), dt, kind="ExternalInput").ap()

    din("xT", [128, KC, NTOK]); din("cT", [128, KC, 2]); din("ada_w", [1024, 6144]); din("ada_bT", [128, 48])
    din("n1g", [128, KC]); din("n2g", [128, KC]); din("fg", [128, KC]); din("mlg", [128, KC])
    din("w_in", [1024, 9224]); din("cwT", [128, 16, 4]); din("cbT", [128, 16]); din("bi", [4, 1]); din("bf", [4, 1])
    din("w_a", [1024, 1024]); din("w_b", [1024, 1024]); din("w_out", [1024, 1024])
    din("router_w", [1024, NE]); din("rb_rep", [128, NE])
    din("w1", [NE, 1024, 2048]); din("b1T", [128, NE, 16]); din("w2", [NE, 1024, 1024]); din("b2", [NE, 1024])
    din("c_ones", [128, 128]); din("c_tri", [128, 128]); din("c_maskD", [128, 4, 512]); din("c_maskLE", [128, 128])
    din("c_ident", [128, 128]); din("c_identb", [128, 128], BF16); din("c_sel4", [4, 4, 128]); din("c_pid", [128, 128])
    outT = nc.dram_tensor("outT", [128, KC, NTOK], F32, kind="ExternalOutput").ap()

    def wview(ap2d):
        return ap2d.rearrange("(kc p) n -> p kc n", p=128)

    with ExitStack() as top:
        def tsb(name, shape, dt=F32):
            return top.enter_context(nc.sbuf_tensor("g_" + name, list(shape), dt))

        modT = tsb("modT", [128, 48, 2])
        A1 = tsb("A1", [128, KC, 2]); A2 = tsb("A2", [128, KC, 2])
        ones = tsb("ones", [128, 128]); n1g = tsb("n1g", [128, KC]); n2g = tsb("n2g", [128, KC])
        fg = tsb("fg", [128, KC]); mlg = tsb("mlg", [128, KC])
        ident = tsb("ident", [128, 128])
        hT = tsb("hT", [128, KC, SEQ], BF16)
        Y = tsb("Y", [128, 2, KC, SEQ], BF16)
        yaT = Y[:, 0]
        ybT = Y[:, 1]
        x1T = Y[:].rearrange("p a k t -> p (a k t)").bitcast(F32).rearrange("p (k t) -> p k t", k=KC)

        P = Prog(nc, "p0")
        cT = P.sb([128, KC, 2], name="cT"); sc = P.sb([128, KC, 2], name="sc")
        abT = P.sb([128, 48], name="abT")
        wbuf = [P.sb([128, KC, 1024], name=f"adaw{i}") for i in range(2)]
        pm = [P.ps(name=f"pm{i}") for i in range(2)]
        for i, (t, nm) in enumerate([(ones, "c_ones"), (n1g, "n1g"), (n2g, "n2g"), (fg, "fg"), (mlg, "mlg"),
                                     (ident, "c_ident"), (cT, "cT"), (abT, "ada_bT")]):
            P.dma("sp", t[:], D[nm], r=(), w=[nm], chan="ld" + nm)
        P.op("act", lambda e: e.activation(out=sc[:], in_=cT[:], func=AF.Silu), r=["cT"], w=["sc"])
        adaw = wview(D["ada_w"])
        for m in range(6):
            wb = wbuf[m % 2]
            P.dma("sp", wb[:], adaw[:, :, m * 1024:(m + 1) * 1024], r=(), w=[f"adaw{m % 2}"], chan=f"adaw{m % 2}")
            pmm = pm[m % 2][:, 0:16].rearrange("p (j b) -> p j b", b=2)
            for jj in range(8):
                for kc in range(KC):
                    P.mm(pmm[:, jj, :], wb[:, kc, jj * 128:(jj + 1) * 128], sc[:, kc, :], kc == 0, kc == KC - 1,
                         r=[f"adaw{m % 2}", "sc"], w=[f"pm{m % 2}"])
            for b in range(2):
                P.op("dve", lambda e, m=m, b=b, pmm=pmm: e.tensor_tensor(
                    modT[:, m * 8:(m + 1) * 8, b], pmm[:, :, b], abT[:, m * 8:(m + 1) * 8], ALU.add),
                    r=[f"pm{m % 2}", "ada_bT"], w=["modT"])
        for b in range(2):
            P.op("dve", lambda e, b=b: e.scalar_tensor_tensor(A1[:, :, b], modT[:, 8:16, b], 1.0, n1g[:], ALU.add, ALU.mult),
                 r=["modT", "n1g"], w=["A1"])
            P.op("dve", lambda e, b=b: e.scalar_tensor_tensor(A2[:, :, b], modT[:, 32:40, b], 1.0, n2g[:], ALU.add, ALU.mult),
                 r=["modT", "n2g"], w=["A2"])
        P.run()
        _tap(nc, taps, "modT", modT[:], [128, 48, 2])
        _tap(nc, taps, "A1", A1[:], [128, KC, 2])

        def SH1(kc, b): return modT[:, 0 + kc, b:b + 1]
        def G1(kc, b): return modT[:, 16 + kc, b:b + 1]
        def SH2(kc, b): return modT[:, 24 + kc, b:b + 1]
        def G2(kc, b): return modT[:, 40 + kc, b:b + 1]

        def norm_phase(name, b, src_dram, src_sb, Aap, SHap, dst_bf, extra=None):
            P = Prog(nc, name)
            xt = [P.sb([128, KC, 512], name=f"xt{i}") for i in range(2)] if src_sb is None else None
            sq = [P.sb([128, KC, 512], name=f"sq{i}") for i in range(2)]
            rt = [P.sb([128, 512], name=f"rt{i}") for i in range(2)]
            rs = [P.sb([128, 512], name=f"rs{i}") for i in range(2)]
            tmp = [P.sb([128, 512], name=f"tmp{i}") for i in range(4)]
            pss = [P.ps(name=f"ss{i}") for i in range(2)]
            ctx = extra(P) if extra else None
            for tt in range(4):
                s = tt % 2
                tok = slice(tt * 512, (tt + 1) * 512)
                if src_sb is None:
                    P.dma("sp", xt[s][:], src_dram[:, :, b * SEQ + tt * 512: b * SEQ + (tt + 1) * 512], r=(), w=[f"xt{s}"], chan=f"xt{s}")
                    xin = xt[s]
                    xk = [f"xt{s}"]
                    xv = lambda kc, xin=xin: xin[:, kc, :]
                    xall = xin[:]
                else:
                    xk = [f"x1.{tt}"]
                    xv = lambda kc, tok=tok: src_sb[:, kc, tok]
                    xall = src_sb[:, :, tok]
                P.op("act", lambda e, s=s, xall=xall: e.activation(out=sq[s][:], in_=xall, func=AF.Square), r=xk, w=[f"sq{s}"])
                for kc in range(KC):
                    P.mm(pss[s][:], ones[:], sq[s][:, kc, :], kc == 0, kc == KC - 1, r=[f"sq{s}"], w=[f"ss{s}"])
                P.op("act", lambda e, s=s: e.activation(out=rt[s][:], in_=pss[s][:], func=AF.Ln, bias=EPS_AP[:], scale=1.0 / D_MODEL),
                     r=[f"ss{s}"], w=[f"rt{s}"])
                P.op("act", lambda e, s=s: e.activation(out=rs[s][:], in_=rt[s][:], func=AF.Exp, scale=-0.5), r=[f"rt{s}"], w=[f"rs{s}"])
                for kc in range(KC):
                    q = kc % 4
                    P.op("dve", lambda e, kc=kc, q=q, s=s, xv=xv: e.scalar_tensor_tensor(
                        tmp[q][:], xv(kc), Aap(kc, b), rs[s][:], ALU.mult, ALU.mult),
                        r=xk + [f"rs{s}"], w=[f"tmp{q}"])
                    if ctx is None:
                        P.op("act", lambda e, kc=kc, q=q, tok=tok: e.activation(
                            out=dst_bf[:, kc, tok], in_=tmp[q][:], func=AF.Identity, bias=SHap(kc, b), scale=1.0),
                            r=[f"tmp{q}"], w=[f"h.{tt}"])
                    else:
                        ctx["per_kc"](P, tt, kc, q, tmp[q], tok)
                if ctx is not None:
                    ctx["per_tile"](P, tt, tok)
            if ctx is not None and "final" in ctx:
                ctx["final"](P)
            P.run()

        EPS_AP = tsb("eps", [128, 1])
        P = Prog(nc, "pc")
        P.op("dve", lambda e: e.memset(EPS_AP[:], EPS), r=(), w=["eps"])
        P.run()

        for b in range(2):
            norm_phase(f"n1_{b}", b, D["xT"], None, lambda kc, b: A1[:, kc, b:b + 1], SH1, hT)
            if b == 0:
                _tap(nc, taps, "hT", hT[:], [128, KC, SEQ], BF16)
            if upto <= 1:
                continue
            scratch = Y[:, 0].rearrange("p k t -> p (k t)").bitcast(F32)
            mlstm_phase(nc, D, b, hT, ybT, scratch, wview, mlg, ones, ident, EPS_AP, taps)
            if b == 0:
                _tap(nc, taps, "ybT", ybT, [128, KC, SEQ], BF16)
            if upto <= 2:
                continue
            sb_attention(nc, D, b, hT, yaT, wview)
            if b == 0:
                _tap(nc, taps, "yaT", yaT, [128, KC, SEQ], BF16)
            if upto <= 3:
                continue
            with nc.sbuf_tensor(f"mergedT{b}", [128, KC, SEQ], BF16) as mergedT:
                merge_phase(nc, D, b, hT, yaT, ybT, mergedT, wview)
                if b == 0:
                    _tap(nc, taps, "mergedT", mergedT[:], [128, KC, SEQ], BF16)
                outproj_phase(nc, D, b, mergedT, x1T, G1, wview)
            if b == 0:
                _tap(nc, taps, "x1T", x1T, [128, KC, SEQ])
            if upto <= 4:
                continue
            with nc.sbuf_tensor(f"gT{b}", [128, SEQ], BF16) as gT:
                norm2_router_phase(nc, D, b, x1T, hT, gT, A2, SH2, G2, ones, ident, EPS_AP, wview)
                if b == 0:
                    _tap(nc, taps, "h2T", hT[:], [128, KC, SEQ], BF16)
                    _tap(nc, taps, "gT", gT[0:NE, :], [NE, SEQ], BF16)
                    _tap(nc, taps, "x1bT", x1T, [128, KC, SEQ])
                if upto <= 5:
                    continue
                moe_phase(nc, D, b, hT, x1T, gT, G2)
            if b == 0:
                _tap(nc, taps, "x2T", x1T, [128, KC, SEQ])
            final_phase(nc, D, b, x1T, fg, ones, EPS_AP, outT)
    return nc


def sb_attention(nc, D, b, hT, yaT, wview):
    P = Prog(nc, f"sb{b}")
    win = wview(D["w_in"])
    ones = P.sb([128, 128], name="ones"); tri = P.sb([128, 128], name="tri")
    maskD = P.sb([128, 4, 512], name="maskD")
    for t, nm in [(ones, "c_ones"), (tri, "c_tri"), (maskD, "c_maskD")]:
        P.dma("sp", t[:], D[nm], r=(), w=[nm], chan="ld" + nm)
    P.op("dve", lambda e: e.tensor_copy(trir[:], tri[:]), r=["c_tri"], w=["trir"])
    P.op("dve", lambda e: e.tensor_copy(onesr[:], ones[:]), r=["c_ones"], w=["onesr"])
    Wq = [P.sb([128, KC, 128], BF16, name=f"wq{i}") for i in range(2)]
    Wk = [P.sb([128, KC, 128], BF16, name=f"wk{i}") for i in range(2)]
    Wv = [P.sb([128, KC, 128], BF16, name=f"wv{i}") for i in range(2)]
    qT = [P.sb([128, SEQ], BF16, name=f"qT{i}") for i in range(2)]
    kTh_ = [[P.sb([128, SEQ], BF16, name=f"kT{i}_{hh}") for hh in range(2)] for i in range(2)]
    V = [P.sb([128, 16, 2, 128], BF16, name=f"V{i}") for i in range(2)]
    for i in range(2):
        P.op("pool", lambda e, i=i: e.memset(kTh_[i][0][64:128, :], 0.0), r=(), w=[f"kz{i}"])
        P.op("pool", lambda e, i=i: e.memset(kTh_[i][1][0:64, :], 0.0), r=(), w=[f"kz{i}"])
        P.op("pool", lambda e, i=i: e.memset(V[i][:], 0.0), r=(), w=[f"vz{i}"])
    et = [P.sb([128, 512], name=f"et{i}") for i in range(4)]
    spt = [P.sb([128, 512], F32R, name=f"spt{i}") for i in range(4)]
    ext = [P.sb([128, 512], name=f"ext{i}") for i in range(4)]
    AT = [P.sb([128, 512], BF16, name=f"AT{i}") for i in range(4)]
    R = [P.sb([128, 512], F32R, name=f"R{i}") for i in range(2)]
    trir = P.sb([128, 128], F32R, name="trir"); onesr = P.sb([128, 128], F32R, name="onesr")
    psz = [P.ps(name=f"z{i}") for i in range(2)]
    pscs = [P.ps(name=f"cs{i}") for i in range(2)]
    psy = [P.ps(name=f"y{i}") for i in range(2)]
    pspj = [P.ps(name=f"pj{i}") for i in range(2)]
    npj = [0]
    it = 0

    def proj_chunks(hp):
        s = hp % 2
        ch = []

        def dmas():
            for (W, col, nm) in [(Wq, COL_Q, "wq"), (Wk, COL_K, "wk"), (Wv, COL_V, "wv")]:
                P.dma("pool", W[s][:], win[:, :, col + hp * 128: col + (hp + 1) * 128], r=(), w=[f"{nm}{s}"], chan=f"{nm}{s}")
        ch.append(dmas)
        for tt in range(4):
            for nm in ("q", "k"):
                def c_(tt=tt, nm=nm):
                    tok = slice(tt * 512, (tt + 1) * 512)
                    W = Wq if nm == "q" else Wk
                    pj = npj[0] % 2
                    npj[0] += 1
                    for kc in range(KC):
                        P.mm(pspj[pj][:], W[s][:, kc, :], hT[:, kc, tok], kc == 0, kc == KC - 1,
                             r=[f"w{nm}{s}", f"h.{tt}"], w=[f"pj{pj}"])
                    if nm == "q":
                        P.op("dve", lambda e, pj=pj, o_=qT[s][:, tok]: e.tensor_scalar(o_, pspj[pj][:], 0.125, None, ALU.mult),
                             r=[f"pj{pj}"], w=[f"qT{s}.{tt}"])
                    else:
                        P.op("dve", lambda e, pj=pj, o_=kTh_[s][0][0:64, tok]: e.tensor_copy(o_, pspj[pj][0:64, :]),
                             r=[f"pj{pj}", f"kz{s}"], w=[f"kT{s}.{tt}"])
                        P.op("dve", lambda e, pj=pj, o_=kTh_[s][1][64:128, tok]: e.tensor_copy(o_, pspj[pj][64:128, :]),
                             r=[f"pj{pj}", f"kz{s}"], w=[f"kT{s}.{tt}"])
                ch.append(c_)
        for g4 in range(4):
            def c_(g4=g4):
                pj = npj[0] % 2
                npj[0] += 1
                pv = pspj[pj][:].rearrange("p (j c) -> p j c", c=128)
                for j in range(4):
                    blk = g4 * 4 + j
                    for kc in range(KC):
                        P.mm(pv[:, j, :], hT[:, kc, blk * 128:(blk + 1) * 128], Wv[s][:, kc, :], kc == 0, kc == KC - 1,
                             r=[f"wv{s}", f"h.{g4}"], w=[f"pj{pj}"])
                P.op("dve", lambda e, o_=V[s][:, g4 * 4:(g4 + 1) * 4, 0, 0:64], pv=pv: e.tensor_copy(o_, pv[:, :, 0:64]),
                     r=[f"pj{pj}", f"vz{s}"], w=[f"V{s}.{g4}"])
                P.op("dve", lambda e, o_=V[s][:, g4 * 4:(g4 + 1) * 4, 1, 64:128], pv=pv: e.tensor_copy(o_, pv[:, :, 64:128]),
                     r=[f"pj{pj}", f"vz{s}"], w=[f"V{s}.{g4}"])
            ch.append(c_)
        return ch

    for c_ in proj_chunks(0):
        c_()
    for hp in range(8):
        s = hp % 2
        nxt = proj_chunks(hp + 1) if hp + 1 < 8 else []
        iters = []
        for qc in range(4):
            nkb = 4 * qc + 4
            for idx, kb in enumerate(range(nkb - 1, -1, -1)):
                for hh in range(2):
                    iters.append((qc, idx, kb, hh))
        base = it
        it += len(iters)
        triS = maskD[:, 0, 0:128]

        def prm(t, iters=iters, base=base):
            qc, idx, kb, hh = iters[t]
            g = base + t
            c0 = 128 * max(0, kb - 4 * qc)
            return qc, idx, kb, hh, g % 4, g % 2, c0

        def SA_mm(t, s=s):
            qc, idx, kb, hh, u, z, c0 = prm(t)
            P.mm(psz[z][:, c0:], kTh_[s][hh][:, kb * 128:(kb + 1) * 128], qT[s][:, qc * 512 + c0:(qc + 1) * 512], True, True,
                 r=[f"kT{s}.{kb // 4}", f"qT{s}.{qc}"], w=[f"z{z}"])

        def SA_exp(t):
            qc, idx, kb, hh, u, z, c0 = prm(t)
            P.op("act", lambda e, u=u, z=z, c0=c0: e.activation(out=et[u][:, c0:], in_=psz[z][:, c0:], func=AF.Exp), r=[f"z{z}"], w=[f"et{u}"])

        def SA_ln(t):
            qc, idx, kb, hh, u, z, c0 = prm(t)
            P.op("act", lambda e, u=u, c0=c0: e.activation(out=spt[u][:, c0:], in_=et[u][:, c0:], func=AF.Ln, bias=1.0, scale=1.0),
                 r=[f"et{u}"], w=[f"spt{u}"])

        def SA_mask(t):
            qc, idx, kb, hh, u, z, c0 = prm(t)
            if kb >= 4 * qc:
                P.op("dve", lambda e, u=u, c0=c0: e.tensor_tensor(spt[u][:, c0:c0 + 128], spt[u][:, c0:c0 + 128], triS, ALU.mult),
                     r=[f"spt{u}", "c_maskD"], w=[f"spt{u}"])
                P.op("dve", lambda e, u=u, c0=c0: e.tensor_tensor(et[u][:, c0:c0 + 128], et[u][:, c0:c0 + 128], triS, ALU.mult),
                     r=[f"et{u}", "c_maskD"], w=[f"et{u}"])

        def SB_mm(t):
            qc, idx, kb, hh, u, z, c0 = prm(t)
            P.mm(pscs[z][:, c0:], trir[:], spt[u][:, c0:], True, idx == 0, r=[f"spt{u}", "trir"], w=[f"cs{z}"])
            if idx > 0:
                P.mm(pscs[z][:, c0:], onesr[:], R[hh][:, c0:], False, True, r=[f"R{hh}", "onesr"], w=[f"cs{z}"])

        def SB_pool(t):
            qc, idx, kb, hh, u, z, c0 = prm(t)
            if kb > 0:
                if idx == 0:
                    if c0 > 0:
                        P.op("dve", lambda e, hh=hh, c0=c0: e.tensor_copy(R[hh][:, 0:c0], maskD[:, 3, 0:c0]), r=["c_maskD"], w=[f"R{hh}"])
                    P.op("dve", lambda e, u=u, hh=hh, c0=c0: e.tensor_copy(R[hh][:, c0:], spt[u][:, c0:]), r=[f"spt{u}"], w=[f"R{hh}"])
                else:
                    P.op("dve", lambda e, u=u, hh=hh, c0=c0: e.tensor_tensor(R[hh][:, c0:], R[hh][:, c0:], spt[u][:, c0:], ALU.add),
                         r=[f"R{hh}", f"spt{u}"], w=[f"R{hh}"])

        def SB_exp(t):
            qc, idx, kb, hh, u, z, c0 = prm(t)
            P.op("act", lambda e, u=u, z=z, c0=c0: e.activation(out=ext[u][:, c0:], in_=pscs[z][:, c0:], func=AF.Exp, scale=-1.0),
                 r=[f"cs{z}"], w=[f"ext{u}"])

        def SB_mult(t):
            qc, idx, kb, hh, u, z, c0 = prm(t)
            P.op("dve", lambda e, u=u, c0=c0: e.tensor_tensor(AT[u][:, c0:], et[u][:, c0:], ext[u][:, c0:], ALU.mult),
                 r=[f"et{u}", f"ext{u}"], w=[f"AT{u}"])

        def SC(t, s=s, hp=hp):
            qc, idx, kb, hh, u, z, c0 = prm(t)
            yb = (hp * 4 + qc) % 2
            P.mm(psy[yb][:, c0:], V[s][:, kb, hh, :], AT[u][:, c0:], idx == 0 and hh == 0, kb == 0 and hh == 1,
                 r=[f"V{s}.{kb // 4}", f"AT{u}"], w=[f"y{yb}.0", f"y{yb}.1"], sgc=True)
            if kb == 0 and hh == 1:
                P.op("dve", lambda e, yb=yb, o_=yaT[:, hp, qc * 512:(qc + 1) * 512]: e.tensor_copy(o_, psy[yb][:]),
                     r=[f"y{yb}.0", f"y{yb}.1"], w=[f"ya.{qc}"])

        n_pairs = len(iters) // 2
        for p in range(n_pairs + 2):
            if nxt and p % 3 == 0:
                nxt.pop(0)()
            if p < n_pairs:
                for f in (SA_mm, SA_exp, SA_ln, SA_mask):
                    f(2 * p)
                    f(2 * p + 1)
            if 0 <= p - 1 < n_pairs:
                for f in (SB_mm, SB_pool, SB_exp, SB_mult):
                    f(2 * (p - 1))
                    f(2 * (p - 1) + 1)
            if 0 <= p - 2 < n_pairs:
                SC(2 * (p - 2))
                SC(2 * (p - 2) + 1)
        while nxt:
            nxt.pop(0)()
    P.run()


def mlstm_phase(nc, D, b, hT, ybT, scratch, wview, mlg, ones, ident, EPS_AP, taps=None):
    win = wview(D["w_in"])
    NUMv = scratch[:, 0:4096].rearrange("p (a t) -> p a t", a=2)
    XP = scratch[:, 4096:6147]
    with ExitStack() as mes:
        def msb(name, shape, dt=F32):
            return mes.enter_context(nc.sbuf_tensor(f"ml{b}_{name}", list(shape), dt))
        Mrow = msb("Mrow", [4, SEQ]); WIrow = msb("WIrow", [4, SEQ]); EMRrow = msb("EMRrow", [4, SEQ])
        GT = msb("GT", [128, 16, 4]); WST = msb("WST", [128, 16, 4]); DEC = msb("DEC", [4, 16])
        sel4 = msb("sel4", [4, 4, 128]); maskLE = msb("maskLE", [128, 128]); identb = msb("identb", [128, 128], BF16)
        cwT = msb("cwT", [128, 16, 4]); cbT = msb("cbT", [128, 16])
        P = Prog(nc, f"mla{b}")
        for t, nm in [(sel4, "c_sel4"), (maskLE, "c_maskLE"), (identb, "c_identb"), (cwT, "cwT"), (cbT, "cbT")]:
            P.dma("sp", t[:], D[nm], r=(), w=[nm], chan="ld" + nm)
        bi = P.sb([4, 1], name="bi"); bfv = P.sb([4, 1], name="bf"); nbf = P.sb([4, 1], name="nbf")
        P.dma("sp", bi[:], D["bi"], r=(), w=["bi"], chan="ldbi")
        P.dma("sp", bfv[:], D["bf"], r=(), w=["bf"], chan="ldbf")
        Wif32 = P.sb([128, KC, 8], name="Wif32")
        Wi = P.sb([128, KC, 4], BF16, name="Wi"); Wf = P.sb([128, KC, 4], BF16, name="Wf")
        P.dma("sp", Wif32[:], win[:, :, COL_I:COL_I + 8], r=(), w=["Wif32"], chan="ldWif")
        P.op("dve", lambda e: e.tensor_copy(Wi[:], Wif32[:, :, 0:4]), r=["Wif32"], w=["Wi"])
        P.op("dve", lambda e: e.tensor_copy(Wf[:], Wif32[:, :, 4:8]), r=["Wif32"], w=["Wf"])
        Grow = P.sb([4, SEQ], name="Grow"); SP = P.sb([4, SEQ], name="SP"); CS = P.sb([4, SEQ], name="CS")
        WSrow = P.sb([4, SEQ], name="WSrow"); Mprev = P.sb([4, 16], name="Mprev")
        psI = [P.ps(name=f"psI{i}") for i in range(2)]
        psF = [P.ps(name=f"psF{i}") for i in range(2)]
        psT = P.ps(name="psT"); psT2 = P.ps(name="psT2")
        P.op("dve", lambda e: e.tensor_scalar(nbf[:], bfv[:], -1.0, None, ALU.mult), r=["bf"], w=["nbf"])
        for tt in range(4):
            s = tt % 2
            tok = slice(tt * 512, (tt + 1) * 512)
            for kc in range(KC):
                P.mm(psI[s][0:4, :], Wi[:, kc, :], hT[:, kc, tok], kc == 0, kc == KC - 1, r=["Wi"], w=[f"psI{s}"])
            for kc in range(KC):
                P.mm(psF[s][0:4, :], Wf[:, kc, :], hT[:, kc, tok], kc == 0, kc == KC - 1, r=["Wf"], w=[f"psF{s}"])
            P.op("dve", lambda e, s=s, tok=tok: e.tensor_scalar(Grow[:, tok], psI[s][0:4, :], bi[:, 0:1], None, ALU.add),
                 r=[f"psI{s}", "bi"], w=["Grow"])
            P.op("act", lambda e, s=s, tok=tok: e.activation(out=SP[:, tok], in_=psF[s][0:4, :], func=AF.Exp, bias=nbf[:, 0:1], scale=-1.0),
                 r=[f"psF{s}", "nbf"], w=["SP"])
        P.op("act", lambda e: e.activation(out=SP[:], in_=SP[:], func=AF.Ln, bias=1.0, scale=1.0), r=["SP"], w=["SP"])
        P.op("dve", lambda e: e.tensor_tensor_scan(CS[:], SP[:], SP[:], 0.0, ALU.add, ALU.max), r=["SP"], w=["CS"])
        P.op("dve", lambda e: e.tensor_tensor(Grow[:], Grow[:], CS[:], ALU.add), r=["Grow", "CS"], w=["Grow"])
        P.op("dve", lambda e: e.tensor_tensor_scan(Mrow[:], Grow[:], Grow[:], 0.0, ALU.max, ALU.max), r=["Grow"], w=["Mrow"])
        M3 = Mrow[:].rearrange("p (c t) -> p c t", t=128)
        G3 = Grow[:].rearrange("p (c t) -> p c t", t=128)
        WI3 = WIrow[:].rearrange("p (c t) -> p c t", t=128)
        WS3 = WSrow[:].rearrange("p (c t) -> p c t", t=128)
        P.op("dve", lambda e: e.memset(Mprev[:, 0:1], 0.0), r=(), w=["Mprev0"])
        P.op("dve", lambda e: e.tensor_copy(Mprev[:, 1:16], M3[:, 0:15, 127]), r=["Mrow"], w=["Mprev"])
        for c in range(16):
            P.op("dve", lambda e, c=c: e.tensor_scalar(WI3[:, c, :], M3[:, c, :], Mprev[:, c:c + 1], None, ALU.subtract),
                 r=["Mrow", "Mprev", "Mprev0"], w=["WIrow"])
            P.op("dve", lambda e, c=c: e.tensor_scalar(WS3[:, c, :], G3[:, c, :], M3[:, c, 127:128], None, ALU.subtract),
                 r=["Mrow", "Grow"], w=["WSrow"])
        P.op("act", lambda e: e.activation(out=WIrow[:], in_=WIrow[:], func=AF.Exp, scale=-1.0), r=["WIrow"], w=["WIrow"])
        P.op("act", lambda e: e.activation(out=WSrow[:], in_=WSrow[:], func=AF.Exp), r=["WSrow"], w=["WSrow"])
        P.op("dve", lambda e: e.tensor_tensor(EMRrow[:], CS[:], Mrow[:], ALU.subtract), r=["CS", "Mrow"], w=["EMRrow"])
        P.op("act", lambda e: e.activation(out=EMRrow[:], in_=EMRrow[:], func=AF.Exp), r=["EMRrow"], w=["EMRrow"])
        P.op("dve", lambda e: e.tensor_tensor(DEC[:], Mprev[:], M3[:, :, 127], ALU.subtract), r=["Mprev", "Mprev0", "Mrow"], w=["DEC"])
        P.op("act", lambda e: e.activation(out=DEC[:], in_=DEC[:], func=AF.Exp), r=["DEC"], w=["DEC"])
        pT = psT[:, 0:64].rearrange("p (c h) -> p c h", h=4)
        pT2 = psT2[:, 0:64].rearrange("p (c h) -> p c h", h=4)
        for blk in range(16):
            P.op("pe", lambda e, blk=blk: e.transpose(pT[:, blk, :], Grow[0:4, blk * 128:(blk + 1) * 128], ident[0:4, 0:4]),
                 r=["Grow"], w=["psT"])
            P.op("pe", lambda e, blk=blk: e.transpose(pT2[:, blk, :], WSrow[0:4, blk * 128:(blk + 1) * 128], ident[0:4, 0:4]),
                 r=["WSrow"], w=["psT2"])
        P.op("dve", lambda e: e.tensor_copy(GT[:], pT), r=["psT"], w=["GT"])
        P.op("dve", lambda e: e.tensor_copy(WST[:], pT2), r=["psT2"], w=["WST"])
        P.run()
        _tap(nc, taps, f"Mrow{b}", Mrow[:], [4, SEQ]); _tap(nc, taps, f"WIrow{b}", WIrow[:], [4, SEQ])
        _tap(nc, taps, f"EMRrow{b}", EMRrow[:], [4, SEQ]); _tap(nc, taps, f"GT{b}", GT[:], [128, 16, 4])
        _tap(nc, taps, f"WST{b}", WST[:], [128, 16, 4]); _tap(nc, taps, f"DEC{b}", DEC[:], [4, 16])

        import os
        ML_STAGE = float(os.environ.get("ML_STAGE", "9"))
        for h in range(4 if ML_STAGE > 0 else 0):
            P = Prog(nc, f"mlh{b}_{h}")
            try:
              _ml_head(P, nc, D, b, h, hT, ybT, scratch, NUMv, XP, win, mlg, ones, EPS_AP, Mrow, WIrow, EMRrow, GT, WST, DEC,
                       sel4, maskLE, identb, cwT, cbT, ML_STAGE)
            except _StopStage:
              pass
            P.run()


class _StopStage(Exception):
    pass


def _ml_head(P, nc, D, b, h, hT, ybT, scratch, NUMv, XP, win, mlg, ones, EPS_AP, Mrow, WIrow, EMRrow, GT, WST, DEC,
             sel4, maskLE, identb, cwT, cbT, ML_STAGE):
    def chk(k):
        if ML_STAGE < k:
            raise _StopStage()
    if True:
        if True:
            Wb = [P.sb([128, KC, 256], BF16, name=f"Wb{i}") for i in range(2)]
            ACC = [NUMv[:, 0, :], NUMv[:, 1, :]]
            qTh = P.sb([128, 2, SEQ], BF16, name="qTh"); kTh = P.sb([128, 2, SEQ], BF16, name="kTh")
            QW = P.sb([128, 2, SEQ], BF16, name="QW")
            KW = P.sb([128, 16, 256], BF16, name="KW"); VA = P.sb([128, 16, 258], BF16, name="VA")
            WT = P.sb([128, 16, 128], BF16, name="WT")
            DEN = P.sb([1, SEQ], name="DEN")
            C = P.sb([128, 2, 258], name="C"); Cbf = P.sb([128, 2, 258], BF16, name="Cbf")
            DECsb = P.sb([128, 16], name="DECsb")
            PT = [P.sb([128, 128], BF16, name=f"PT{i}") for i in range(2)]
            B = [P.ps(name=f"B{i}") for i in range(8)]
            Bb = [bk[:].bitcast(BF16) for bk in B]
            nb_ = [0]

            def nbank():
                nb_[0] += 1
                return nb_[0] % 8
            wslot = [0]

            def loadW(col):
                sl = wslot[0] % 2
                wslot[0] += 1
                P.dma("pool", Wb[sl][:], win[:, :, col + h * 256: col + (h + 1) * 256], r=(), w=[f"Wb{sl}"], chan=f"Wb{sl}")
                return sl
            XPb = P.sb([128, SEQ + 3], name="XPb")
            XPs = [XP, XPb[:]]
            P.op("dve", lambda e: e.memset(XP[:, 0:3], 0.0), r=(), w=["XP0"])
            P.op("dve", lambda e: e.memset(XPb[:, 0:3], 0.0), r=(), w=["XP0"])
            P.op("dve", lambda e: e.memset(VA[:, :, 256:257], 1.0), r=(), w=["VA1"])
            for wi, (col, dstT, cbase) in enumerate([(COL_MQ, qTh, 0), (COL_MK, kTh, 8)]):
                sl = loadW(col)
                chk(1.1)
                for dch in range(2):
                    cidx = cbase + h * 2 + dch
                    xi = (wi * 2 + dch) % 2
                    XPc = XPs[xi]
                    for tt in range(4):
                        tok = slice(tt * 512, (tt + 1) * 512)
                        bk = nbank()
                        for kc in range(KC):
                            P.mm(B[bk][:], Wb[sl][:, kc, dch * 128:(dch + 1) * 128], hT[:, kc, tok], kc == 0, kc == KC - 1,
                                 r=[f"Wb{sl}"], w=[f"B{bk}"])
                        P.op("act", lambda e, bk=bk, tt=tt, XPc=XPc: e.activation(out=XPc[:, 3 + tt * 512: 3 + (tt + 1) * 512], in_=B[bk][:], func=AF.Copy),
                             r=[f"B{bk}"], w=[f"XP{xi}.{tt}"])
                    chk(1.2)
                    a = ACC[dch]
                    xk = [f"XP{xi}.{t_}" for t_ in range(4)] + ["XP0"]
                    P.op("dve", lambda e, a=a, cidx=cidx, XPc=XPc: e.tensor_scalar(a, XPc[:, 0:SEQ], cwT[:, cidx, 0:1], cbT[:, cidx:cidx + 1], ALU.mult, ALU.add),
                         r=xk, w=[f"N{dch}"])
                    for tap in range(1, 4):
                        P.op("dve", lambda e, a=a, cidx=cidx, tap=tap, XPc=XPc: e.scalar_tensor_tensor(
                            a, XPc[:, tap:tap + SEQ], cwT[:, cidx, tap:tap + 1], a, ALU.mult, ALU.add),
                            r=xk + [f"N{dch}"], w=[f"N{dch}"])
                    chk(1.3)
                    if wi == 0:
                        P.op("act", lambda e, a=a, o_=qTh[:, dch, :]: e.activation(out=o_, in_=a, func=AF.Silu), r=[f"N{dch}"], w=[f"qTh{dch}"])
                    else:
                        P.op("act", lambda e, a=a: e.activation(out=a, in_=a, func=AF.Silu), r=[f"N{dch}"], w=[f"N{dch}"])
                        P.op("dve", lambda e, a=a, o_=kTh[:, dch, :]: e.tensor_scalar(o_, a, 1.0 / 16.0, None, ALU.mult),
                             r=[f"N{dch}"], w=[f"kTh{dch}"])
            chk(2)
            sl = loadW(COL_MV)
            for sb2 in range(8):
                bk = nbank()
                pv = B[bk][:].rearrange("p (j c) -> p j c", c=256)
                for j in range(2):
                    blk = sb2 * 2 + j
                    for kc in range(KC):
                        P.mm(pv[:, j, :], hT[:, kc, blk * 128:(blk + 1) * 128], Wb[sl][:, kc, :], kc == 0, kc == KC - 1,
                             r=[f"Wb{sl}"], w=[f"B{bk}"])
                P.op("act", lambda e, pv=pv, o_=VA[:, sb2 * 2:sb2 * 2 + 2, 0:256]: e.activation(out=o_, in_=pv, func=AF.Copy),
                     r=[f"B{bk}"], w=[f"VA.{sb2}"])
            slo = loadW(COL_MO)
            chk(3)
            TD = ACC[1]
            TD3 = TD.rearrange("p (c t) -> p c t", t=128)
            for tt in range(4):
                tok = slice(tt * 512, (tt + 1) * 512)
                bk = nbank()
                P.mm(B[bk][:], sel4[0:4, h, :], Mrow[0:4, tok], True, True, r=(), w=[f"B{bk}"])
                for j in range(4):
                    c = tt * 4 + j
                    P.op("dve", lambda e, bk=bk, j=j, c=c: e.tensor_scalar(
                        TD3[:, c, :], B[bk][:, j * 128:(j + 1) * 128], GT[:, c, h:h + 1], 0.0, ALU.subtract, ALU.max),
                        r=[f"B{bk}", "kTh1"], w=["N1"])
                bk = nbank()
                P.mm(B[bk][:], sel4[0:4, h, :], WIrow[0:4, tok], True, True, r=(), w=[f"B{bk}"])
                for dch in range(2):
                    P.op("dve", lambda e, bk=bk, o_=QW[:, dch, tok], i_=qTh[:, dch, tok]: e.tensor_tensor(o_, i_, B[bk][:], ALU.mult),
                         r=[f"B{bk}", f"qTh{dch}"], w=[f"QW{dch}"])
            P.op("act", lambda e: e.activation(out=WT[:].rearrange("p c t -> p (c t)"), in_=TD, func=AF.Exp, scale=-1.0), r=["N1"], w=["WT"])
            for c in range(16):
                P.op("pool", lambda e, c=c: e.tensor_tensor(WT[:, c, :], WT[:, c, :], maskLE[:], ALU.mult), r=["WT"], w=["WT"])
            bk = nbank()
            P.mm(B[bk][:, 0:16], sel4[0:4, h, :], DEC[0:4, :], True, True, r=(), w=[f"B{bk}"])
            P.op("act", lambda e, bk=bk: e.activation(out=DECsb[:], in_=B[bk][:, 0:16], func=AF.Copy), r=[f"B{bk}"], w=["DECsb"])
            for sbk in range(16):
                bk = nbank()
                for dch in range(2):
                    P.op("pe", lambda e, bk=bk, dch=dch, sbk=sbk: e.transpose(
                        Bb[bk][:, dch * 128:(dch + 1) * 128], kTh[:, dch, sbk * 128:(sbk + 1) * 128], identb[:]),
                        r=[f"kTh{dch}"], w=[f"B{bk}"])
                P.op("dve", lambda e, bk=bk, sbk=sbk: e.tensor_scalar(KW[:, sbk, :], Bb[bk][:, 0:256], WST[:, sbk, h:h + 1], None, ALU.mult),
                     r=[f"B{bk}"], w=[f"KW.{sbk}"])
            chk(4)
            bS = [0, 1]; bN = [2, 3]; bD = 4; bC = [5, 6]
            def s_pt(c):
                blk = slice(c * 128, (c + 1) * 128)
                s = c % 2
                for dch in range(2):
                    P.mm(B[bS[s]][:, 0:128], kTh[:, dch, blk], qTh[:, dch, blk], dch == 0, dch == 1,
                         r=[f"kTh{dch}", f"qTh{dch}"], w=[f"B{bS[s]}"])
                P.op("dve", lambda e, s=s, c=c: e.tensor_tensor(PT[s][:], B[bS[s]][:, 0:128], WT[:, c, :], ALU.mult),
                     r=[f"B{bS[s]}", "WT"], w=[f"PT{s}"])
            s_pt(0)
            for c in range(16):
                blk = slice(c * 128, (c + 1) * 128)
                cs4 = slice((c % 4) * 128, (c % 4 + 1) * 128)
                s = c % 2
                if c + 1 < 16:
                    s_pt(c + 1)
                for ech in range(3):
                    if ech < 2:
                        outp = B[bN[ech]][:, cs4]; wk = f"B{bN[ech]}"
                        cs_ = slice(ech * 128, (ech + 1) * 128)
                    else:
                        outp = B[bD][0:1, cs4]; wk = f"B{bD}"
                        cs_ = slice(256, 257)
                    if c > 0:
                        for dch in range(2):
                            P.mm(outp, Cbf[:, dch, cs_], QW[:, dch, blk], dch == 0, False, r=[f"Cbf{dch}", f"QW{dch}"], w=[wk])
                    P.mm(outp, VA[:, c, cs_], PT[s][:], c == 0, True, r=[f"VA.{c // 2}", "VA1", f"PT{s}"], w=[wk])
                if c % 4 == 3:
                    t4 = slice((c // 4) * 512, (c // 4 + 1) * 512)
                    for ech in range(2):
                        P.op("act", lambda e, ech=ech, t4=t4: e.activation(out=NUMv[:, ech, t4], in_=B[bN[ech]][:], func=AF.Copy),
                             r=[f"B{bN[ech]}"], w=[f"NUM{ech}.{c // 4}"])
                    P.op("dve", lambda e, t4=t4: e.tensor_copy(DEN[0:1, t4], B[bD][0:1, :]), r=[f"B{bD}"], w=[f"DEN.{c // 4}"])
                if c < 15:
                    for dch in range(2):
                        P.mm(B[bC[dch]][:, 0:257], KW[:, c, dch * 128:(dch + 1) * 128], VA[:, c, 0:257], True, True,
                             r=[f"KW.{c}", f"VA.{c // 2}", "VA1"], w=[f"B{bC[dch]}"])
                        if c == 0:
                            P.op("dve", lambda e, dch=dch: e.tensor_copy(C[:, dch, 0:257], B[bC[dch]][:, 0:257]),
                                 r=[f"B{bC[dch]}"], w=[f"C{dch}"])
                        else:
                            P.op("dve", lambda e, dch=dch, c=c: e.scalar_tensor_tensor(
                                C[:, dch, 0:257], C[:, dch, 0:257], DECsb[:, c:c + 1], B[bC[dch]][:, 0:257], ALU.mult, ALU.add),
                                r=[f"B{bC[dch]}", f"C{dch}", "DECsb"], w=[f"C{dch}"])
                        P.op("act", lambda e, dch=dch: e.activation(out=Cbf[:, dch, 0:257], in_=C[:, dch, 0:257], func=AF.Copy),
                             r=[f"C{dch}"], w=[f"Cbf{dch}"])
            chk(5)
            T = [scratch[:, 4096 + i * 512: 4096 + (i + 1) * 512] for i in range(4)] + \
                [scratch[:, 6148 + i * 512: 6148 + (i + 1) * 512] for i in range(3)]
            T.append(P.sb([128, 512], name="T7")[:]); T.append(P.sb([128, 512], name="T8")[:])
            for tt in range(4):
                tok = slice(tt * 512, (tt + 1) * 512)
                bA = nbank()
                P.mm(B[bA][:], ones[0:1, :], DEN[0:1, tok], True, True, r=[f"DEN.{tt}"], w=[f"B{bA}"])
                bB = nbank()
                P.mm(B[bB][:], sel4[0:4, h, :], EMRrow[0:4, tok], True, True, r=(), w=[f"B{bB}"])
                P.op("act", lambda e, bB=bB: e.activation(out=T[0], in_=B[bB][:], func=AF.Copy), r=[f"B{bB}"], w=["T0"])
                P.op("act", lambda e, bA=bA: e.activation(out=T[1], in_=B[bA][:], func=AF.Abs), r=[f"B{bA}"], w=["T1"])
                P.op("dve", lambda e: e.tensor_tensor(T[1], T[1], T[0], ALU.max), r=["T1", "T0"], w=["T1"])
                P.op("act", lambda e: e.activation(out=T[1], in_=T[1], func=AF.Ln), r=["T1"], w=["T1"])
                P.op("act", lambda e: e.activation(out=T[1], in_=T[1], func=AF.Exp, scale=-1.0), r=["T1"], w=["T1"])
                for ech in range(2):
                    P.op("dve", lambda e, ech=ech, tok=tok: e.tensor_tensor(NUMv[:, ech, tok], NUMv[:, ech, tok], T[1], ALU.mult),
                         r=["T1", f"NUM{ech}.{tt}"], w=[f"NUM{ech}.{tt}"])
                    P.op("act", lambda e, ech=ech, tok=tok: e.activation(out=T[2 + ech], in_=NUMv[:, ech, tok], func=AF.Square),
                         r=[f"NUM{ech}.{tt}"], w=[f"T{2 + ech}"])
                bC2 = nbank()
                for ech in range(2):
                    P.mm(B[bC2][:], ones[:], T[2 + ech], ech == 0, ech == 1, r=[f"T{2 + ech}"], w=[f"B{bC2}"])
                P.op("act", lambda e, bC2=bC2: e.activation(out=T[4], in_=B[bC2][:], func=AF.Ln, bias=EPS_AP[:], scale=1.0 / 256.0),
                     r=[f"B{bC2}"], w=["T4"])
                P.op("act", lambda e: e.activation(out=T[4], in_=T[4], func=AF.Exp, scale=-0.5), r=["T4"], w=["T4"])
                for ech in range(2):
                    bo = nbank()
                    for kc in range(KC):
                        P.mm(B[bo][:], Wb[slo][:, kc, ech * 128:(ech + 1) * 128], hT[:, kc, tok], kc == 0, kc == KC - 1,
                             r=[f"Wb{slo}"], w=[f"B{bo}"])
                    P.op("act", lambda e, bo=bo, ech=ech: e.activation(out=T[5 + ech], in_=B[bo][:], func=AF.Sigmoid), r=[f"B{bo}"], w=[f"T{5 + ech}"])
                    P.op("dve", lambda e, ech=ech, tok=tok: e.scalar_tensor_tensor(
                        T[7 + ech], NUMv[:, ech, tok], mlg[:, h * 2 + ech: h * 2 + ech + 1], T[4], ALU.mult, ALU.mult),
                        r=[f"NUM{ech}.{tt}", "T4"], w=[f"T{7 + ech}"])
                    P.op("pool", lambda e, ech=ech, o_=ybT[:, h * 2 + ech, tok]: e.tensor_tensor(o_, T[7 + ech], T[5 + ech], ALU.mult),
                         r=[f"T{7 + ech}", f"T{5 + ech}"], w=[f"yb.{tt}"])


def merge_phase(nc, D, b, hT, yaT, ybT, mergedT, wview):
    P = Prog(nc, f"mg{b}")
    win = wview(D["w_in"]); wa = wview(D["w_a"]); wb = wview(D["w_b"])
    W = {nm: [P.sb([128, KC, 128], BF16, name=f"{nm}{i}") for i in range(2)] for nm in ("wa", "wb", "wga", "wgb")}
    sa = [P.sb([128, 512], name=f"sa{i}") for i in range(2)]
    sg = [P.sb([128, 512], name=f"sg{i}") for i in range(2)]
    B = [P.ps(name=f"B{i}") for i in range(8)]
    it = 0
    for fo in range(8):
        s = fo % 2
        fsl = slice(fo * 128, (fo + 1) * 128)
        P.dma("pool", W["wa"][s][:], wa[:, :, fsl], r=(), w=[f"wa{s}"], chan=f"mwa{s}")
        P.dma("pool", W["wb"][s][:], wb[:, :, fsl], r=(), w=[f"wb{s}"], chan=f"mwb{s}")
        P.dma("pool", W["wga"][s][:], win[:, :, COL_GA + fo * 128: COL_GA + (fo + 1) * 128], r=(), w=[f"wga{s}"], chan=f"mwga{s}")
        P.dma("pool", W["wgb"][s][:], win[:, :, COL_GB + fo * 128: COL_GB + (fo + 1) * 128], r=(), w=[f"wgb{s}"], chan=f"mwgb{s}")
        for tt in range(4):
            tok = slice(tt * 512, (tt + 1) * 512)
            g = it % 2
            it += 1
            bA, bB, bGA, bGB = 4 * g, 4 * g + 1, 4 * g + 2, 4 * g + 3
            for (bk, nm, src) in [(bA, "wa", yaT), (bB, "wb", ybT), (bGA, "wga", hT), (bGB, "wgb", hT)]:
                for kc in range(KC):
                    P.mm(B[bk][:], W[nm][s][:, kc, :], src[:, kc, tok], kc == 0, kc == KC - 1, r=[f"{nm}{s}"], w=[f"B{bk}"])
            P.op("act", lambda e, g=g, bGA=bGA: e.activation(out=sa[g][:], in_=B[bGA][:], func=AF.Sigmoid), r=[f"B{bGA}"], w=[f"sa{g}"])
            P.op("act", lambda e, g=g, bGB=bGB: e.activation(out=sg[g][:], in_=B[bGB][:], func=AF.Sigmoid), r=[f"B{bGB}"], w=[f"sg{g}"])
            P.op("dve", lambda e, g=g, bA=bA: e.tensor_tensor(sa[g][:], sa[g][:], B[bA][:], ALU.mult), r=[f"sa{g}", f"B{bA}"], w=[f"sa{g}"])
            P.op("dve", lambda e, g=g, bB=bB: e.tensor_tensor(sg[g][:], sg[g][:], B[bB][:], ALU.mult), r=[f"sg{g}", f"B{bB}"], w=[f"sg{g}"])
            P.op("pool", lambda e, g=g, o_=mergedT[:, fo, tok]: e.tensor_tensor(o_, sa[g][:], sg[g][:], ALU.add),
                 r=[f"sa{g}", f"sg{g}"], w=[f"mg.{tt}"])
    P.run()


def outproj_phase(nc, D, b, mergedT, x1T, G1, wview):
    P = Prog(nc, f"op{b}")
    wo = P.sb([128, KC, 1024], BF16, name="wo")
    P.dma("pool", wo[:], wview(D["w_out"]), r=(), w=["wo"], chan="ldwo")
    xt = [P.sb([128, KC, 512], name=f"xt{i}") for i in range(2)]
    B = [P.ps(name=f"B{i}") for i in range(4)]
    it = 0
    for tt in range(4):
        s = tt % 2
        tok = slice(tt * 512, (tt + 1) * 512)
        P.dma("sp", xt[s][:], D["xT"][:, :, b * SEQ + tt * 512: b * SEQ + (tt + 1) * 512], r=(), w=[f"xt{s}"], chan=f"oxt{s}")
        for fo in range(8):
            bk = it % 4
            it += 1
            for kc in range(KC):
                P.mm(B[bk][:], wo[:, kc, fo * 128:(fo + 1) * 128], mergedT[:, kc, tok], kc == 0, kc == KC - 1, r=["wo"], w=[f"B{bk}"])
            P.op("dve", lambda e, bk=bk, fo=fo, s=s, tok=tok: e.scalar_tensor_tensor(
                x1T[:, fo, tok], B[bk][:], G1(fo, b), xt[s][:, fo, :], ALU.mult, ALU.add),
                r=[f"B{bk}", f"xt{s}"], w=[f"x1.{tt}"])
    P.run()


def norm2_router_phase(nc, D, b, x1T, h2T, gT, A2, SH2, G2, ones, ident, EPS_AP, wview):
    P = Prog(nc, f"n2_{b}")
    rw = P.sb([128, KC, NE], name="rw"); rb = P.sb([128, NE], name="rb"); b2n = P.sb([NE, 1024], name="b2n")
    gTb = gT
    gT = P.sb([NE, SEQ], name="gTf")
    P.op("pool", lambda e: e.memset(gTb[:], 0.0), r=(), w=["gTbz"])
    P.dma("sp", rw[:], wview(D["router_w"]), r=(), w=["rw"], chan="ldrw")
    P.dma("sp", rb[:], D["rb_rep"], r=(), w=["rb"], chan="ldrb")
    P.dma("sp", b2n[:], D["b2"], r=(), w=["b2n"], chan="ldb2")
    sq = [P.sb([128, KC, 512], name=f"sq{i}") for i in range(2)]
    h2f = [P.sb([128, KC, 512], name=f"h2f{i}") for i in range(2)]
    rt = [P.sb([128, 512], name=f"rt{i}") for i in range(2)]
    tmp = [P.sb([128, 512], name=f"tmp{i}") for i in range(4)]
    lg = P.sb([128, 4, NE], name="lg"); m8 = P.sb([128, 4, 8], name="m8"); negm = P.sb([128, 4], name="negm")
    ex = P.sb([128, 4, NE], name="ex"); msk = P.sb([128, 4, NE], name="msk"); ssum = P.sb([128, 4], name="ssum")
    gates = P.sb([128, 4, NE], name="gates")
    pss = [P.ps(name=f"ss{i}") for i in range(2)]
    psL = [P.ps(name=f"psL{i}") for i in range(2)]
    psG = [P.ps(name=f"psG{i}") for i in range(2)]
    psB = [P.ps(name=f"psB{i}") for i in range(2)]
    for tt in range(4):
        s = tt % 2
        tok = slice(tt * 512, (tt + 1) * 512)
        xk = [f"x1.{tt}"]
        P.op("act", lambda e, s=s, tok=tok: e.activation(out=sq[s][:], in_=x1T[:, :, tok], func=AF.Square), r=xk, w=[f"sq{s}"])
        for kc in range(KC):
            P.mm(pss[s][:], ones[:], sq[s][:, kc, :], kc == 0, kc == KC - 1, r=[f"sq{s}"], w=[f"ss{s}"])
        P.op("act", lambda e, s=s: e.activation(out=rt[s][:], in_=pss[s][:], func=AF.Ln, bias=EPS_AP[:], scale=1.0 / D_MODEL),
             r=[f"ss{s}"], w=[f"rt{s}"])
        P.op("act", lambda e, s=s: e.activation(out=rt[s][:], in_=rt[s][:], func=AF.Exp, scale=-0.5), r=[f"rt{s}"], w=[f"rt{s}"])
        for kc in range(KC):
            q = kc % 4
            P.op("dve", lambda e, kc=kc, q=q, s=s, tok=tok: e.scalar_tensor_tensor(
                tmp[q][:], x1T[:, kc, tok], A2[:, kc, b:b + 1], rt[s][:], ALU.mult, ALU.mult),
                r=xk + [f"rt{s}"], w=[f"tmp{q}"])
            P.op("act", lambda e, kc=kc, q=q, s=s: e.activation(
                out=h2f[s][:, kc, :], in_=tmp[q][:], func=AF.Identity, bias=SH2(kc, b), scale=1.0),
                r=[f"tmp{q}"], w=[f"h2f{s}.{kc}"])
            P.op("act", lambda e, kc=kc, q=q, tok=tok: e.activation(
                out=h2T[:, kc, tok], in_=tmp[q][:], func=AF.Identity, bias=SH2(kc, b), scale=1.0),
                r=[f"tmp{q}"], w=[f"h2.{tt}"])
        pl4 = psL[s][:, 0:4 * NE].rearrange("p (j n) -> p j n", n=NE)
        for blk in range(4):
            for kc in range(KC):
                P.mm(pl4[:, blk, :], h2f[s][:, kc, blk * 128:(blk + 1) * 128], rw[:, kc, :], kc == 0, kc == KC - 1,
                     r=[f"h2f{s}.{kc}", "rw"], w=[f"psL{s}"])
        for blk in range(4):
            P.op("dve", lambda e, blk=blk, pl4=pl4: e.tensor_tensor(lg[:, blk, :], pl4[:, blk, :], rb[:], ALU.add),
                 r=[f"psL{s}", "rb"], w=[f"lg{blk}"])
            P.op("dve", lambda e, blk=blk: e.max(m8[:, blk, :], lg[:, blk, :]), r=[f"lg{blk}"], w=[f"m8{blk}"])
            P.op("dve", lambda e, blk=blk: e.tensor_scalar(negm[:, blk:blk + 1], m8[:, blk, 0:1], -1.0, None, ALU.mult),
                 r=[f"m8{blk}"], w=[f"negm{blk}"])
            P.op("act", lambda e, blk=blk: e.activation(out=ex[:, blk, :], in_=lg[:, blk, :], func=AF.Exp, bias=negm[:, blk:blk + 1], scale=1.0),
                 r=[f"lg{blk}", f"negm{blk}"], w=[f"ex{blk}"])
            P.op("dve", lambda e, blk=blk: e.tensor_scalar(msk[:, blk, :], lg[:, blk, :], m8[:, blk, 3:4], None, ALU.is_ge),
                 r=[f"lg{blk}", f"m8{blk}"], w=[f"msk{blk}"])
            P.op("dve", lambda e, blk=blk: e.tensor_tensor(ex[:, blk, :], ex[:, blk, :], msk[:, blk, :], ALU.mult),
                 r=[f"ex{blk}", f"msk{blk}"], w=[f"ex{blk}"])
            P.op("dve", lambda e, blk=blk: e.reduce_sum(ssum[:, blk:blk + 1], ex[:, blk, :], mybir.AxisListType.X),
                 r=[f"ex{blk}"], w=[f"ssum{blk}"])
            P.op("dve", lambda e, blk=blk: e.reciprocal(ssum[:, blk:blk + 1], ssum[:, blk:blk + 1]), r=[f"ssum{blk}"], w=[f"ssum{blk}"])
            P.op("dve", lambda e, blk=blk: e.tensor_scalar(gates[:, blk, :], ex[:, blk, :], ssum[:, blk:blk + 1], None, ALU.mult),
                 r=[f"ex{blk}", f"ssum{blk}"], w=[f"gates{blk}"])
            P.op("pe", lambda e, blk=blk, s=s: e.transpose(psG[s][0:NE, blk * 128:(blk + 1) * 128], gates[:, blk, :], ident[:]),
                 r=[f"gates{blk}"], w=[f"psG{s}"])
        P.op("act", lambda e, s=s, tok=tok: e.activation(out=gT[0:NE, tok], in_=psG[s][0:NE, :], func=AF.Copy), r=[f"psG{s}"], w=[f"gT.{tt}"])
        P.op("dve", lambda e, tok=tok: e.tensor_copy(gTb[0:NE, tok], gT[0:NE, tok]), r=[f"gT.{tt}", "gTbz"], w=[f"gTb.{tt}"])
        for fo in range(8):
            bb = fo % 2
            P.mm(psB[bb][:], b2n[0:NE, fo * 128:(fo + 1) * 128], gT[0:NE, tok], True, True, r=["b2n", f"gT.{tt}"], w=[f"psB{bb}"])
            P.op("dve", lambda e, bb=bb, fo=fo, tok=tok: e.scalar_tensor_tensor(
                x1T[:, fo, tok], psB[bb][:], G2(fo, b), x1T[:, fo, tok], ALU.mult, ALU.add),
                r=[f"psB{bb}", f"x1.{tt}"], w=[f"x1.{tt}"])
    P.run()


def moe_phase(nc, D, b, h2T, x1T, gT, G2):
    P = Prog(nc, f"moe{b}")
    w1 = D["w1"].rearrange("e (kc p) n -> e p kc n", p=128)
    w2 = D["w2"].rearrange("e (kc p) n -> e p kc n", p=128)
    b1 = P.sb([128, NE, 16], name="b1"); pid = P.sb([128, 128], name="pid")
    P.dma("sp", b1[:], D["b1T"], r=(), w=["b1"], chan="ldb1")
    P.dma("sp", pid[:], D["c_pid"], r=(), w=["pid"], chan="ldpid")
    P.op("dve", lambda e_: e_.tensor_scalar(b1[:, :, 8:16], b1[:, :, 8:16], 1.0, None, ALU.add), r=["b1"], w=["b1"])
    sel = [P.sb([128, 128], BF16, name=f"sel{i}") for i in range(2)]
    Wg = [P.sb([128, KC, 512], BF16, name=f"Wg{i}") for i in range(3)]
    Wl = [P.sb([128, KC, 512], BF16, name=f"Wl{i}") for i in range(3)]
    W2 = [P.sb([128, 4, 1024], BF16, name=f"W2{i}") for i in range(3)]
    actT = [P.sb([128, 4, 512], BF16, name=f"act{i}") for i in range(3)]
    gsb = [P.sb([128, 512], BF16, name=f"gsb{i}") for i in range(2)]
    Ta = [P.sb([128, 512], name=f"Ta{i}") for i in range(2)]
    Tb = [P.sb([128, 512], name=f"Tb{i}") for i in range(2)]
    Tc = [P.sb([128, 512], name=f"Tc{i}") for i in range(2)]
    pg = [P.ps(name=f"pg{i}") for i in range(2)]
    pl = [P.ps(name=f"pl{i}") for i in range(2)]
    po = [P.ps(name=f"po{i}") for i in range(3)]
    pgt = [P.ps(name=f"pgt{i}") for i in range(1)]
    units = [(e, hf) for e in range(NE) for hf in range(2)]

    def load(u):
        e, hf = units[u]
        r = u % 3
        P.dma("pool", Wg[r][:], w1[e][:, :, hf * 512:(hf + 1) * 512], r=(), w=[f"Wg{r}"], chan=f"Wg{r}")
        P.dma("pool", Wl[r][:], w1[e][:, :, 1024 + hf * 512: 1024 + (hf + 1) * 512], r=(), w=[f"Wl{r}"], chan=f"Wl{r}")
        P.dma("pool", W2[r][:], w2[e][:, hf * 4:(hf + 1) * 4, :], r=(), w=[f"W2{r}"], chan=f"W2{r}")
    load(0)
    steps = [(u, tt) for u in range(len(units)) for tt in range(4)]

    def SA_pre(t):
        u, tt = steps[t]
        e, hf = units[u]
        if tt == 0:
            if u + 1 < len(units):
                load(u + 1)
            if hf == 0:
                P.op("dve", lambda e_, e=e: e_.tensor_scalar(sel[e % 2][:], pid[:], float(e), None, ALU.is_equal), r=["pid"], w=[f"sel{e % 2}"])
        tok = slice(tt * 512, (tt + 1) * 512)
        ga = t % 2
        P.mm(pgt[0][:], sel[e % 2][:], gT[:, tok], True, True, r=[f"sel{e % 2}"], w=["pgt0"])
        P.op("act", lambda e_, ga=ga: e_.activation(out=gsb[ga][:], in_=pgt[0][:], func=AF.Copy, scale=1.0 / 1.702), r=["pgt0"], w=[f"gsb{ga}"])

    def SA_head(t, j):
        u, tt = steps[t]
        e, hf = units[u]
        r = u % 3
        tok = slice(tt * 512, (tt + 1) * 512)
        jj = hf * 4 + j
        z = j % 2
        for kc in range(KC):
            P.mm(pg[z][:], Wg[r][:, kc, j * 128:(j + 1) * 128], h2T[:, kc, tok], kc == 0, kc == KC - 1, r=[f"Wg{r}"], w=[f"pg{z}"])
        for kc in range(KC):
            P.mm(pl[z][:], Wl[r][:, kc, j * 128:(j + 1) * 128], h2T[:, kc, tok], kc == 0, kc == KC - 1, r=[f"Wl{r}"], w=[f"pl{z}"])
        P.op("dve", lambda e_, z=z, e=e, jj=jj: e_.tensor_scalar(Ta[z][:], pg[z][:], b1[:, e, jj:jj + 1], 7.0, ALU.add, ALU.min),
             r=[f"pg{z}", "b1"], w=[f"Ta{z}"])
        P.op("act", lambda e_, z=z: e_.activation(out=Tb[z][:], in_=Ta[z][:], func=AF.Silu, scale=1.702), r=[f"Ta{z}"], w=[f"Tb{z}"])
        P.op("dve", lambda e_, z=z, e=e, jj=jj: e_.tensor_scalar(Tc[z][:], pl[z][:], b1[:, e, 8 + jj:9 + jj], -6.0, ALU.add, ALU.max),
             r=[f"pl{z}", "b1"], w=[f"Tc{z}"])

    def SA_tail(t, j):
        z = j % 2
        a = t % 3
        ga = t % 2
        P.op("dve", lambda e_, z=z: e_.scalar_tensor_tensor(Tc[z][:], Tc[z][:], 8.0, Tb[z][:], ALU.min, ALU.mult),
             r=[f"Tb{z}", f"Tc{z}"], w=[f"Tc{z}"])
        P.op("dve", lambda e_, z=z, a=a, j=j, ga=ga: e_.tensor_tensor(actT[a][:, j, :], Tc[z][:], gsb[ga][:], ALU.mult),
             r=[f"Tc{z}", f"gsb{ga}"], w=[f"act{a}.{j}"])

    def SB(t):
        u, tt = steps[t]
        r = u % 3
        tok = slice(tt * 512, (tt + 1) * 512)
        a = t % 3
        for fo in range(8):
            y = (t * 8 + fo) % 3
            for j in range(4):
                P.mm(po[y][:], W2[r][:, j, fo * 128:(fo + 1) * 128], actT[a][:, j, :], j == 0, j == 3,
                     r=[f"W2{r}", f"act{a}.{j}"], w=[f"po{y}"])
            P.op("dve", lambda e_, y=y, fo=fo, tok=tok: e_.scalar_tensor_tensor(
                x1T[:, fo, tok], po[y][:], G2(fo, b), x1T[:, fo, tok], ALU.mult, ALU.add),
                r=[f"po{y}", f"x2.{tt}.{fo}"], w=[f"x2.{tt}.{fo}"])

    n_st = len(steps)
    for t in range(n_st + 2):
        act_ = t < n_st
        if act_:
            SA_pre(t)
            SA_head(t, 0)
            SA_head(t, 1)
            SA_tail(t, 0)
        if t >= 2:
            SB(t - 2)
        if act_:
            SA_head(t, 2)
            SA_tail(t, 1)
            SA_head(t, 3)
            SA_tail(t, 2)
            SA_tail(t, 3)
    P.run()


def final_phase(nc, D, b, x2T, fg, ones, EPS_AP, outT):
    P = Prog(nc, f"fin{b}")
    sq = [P.sb([128, KC, 512], name=f"sq{i}") for i in range(2)]
    ot = [P.sb([128, KC, 512], name=f"ot{i}") for i in range(2)]
    rt = [P.sb([128, 512], name=f"rt{i}") for i in range(2)]
    pss = [P.ps(name=f"ss{i}") for i in range(2)]
    for tt in range(4):
        s = tt % 2
        tok = slice(tt * 512, (tt + 1) * 512)
        P.op("act", lambda e, s=s, tok=tok: e.activation(out=sq[s][:], in_=x2T[:, :, tok], func=AF.Square), r=(), w=[f"sq{s}"])
        for kc in range(KC):
            P.mm(pss[s][:], ones[:], sq[s][:, kc, :], kc == 0, kc == KC - 1, r=[f"sq{s}"], w=[f"ss{s}"])
        P.op("act", lambda e, s=s: e.activation(out=rt[s][:], in_=pss[s][:], func=AF.Ln, bias=EPS_AP[:], scale=1.0 / D_MODEL),
             r=[f"ss{s}"], w=[f"rt{s}"])
        P.op("act", lambda e, s=s: e.activation(out=rt[s][:], in_=rt[s][:], func=AF.Exp, scale=-0.5), r=[f"rt{s}"], w=[f"rt{s}"])
        for kc in range(KC):
            P.op("dve", lambda e, kc=kc, s=s, tok=tok: e.scalar_tensor_tensor(
                ot[s][:, kc, :], x2T[:, kc, tok], fg[:, kc:kc + 1], rt[s][:], ALU.mult, ALU.mult),
                r=[f"rt{s}"], w=[f"ot{s}"])
        P.dma("sp", outT[:, :, b * SEQ + tt * 512: b * SEQ + (tt + 1) * 512], ot[s][:], r=[f"ot{s}"], w=(), chan=f"out{s}")
    P.run()


def _consts():
    p = np.arange(128)
    c = {}
    c["c_ones"] = np.ones((128, 128), np.float32)
    c["c_tri"] = (p[:, None] >= p[None, :]).astype(np.float32)
    mD = np.zeros((128, 4, 512), np.float32)
    for i in range(4):
        for j in range(4):
            if j > i:
                mD[:, i, j * 128:(j + 1) * 128] = 1.0
            elif j == i:
                mD[:, i, j * 128:(j + 1) * 128] = (p[None, :] > p[:, None]).astype(np.float32)
    c["c_maskD"] = mD
    c["c_maskLE"] = (p[:, None] <= p[None, :]).astype(np.float32)
    c["c_ident"] = np.eye(128, dtype=np.float32)
    c["c_identb"] = np.eye(128, dtype=np.float32).astype(ml_dtypes.bfloat16)
    s4 = np.zeros((4, 4, 128), np.float32)
    for h in range(4):
        s4[h, h, :] = 1.0
    c["c_sel4"] = s4
    pid = np.full((128, 128), -1.0, np.float32)
    pid[:32, :] = np.arange(32, dtype=np.float32)[:, None]
    c["c_pid"] = pid
    return c


def _fm(v, nch):
    return np.ascontiguousarray(np.asarray(v, np.float32).reshape(nch, 128).T)


def make_in_maps(inp, cores=range(NCORES)):
    f = lambda a: np.ascontiguousarray(np.asarray(a, np.float32))
    shared = {
        "ada_w": f(inp["ada_w"][0]), "ada_bT": _fm(inp["ada_b"][0], 48),
        "n1g": _fm(inp["norm1_g"][0], 8), "n2g": _fm(inp["norm2_g"][0], 8), "fg": _fm(inp["final_g"], 8),
        "mlg": _fm(inp["ml_norm_g"][0], 8),
        "w_in": f(inp["w_in"][0]),
        "cwT": np.ascontiguousarray(np.asarray(inp["conv_w"][0], np.float32).reshape(4, 16, 128).transpose(2, 1, 0)),
        "cbT": _fm(inp["conv_b"][0], 16),
        "bi": f(inp["ml_b_i"][0]).reshape(4, 1), "bf": f(inp["ml_b_f"][0]).reshape(4, 1),
        "w_a": f(inp["w_branch_a"][0]), "w_b": f(inp["w_branch_b"][0]), "w_out": f(inp["w_out"][0]),
        "router_w": f(inp["router_w"][0]),
        "rb_rep": np.ascontiguousarray(np.broadcast_to(np.asarray(inp["router_b"][0], np.float32)[None, :], (128, NE))),
        "w1": f(inp["expert_w1"][0]),
        "b1T": np.ascontiguousarray(np.asarray(inp["expert_b1"][0], np.float32).reshape(NE, 16, 128).transpose(2, 0, 1)),
        "w2": f(inp["expert_w2"][0]), "b2": f(inp["expert_b2"][0]),
    }
    shared.update(_consts())
    x = np.asarray(inp["x"], np.float32)
    c = np.asarray(inp["c"], np.float32)
    maps = []
    for i in cores:
        xs = x[2 * i:2 * i + 2].reshape(NTOK, KC, 128)
        m = dict(shared)
        m["xT"] = np.ascontiguousarray(xs.transpose(2, 1, 0))
        m["cT"] = np.ascontiguousarray(c[2 * i:2 * i + 2].reshape(2, KC, 128).transpose(2, 1, 0))
        maps.append(m)
    return maps


def kernel(**inputs):
    nc = build()
    maps = make_in_maps(inputs)
    res = run_bass_kernel_spmd(nc, maps, core_ids=list(range(NCORES)))
    out = np.empty((16, SEQ, D_MODEL), np.float32)
    for i in range(NCORES):
        o = res.results[i]["outT"]
        out[2 * i:2 * i + 2] = o.transpose(2, 1, 0).reshape(2, SEQ, D_MODEL)
    return out
```
